# Optimizing a Trainium2 kernel written in Bass

```python
import jax
import jax.numpy as jnp
from jax import lax
import numpy as np


D_MODEL = 1024
BATCH = 8
SEQ = 4096
DEPTH = 2

CHUNK = 64
Q_BLOCK = 128
N_MEM = 256
MEM_HEADS = 4
MEM_W = D_MODEL // 4
MEM_HD = MEM_W // MEM_HEADS
SELF_W = D_MODEL - MEM_W
GLA_HEADS = 4
GLA_KDIM = SELF_W // 2
GLA_DK = GLA_KDIM // GLA_HEADS
GLA_DV = SELF_W // GLA_HEADS
GLA_GATE_RANK = 16
GLA_TAU = 16.0
FOX_HD = 64
FOX_HEADS = SELF_W // FOX_HD
N_EXPERTS = 16
N_GROUPS = 4
EXPERTS_PER_GROUP = N_EXPERTS // N_GROUPS
TOP_K = 2
D_EXPERT = D_MODEL // 2
N_A_LAYERS = DEPTH // 2
N_B_LAYERS = DEPTH - N_A_LAYERS
DN_ALPHA = (2.0 * DEPTH) ** 0.25
DN_BETA = (8.0 * DEPTH) ** -0.25
LN_EPS = 1e-5
RMS_EPS = 1e-6
A_IN = 2 * GLA_KDIM + 2 * SELF_W + GLA_GATE_RANK + MEM_W
A_SPLITS = (GLA_KDIM, 2 * GLA_KDIM, 2 * GLA_KDIM + SELF_W, 2 * GLA_KDIM + SELF_W + GLA_GATE_RANK, 2 * GLA_KDIM + 2 * SELF_W + GLA_GATE_RANK)
B_IN = 2 * SELF_W + MEM_W
B_SPLITS = (SELF_W, 2 * SELF_W)
KV_SHARED = 2 * SELF_W + FOX_HEADS
KV_SPLITS = (SELF_W, 2 * SELF_W)

kernel_name = 'yoco_gla_fox_grouped_moe_block'


def layer_norm(x, g, b):
    xf = x.astype(jnp.float32)
    mu = jnp.mean(xf, axis=-1, keepdims=True)
    var = jnp.mean(jnp.square(xf - mu), axis=-1, keepdims=True)
    return ((xf - mu) * lax.rsqrt(var + LN_EPS) * g + b).astype(x.dtype)


def rms_norm(x, g):
    xf = x.astype(jnp.float32)
    return xf * lax.rsqrt(jnp.mean(xf * xf, axis=-1, keepdims=True) + RMS_EPS) * g


def split_heads(t, n):
    return t.reshape(t.shape[:-1] + (n, t.shape[-1] // n))


def gla_chunked(q, k, v, log_a):
    B, S, H, dk = q.shape
    dv = v.shape[-1]
    nc = S // CHUNK
    qc = q.reshape(B, nc, CHUNK, H, dk)
    kc = k.reshape(B, nc, CHUNK, H, dk)
    vc = v.reshape(B, nc, CHUNK, H, dv)
    b = jnp.cumsum(log_a.reshape(B, nc, CHUNK, H, dk), axis=2)
    b_last = b[:, :, -1]
    q_dec = qc * jnp.exp(b)
    k_inv = kc * jnp.exp(-b)
    k_end = kc * jnp.exp(b_last[:, :, None] - b)
    causal = jnp.tril(jnp.ones((CHUNK, CHUNK), dtype=bool))
    att = jnp.einsum('bnthk,bnshk->bnhts', q_dec, k_inv)
    att = jnp.where(causal, att, 0.0)
    o_intra = jnp.einsum('bnhts,bnshv->bnthv', att, vc)
    chunk_kv = jnp.einsum('bnshk,bnshv->bnhkv', k_end, vc)

    def step(state, inp):
        decay, kv = inp
        return decay[..., None] * state + kv, state

    state0 = jnp.zeros((B, H, dk, dv), dtype=chunk_kv.dtype)
    _, states = lax.scan(step, state0, (jnp.moveaxis(jnp.exp(b_last), 1, 0), jnp.moveaxis(chunk_kv, 1, 0)))
    states = jnp.moveaxis(states, 0, 1)
    o_inter = jnp.einsum('bnthk,bnhkv->bnthv', q_dec, states)
    return (o_intra + o_inter).reshape(B, S, H, dv)


def fox_attention(q, k, v, cum_logf):
    S = q.shape[2]
    scale = FOX_HD ** -0.5
    outs = []
    for blk in range(S // Q_BLOCK):
        lo = blk * Q_BLOCK
        hi = lo + Q_BLOCK
        logits = jnp.einsum('bhqd,bhkd->bhqk', q[:, :, lo:hi], k[:, :, :hi]).astype(jnp.float32) * scale
        logits = logits + cum_logf[:, :, lo:hi, None] - cum_logf[:, :, None, :hi]
        mask = (lo + jnp.arange(Q_BLOCK))[:, None] >= jnp.arange(hi)[None, :]
        p = jax.nn.softmax(jnp.where(mask, logits, -jnp.inf), axis=-1)
        outs.append(jnp.einsum('bhqk,bhkd->bhqd', p.astype(v.dtype), v[:, :, :hi]))
    return jnp.concatenate(outs, axis=2)


def mem_attention(q, mk, mv):
    logits = jnp.einsum('bshd,bmhd->bhsm', q, mk).astype(jnp.float32) * (MEM_HD ** -0.5)
    p = jax.nn.softmax(logits, axis=-1)
    return jnp.einsum('bhsm,bmhd->bshd', p.astype(mv.dtype), mv)


def grouped_moe(x, w_router, b_router, w_gate, w_up, w_down):
    B, S, D = x.shape
    xt = x.reshape(B * S, D)
    n = xt.shape[0]
    affinity = jax.nn.sigmoid((xt @ w_router).astype(jnp.float32))
    sel = (affinity + b_router.astype(jnp.float32)).reshape(n, N_GROUPS, EXPERTS_PER_GROUP)
    group_score = jnp.sum(lax.top_k(sel, TOP_K)[0], axis=-1)
    g_idx = jnp.argmax(group_score, axis=-1)
    in_group = jnp.take_along_axis(sel, jnp.broadcast_to(g_idx[:, None, None], (n, 1, EXPERTS_PER_GROUP)), axis=1)[:, 0]
    _, local = lax.top_k(in_group, TOP_K)
    e_idx = g_idx[:, None] * EXPERTS_PER_GROUP + local
    w = jnp.take_along_axis(affinity, e_idx, axis=-1)
    w = w / jnp.sum(w, axis=-1, keepdims=True)
    gates = jnp.sum(jax.nn.one_hot(e_idx, N_EXPERTS, dtype=jnp.float32) * w[..., None], axis=1).astype(x.dtype)
    out = jnp.zeros_like(xt)
    for e in range(N_EXPERTS):
        h = jax.nn.silu(xt @ w_gate[e]) * (xt @ w_up[e])
        out = out + gates[:, e:e + 1] * (h @ w_down[e])
    return out.reshape(B, S, D)


def setup_inputs(seed: int = 0) -> dict:
    key = jax.random.key(seed)
    ks = iter(jax.random.split(key, 32))

    def nrm(shape, scale):
        return jax.random.normal(next(ks), shape, jnp.float32) * scale

    d = D_MODEL
    x = nrm((BATCH, SEQ, d), 1.0)
    mem = nrm((BATCH, N_MEM, d), 1.0)
    col_a = jnp.concatenate([jnp.ones((2 * GLA_KDIM,)), jnp.full((SELF_W,), DN_BETA), jnp.ones((GLA_GATE_RANK + SELF_W + MEM_W,))])
    w_in_a = nrm((N_A_LAYERS, d, A_IN), d ** -0.5) * col_a
    w_gate_up_a = nrm((N_A_LAYERS, GLA_GATE_RANK, GLA_KDIM), GLA_GATE_RANK ** -0.5)
    b_gate_a = nrm((N_A_LAYERS, GLA_KDIM), 0.1)
    gla_norm_g = 1.0 + nrm((N_A_LAYERS, GLA_DV), 0.02)
    w_in_b = nrm((N_B_LAYERS, d, B_IN), d ** -0.5)
    q_norm_g = 1.0 + nrm((N_B_LAYERS, FOX_HD), 0.02)
    col_kv = jnp.concatenate([jnp.ones((SELF_W,)), jnp.full((SELF_W,), DN_BETA), jnp.ones((FOX_HEADS,))])
    w_kv_shared = nrm((d, KV_SHARED), d ** -0.5) * col_kv
    b_forget = 3.0 + nrm((FOX_HEADS,), 0.5)
    k_norm_g = 1.0 + nrm((FOX_HD,), 0.02)
    col_m = jnp.concatenate([jnp.ones((MEM_W,)), jnp.full((MEM_W,), DN_BETA)])
    w_mem_kv = nrm((DEPTH, d, 2 * MEM_W), d ** -0.5) * col_m
    w_out = nrm((DEPTH, d, d), d ** -0.5 * DN_BETA)
    ln_mix_g = 1.0 + nrm((DEPTH, d), 0.02)
    ln_mix_b = nrm((DEPTH, d), 0.02)
    ln_ffn_g = 1.0 + nrm((DEPTH, d), 0.02)
    ln_ffn_b = nrm((DEPTH, d), 0.02)
    w_router = nrm((d, N_EXPERTS), d ** -0.5)
    b_router = nrm((N_EXPERTS,), 0.01)
    w_exp_gate = nrm((DEPTH, N_EXPERTS, d, D_EXPERT), d ** -0.5)
    w_exp_up = nrm((DEPTH, N_EXPERTS, d, D_EXPERT), d ** -0.5 * DN_BETA)
    w_exp_down = nrm((DEPTH, N_EXPERTS, D_EXPERT, d), D_EXPERT ** -0.5 * DN_BETA)
    return {'x': x, 'mem': mem, 'w_in_a': w_in_a, 'w_gate_up_a': w_gate_up_a, 'b_gate_a': b_gate_a,
            'gla_norm_g': gla_norm_g, 'w_in_b': w_in_b, 'q_norm_g': q_norm_g, 'w_kv_shared': w_kv_shared,
            'b_forget': b_forget, 'k_norm_g': k_norm_g, 'w_mem_kv': w_mem_kv, 'w_out': w_out,
            'ln_mix_g': ln_mix_g, 'ln_mix_b': ln_mix_b, 'ln_ffn_g': ln_ffn_g, 'ln_ffn_b': ln_ffn_b,
            'w_router': w_router, 'b_router': b_router, 'w_exp_gate': w_exp_gate, 'w_exp_up': w_exp_up,
            'w_exp_down': w_exp_down}


def reference(x, mem, w_in_a, w_gate_up_a, b_gate_a, gla_norm_g, w_in_b, q_norm_g, w_kv_shared,
              b_forget, k_norm_g, w_mem_kv, w_out, ln_mix_g, ln_mix_b, ln_ffn_g, ln_ffn_b,
              w_router, b_router, w_exp_gate, w_exp_up, w_exp_down):
    B, S, D = x.shape
    shared_k = shared_v = shared_cum = None
    for layer in range(DEPTH):
        if layer < N_A_LAYERS:
            i = layer
            h = x @ w_in_a[i]
            q, k, v, g_lr, r, mq = jnp.split(h, A_SPLITS, axis=-1)
            log_a = jax.nn.log_sigmoid((g_lr @ w_gate_up_a[i] + b_gate_a[i]).astype(jnp.float32)) / GLA_TAU
            o = gla_chunked(split_heads(q, GLA_HEADS) * (GLA_DK ** -0.5), split_heads(k, GLA_HEADS),
                            split_heads(v, GLA_HEADS), split_heads(log_a, GLA_HEADS))
            o = rms_norm(o, gla_norm_g[i]).astype(x.dtype).reshape(B, S, SELF_W) * jax.nn.silu(r)
        else:
            if shared_k is None:
                kvf = x @ w_kv_shared
                ks_, vs_, f_logit = jnp.split(kvf, KV_SPLITS, axis=-1)
                shared_k = jnp.transpose(rms_norm(split_heads(ks_, FOX_HEADS), k_norm_g).astype(x.dtype), (0, 2, 1, 3))
                shared_v = jnp.transpose(split_heads(vs_, FOX_HEADS), (0, 2, 1, 3))
                log_f = jax.nn.log_sigmoid(f_logit.astype(jnp.float32) + b_forget)
                shared_cum = jnp.transpose(jnp.cumsum(log_f, axis=1), (0, 2, 1))
            j = layer - N_A_LAYERS
            h = x @ w_in_b[j]
            q, og, mq = jnp.split(h, B_SPLITS, axis=-1)
            q = jnp.transpose(rms_norm(split_heads(q, FOX_HEADS), q_norm_g[j]).astype(x.dtype), (0, 2, 1, 3))
            o = fox_attention(q, shared_k, shared_v, shared_cum)
            o = jnp.transpose(o, (0, 2, 1, 3)).reshape(B, S, SELF_W) * jax.nn.sigmoid(og)
        mkv = mem @ w_mem_kv[layer]
        mk, mv = jnp.split(mkv, 2, axis=-1)
        m_out = mem_attention(split_heads(mq, MEM_HEADS), split_heads(mk, MEM_HEADS), split_heads(mv, MEM_HEADS))
        y = jnp.concatenate([o, m_out.reshape(B, S, MEM_W)], axis=-1) @ w_out[layer]
        x = layer_norm(DN_ALPHA * x + y, ln_mix_g[layer], ln_mix_b[layer])
        f = grouped_moe(x, w_router, b_router, w_exp_gate[layer], w_exp_up[layer], w_exp_down[layer])
        x = layer_norm(DN_ALPHA * x + f, ln_ffn_g[layer], ln_ffn_b[layer])
    return x
```

```python
import contextlib
import os
import threading
import numpy as np
import concourse.bass as bass
import concourse.mybir as mybir
from concourse.bass_utils import run_bass_kernel_spmd

F32 = mybir.dt.float32
BF16 = mybir.dt.bfloat16
AF = mybir.ActivationFunctionType
ALU = mybir.AluOpType
AX = mybir.AxisListType

S = 4096
D = 1024
NTILE = S // 128
N_CORES = 8
ALPHA = (2.0 * 2) ** 0.25
LN_EPS = 1e-5
RMS_EPS = 1e-6
A_IN = 2576
B_IN = 1792
KV_IN = 1548
NEXP = 16
ST_TOK = 2048
ST_TILES = ST_TOK // 128


class Coop:
    def __init__(self):
        self.tl = threading.local()

    def switch(self):
        st = getattr(self.tl, "st", None)
        if st is None:
            return
        i, sems, done, n = st
        for k in range(1, n):
            j = (i + k) % n
            if not done[j]:
                sems[j].release()
                sems[i].acquire()
                return

    def run(self, fns):
        n = len(fns)
        if n == 1:
            fns[0]()
            return
        sems = [threading.Semaphore(0) for _ in range(n)]
        done = [False] * n
        main = threading.Semaphore(0)
        errs = []

        def worker(i):
            sems[i].acquire()
            self.tl.st = (i, sems, done, n)
            try:
                fns[i]()
            except BaseException as e:
                errs.append(e)
            done[i] = True
            for k in range(1, n):
                j = (i + k) % n
                if not done[j]:
                    sems[j].release()
                    return
            main.release()

        ths = [threading.Thread(target=worker, args=(i,)) for i in range(n)]
        for t in ths:
            t.start()
        sems[0].release()
        main.acquire()
        for t in ths:
            t.join()
        if errs:
            raise errs[0]


COOP = Coop()


class Res:
    __slots__ = ("name", "w", "r", "excl")

    def __init__(self, name, excl=False):
        self.name = name
        self.w = None
        self.r = []
        self.excl = excl


class Trk:
    def __init__(self, nc):
        self.nc = nc
        self.eng = {"pe": nc.tensor, "act": nc.scalar, "dve": nc.vector, "pool": nc.gpsimd, "sp": nc.sync}
        self.sem = {}
        self.cnt = {}
        self.known = {e: {} for e in self.eng}
        self._stack = []
        for e in self.eng:
            cm = nc.semaphore("s_" + e)
            self.sem[e] = cm.__enter__()
            self._stack.append(cm)
            self.cnt[e] = 0
        self.dsem = {}
        self.dcnt = {}
        self.nwait = 0
        self.ninst = 0

    def close(self):
        for cm in reversed(self._stack):
            cm.__exit__(None, None, None)

    def _dma_sem(self, key):
        if key not in self.dsem:
            cm = self.nc.semaphore("d_" + key)
            self.dsem[key] = cm.__enter__()
            self._stack.append(cm)
            self.dcnt[key] = 0
        return self.dsem[key]

    def _need(self, e, toks):
        best = {}
        for t in toks:
            if t is None:
                continue
            name, sem, val, src = t
            if src == "pe" and e == "pe":
                continue
            if self.known[e].get(name, 0) >= val:
                continue
            if best.get(name, (None, 0))[1] < val:
                best[name] = (sem, val)
        for name, (sem, val) in best.items():
            self.eng[e].wait_ge(sem, val)
            self.known[e][name] = val
            self.nwait += 1

    @staticmethod
    def _compact(lst):
        best = {}
        for t in lst:
            if t[0] not in best or best[t[0]][2] < t[2]:
                best[t[0]] = t
        return list(best.values())

    def op(self, e, fn, reads=(), writes=(), signal=True):
        toks = []
        for r in reads:
            toks.append(r.w)
            if r.excl and e != "pe":
                toks.extend(r.r)
        for w in writes:
            toks.append(w.w)
            toks.extend(w.r)
        self._need(e, toks)
        ins = fn()
        self.ninst += 1
        if signal:
            self.cnt[e] += 1
            ins.then_inc(self.sem[e], 1)
            tok = ("E" + e, self.sem[e], self.cnt[e], e)
        else:
            tok = ("E" + e, self.sem[e], self.cnt[e] + 1, e)
        for r in reads:
            if r in writes:
                continue
            r.r.append(tok)
            if len(r.r) > 16:
                r.r = self._compact(r.r)
        for w in writes:
            w.w = tok
            w.r = []
        COOP.switch()
        return ins

    def dma(self, q, key, out, in_, reads=(), writes=()):
        toks = []
        for r in reads:
            toks.append(r.w)
        for w in writes:
            toks.append(w.w)
            toks.extend(w.r)
        self._need(q, toks)
        sem = self._dma_sem(key)
        self.dcnt[key] += 16
        self.eng[q].dma_start(out=out, in_=in_).then_inc(sem, 16)
        self.ninst += 1
        tok = ("D" + key, sem, self.dcnt[key], "dma")
        for r in reads:
            r.r.append(tok)
            if len(r.r) > 16:
                r.r = self._compact(r.r)
        for w in writes:
            w.w = tok
            w.r = []
        COOP.switch()
        return tok


def _barrier(T):
    for e in T.eng:
        for f in T.eng:
            if f != e and T.cnt[f] > 0 and T.known[e].get("E" + f, 0) < T.cnt[f]:
                T.eng[e].wait_ge(T.sem[f], T.cnt[f])
                T.known[e]["E" + f] = T.cnt[f]
                T.nwait += 1
        for key in list(T.dsem.keys()):
            if T.known[e].get("D" + key, 0) < T.dcnt[key]:
                T.eng[e].wait_ge(T.dsem[key], T.dcnt[key])
                T.known[e]["D" + key] = T.dcnt[key]
                T.nwait += 1


class Bank:
    def __init__(self, f, res):
        self.f = f
        self.b = f.bitcast(BF16)
        self.res = res


def build(dbg=False, stop_after=None):
    nc = bass.Bass("TRN2", target_bir_lowering=False)

    def din(name, shape):
        return nc.dram_tensor(name, list(shape), F32, kind="ExternalInput").ap()

    x = din("x", [S, D])
    mem = din("mem", [256, D])
    w_in_a = din("w_in_a", [D, A_IN])
    w_gate_up_a = din("w_gate_up_a", [16, 384])
    b_gate_a = din("b_gate_a", [1, 384])
    gla_norm_g = din("gla_norm_g", [1, 192])
    w_in_b = din("w_in_b", [D, B_IN])
    q_norm_g = din("q_norm_g", [1, 64])
    w_kv_shared = din("w_kv_shared", [D, KV_IN])
    b_forget = din("b_forget", [1, 12])
    k_norm_g = din("k_norm_g", [1, 64])
    w_mem_kv = din("w_mem_kv", [2, D, 512])
    w_out = din("w_out", [2, D, D])
    ln_mix_g = din("ln_mix_g", [2, D])
    ln_mix_b = din("ln_mix_b", [2, D])
    ln_ffn_g = din("ln_ffn_g", [2, D])
    ln_ffn_b = din("ln_ffn_b", [2, D])
    w_router = din("w_router", [D, 16])
    b_router = din("b_router", [1, 16])
    w_exp_gate = din("w_exp_gate", [2, NEXP, D, 512])
    w_exp_up = din("w_exp_up", [2, NEXP, D, 512])
    w_exp_down = din("w_exp_down", [2, NEXP, 512, D])
    c_ident = din("c_ident", [128, 128])
    c_triu = din("c_triu", [128, 128])

    out = nc.dram_tensor("out", [S, D], F32, kind="ExternalOutput").ap()
    kscr = "ExternalOutput" if dbg else "Internal"
    XA = nc.dram_tensor("XA", [S, D], F32, kind=kscr).ap()
    XB = nc.dram_tensor("XB", [S, D], F32, kind=kscr).ap()
    XT = nc.dram_tensor("XT", [128, 8, S], BF16, kind="Internal").ap()
    QT = nc.dram_tensor("QT", [12, 66, S], BF16, kind="Internal").ap()
    KT = nc.dram_tensor("KT", [12, 66, S], BF16, kind="Internal").ap()
    OGQ = nc.dram_tensor("OGQ", [S, D], BF16, kind="Internal").ap()
    if dbg:
        DG = nc.dram_tensor("DG", [128, 2, NTILE, 16], F32, kind="ExternalOutput").ap()
        DCAT = nc.dram_tensor("DCAT", [2, S, D], BF16, kind="ExternalOutput").ap()

    T = Trk(nc)
    out_toks = []
    R_XA = [Res('XA%d' % i) for i in range(NTILE)]
    R_XB = [Res('XB%d' % i) for i in range(NTILE)]
    R_XTd = [Res('XTd%d' % i) for i in range(NTILE)]
    R_OGQ = [Res('OGQ%d' % i) for i in range(NTILE)]
    R_QTd = [Res('QTd%d' % i) for i in range(NTILE)]
    R_KTd = [Res('KTd%d' % i) for i in range(NTILE)]

    with contextlib.ExitStack() as ges:
        uid = [0]

        def sbt(es, name, shape, dt):
            uid[0] += 1
            return es.enter_context(nc.sbuf_tensor("%s_%d" % (name, uid[0]), list(shape), dt))

        banks = []
        for i in range(8):
            pt = ges.enter_context(nc.psum_tensor("pb%d" % i, [128, 512], F32))
            banks.append(Bank(pt[:], Res("pb%d" % i, excl=True)))
        rots = {}
        rtl = threading.local()

        def use_rot(name, lst):
            if name not in rots:
                rots[name] = {"lst": list(lst), "i": 0}
            rtl.cur = rots[name]

        def set_rot(lst):
            use_rot("main%s" % (tuple(lst),), lst)

        def nb():
            rot = rtl.cur
            b = banks[rot["lst"][rot["i"] % len(rot["lst"])]]
            rot["i"] += 1
            return b

        def op(e, fn, reads=(), writes=(), signal=True):
            return T.op(e, fn, reads, writes, signal)

        def mm(o, lhsT, rhs, start, stop, reads, writes, signal=True):
            return T.op("pe", lambda: nc.tensor.matmul(o, lhsT, rhs, start=start, stop=stop), reads, writes, signal)

        def tr(o, in_, ident, reads, writes, signal=True):
            return T.op("pe", lambda: nc.tensor.transpose(o, in_, ident), reads, writes, signal)

        id_f = sbt(ges, "id_f", [128, 128], F32)
        id_b = sbt(ges, "id_b", [128, 128], BF16)
        triu = sbt(ges, "triu", [128, 128], F32)
        triu_b = sbt(ges, "triu_b", [128, 128], BF16)
        ones_f = sbt(ges, "ones_f", [128, 128], F32)
        cst = sbt(ges, "cst", [128, 4], F32)
        gates_all = sbt(ges, "gates_all", [128, NTILE, 16], F32)
        mkT = sbt(ges, "mkT", [128, 2, 2, 256], BF16)
        mv_aug = sbt(ges, "mv_aug", [128, 2, 2, 4, 72], BF16)
        wr_f = sbt(ges, "wr_f", [128, 8, 16], F32)
        br_b = sbt(ges, "br_b", [128, 16], F32)
        R_const = Res("const")
        R_gates = [Res("gates%d" % i) for i in range(NTILE)]
        R_mk = Res("mk")
        T.dma("sp", "c0", id_f[:], c_ident, writes=[R_const])
        T.dma("sp", "c1", triu[:], c_triu, writes=[R_const])
        T.dma("pool", "c2", id_b[:], c_ident, writes=[R_const])
        T.dma("pool", "c3", triu_b[:], c_triu, writes=[R_const])
        T.dma("sp", "c4", wr_f[:], w_router.rearrange("(kc p) e -> p kc e", p=128), writes=[R_const])
        T.dma("sp", "c5", br_b[:], b_router.broadcast_to([128, 16]), writes=[R_const])
        op("dve", lambda: nc.vector.memset(ones_f[:], 1.0), writes=[R_const])
        op("dve", lambda: nc.vector.memset(cst[:, 0:1], LN_EPS), writes=[R_const])
        op("dve", lambda: nc.vector.memset(cst[:, 1:2], RMS_EPS), writes=[R_const])
        op("dve", lambda: nc.vector.memset(cst[:, 2:3], 1.0), writes=[R_const])
        op("dve", lambda: nc.vector.memset(mv_aug[:], 1.0), writes=[R_mk])
        EPS_LN = cst[:, 0:1]
        EPS_RMS = cst[:, 1:2]
        ONE = cst[:, 2:3]

        with contextlib.ExitStack() as es:
            set_rot(range(8))
            mem_f = sbt(es, "mem_f", [128, 2, D], F32)
            memT = sbt(es, "memT", [128, 8, 256], BF16)
            wm = sbt(es, "wm", [128, 8, 512], BF16)
            R_memf, R_memT, R_wm = Res("memf"), Res("memT"), Res("wm")
            T.dma("sp", "p0a", mem_f[:], mem.rearrange("(mc p) d -> p mc d", p=128), writes=[R_memf])
            for mc in range(2):
                for half in range(2):
                    bk = nb()
                    for q in range(4):
                        kc = half * 4 + q
                        tr(bk.f[:, q * 128:(q + 1) * 128], mem_f[:, mc, kc * 128:(kc + 1) * 128], id_f[:],
                           [R_memf, R_const], [bk.res], signal=(q == 3))
                    op("act", lambda: nc.scalar.copy(memT[:, half * 4:(half + 1) * 4, mc * 128:(mc + 1) * 128],
                                                     bk.f[:].rearrange("p (q t) -> p q t", q=4)),
                       [bk.res], [R_memT])
            for l in range(2):
                T.dma("pool", "p0b", wm[:], w_mem_kv[l].rearrange("(kc p) n -> p kc n", p=128), writes=[R_wm])
                for j in range(2):
                    bk = nb()
                    for kc in range(8):
                        mm(bk.f[:, 0:256], wm[:, kc, j * 128:(j + 1) * 128], memT[:, kc, :], kc == 0, kc == 7,
                           [R_wm, R_memT], [bk.res], signal=(kc == 7))
                    op("act", lambda: nc.scalar.copy(mkT[:, l, j, :], bk.f[:, 0:256]), [bk.res], [R_mk])
                for mc in range(2):
                    bk = nb()
                    for kc in range(8):
                        mm(bk.f[:, 0:256], memT[:, kc, mc * 128:(mc + 1) * 128], wm[:, kc, 256:512], kc == 0, kc == 7,
                           [R_wm, R_memT], [bk.res], signal=(kc == 7))
                    op("dve", lambda: nc.vector.tensor_copy(mv_aug[:, l, mc, :, 0:64],
                                                            bk.f[:, 0:256].rearrange("p (h e) -> p h e", h=4)),
                       [bk.res], [R_mk])

        _barrier(T)
        def layer_norm(es_tiles, zt, R_zt, g_b, b_b, R_gb, R_ln, outap, R_out):
            st, mv, rs = es_tiles
            op("dve", lambda: nc.vector.bn_stats(st[:, 0:6], zt[:, 0:512]), [R_zt], [R_ln])
            op("dve", lambda: nc.vector.bn_stats(st[:, 6:12], zt[:, 512:1024]), [R_zt], [R_ln])
            op("dve", lambda: nc.vector.bn_aggr(mv[:, 0:2], st[:, 0:12]), [R_ln], [R_ln])
            op("act", lambda: nc.scalar.activation(rs[:, 0:1], mv[:, 1:2], AF.Sqrt, bias=EPS_LN, scale=1.0), [R_ln, R_const], [R_ln])
            op("dve", lambda: nc.vector.reciprocal(rs[:, 1:2], rs[:, 0:1]), [R_ln], [R_ln])
            op("dve", lambda: nc.vector.scalar_tensor_tensor(out=zt, in0=zt, scalar=mv[:, 0:1], in1=g_b, op0=ALU.subtract, op1=ALU.mult), [R_zt, R_ln, R_gb], [R_zt])
            op("dve", lambda: nc.vector.scalar_tensor_tensor(out=outap, in0=zt, scalar=rs[:, 1:2], in1=b_b, op0=ALU.mult, op1=ALU.add), [R_zt, R_ln, R_gb], [R_out])

        def mem_attn(l, tl, mq_b, R_mq, cat_mem, R_cat):
            mqT, pexp, rd, R_t = tl
            bk = nb()
            for j in range(2):
                tr(bk.b[:, j * 128:(j + 1) * 128], mq_b[:, j * 128:(j + 1) * 128], id_b[:], [R_mq, R_const], [bk.res], signal=(j == 1))
            op("act", lambda: nc.scalar.copy(mqT[:], bk.b[:, 0:256].rearrange("p (j t) -> p j t", j=2)), [bk.res], [R_t])
            MA = int(os.environ.get("MA", 99))
            if MA < 2:
                return
            bkh = [nb(), nb()]
            for hp in range(2):
                for hh in range(2):
                    pb = hh * 64
                    for mc in range(2):
                        mm(bkh[hh].f[:, (hp * 2 + mc) * 128:(hp * 2 + mc + 1) * 128], mkT[pb:pb + 64, l, hp, mc * 128:(mc + 1) * 128],
                           mqT[pb:pb + 64, hp, :], True, True, [R_mk, R_t], [bkh[hh].res], signal=(hp == 1 and mc == 1))
            for hh in range(2):
                op("act", lambda: nc.scalar.activation(pexp[:, hh * 4:(hh + 1) * 4, :], bkh[hh].f[:].rearrange("p (a t) -> p a t", a=4),
                                                       AF.Exp, scale=0.125), [bkh[hh].res], [R_t])
            if MA < 3:
                return
            bk = nb()
            for h in range(4):
                for mc in range(2):
                    mm(bk.f[:, h * 128:h * 128 + 65], pexp[:, (h % 2) * 4 + (h // 2) * 2 + mc, :], mv_aug[:, l, mc, h, 0:65], mc == 0, mc == 1,
                       [R_t, R_mk], [bk.res], signal=(h == 3 and mc == 1))
            pv = bk.f[:].rearrange("p (h e) -> p h e", e=128)
            if MA < 4:
                return
            op("dve", lambda: nc.vector.reciprocal(rd[:, 0:4], pv[:, :, 64]), [bk.res], [R_t])
            op("dve", lambda: nc.vector.tensor_tensor(out=cat_mem.rearrange("p (h e) -> p h e", h=4), in0=pv[:, :, 0:64],
                                                      in1=rd[:, 0:4].unsqueeze(2).broadcast_to([128, 4, 64]), op=ALU.mult),
               [bk.res, R_t], [R_cat])

        def out_ln_router(l, i, tl, cat, R_cat, xt, R_xt, wout_b, g_b, b_b, R_w, R_gb, part="ab"):
            catT, R_catT, lnt, R_lnt, x1T_f, x1T_b, R_x1T, rt, R_rt = tl
            if "a" in part:
                out_ln_a(l, i, tl, cat, R_cat, xt, R_xt, wout_b, g_b, b_b, R_w, R_gb)
            if "b" in part:
                out_ln_b(l, i, tl, xt, R_xt)

        def out_ln_a(l, i, tl, cat, R_cat, xt, R_xt, wout_b, g_b, b_b, R_w, R_gb):
            catT, R_catT, lnt, R_lnt, x1T_f, x1T_b, R_x1T, rt, R_rt = tl
            rc = list(R_cat) if isinstance(R_cat, (list, tuple)) else [R_cat]
            if dbg:
                T.dma("sp", "dcat", DCAT[l, i * 128:(i + 1) * 128, :], cat[:], reads=rc)
            bk = nb()
            for kc in range(8):
                tr(bk.b[:, kc * 128:(kc + 1) * 128], cat[:, kc * 128:(kc + 1) * 128], id_b[:], rc + [R_const], [bk.res], signal=(kc == 7))
            op("act", lambda: nc.scalar.copy(catT[:], bk.b[:].rearrange("p (k t) -> p k t", k=8)), [bk.res], [R_catT])
            for half in range(2):
                bk = nb()
                for kc in range(8):
                    mm(bk.f[:], catT[:, kc, :], wout_b[:, kc, half * 512:(half + 1) * 512], kc == 0, kc == 7,
                       [R_catT, R_w], [bk.res], signal=(kc == 7))
                op("dve", lambda: nc.vector.scalar_tensor_tensor(out=xt[:, half * 512:(half + 1) * 512], in0=xt[:, half * 512:(half + 1) * 512],
                                                                 scalar=ALPHA, in1=bk.f[:], op0=ALU.mult, op1=ALU.add),
                   [R_xt, bk.res], [R_xt])
            layer_norm(lnt, xt[:], R_xt, g_b[:], b_b[:], R_gb, R_lnt, xt[:], R_xt)
            T.dma("sp", "sXA%d" % (i % 2), XA[i * 128:(i + 1) * 128, :], xt[:], reads=[R_xt], writes=[R_XA[i]])

        def out_ln_b(l, i, tl, xt, R_xt):
            catT, R_catT, lnt, R_lnt, x1T_f, x1T_b, R_x1T, rt, R_rt = tl
            for half in range(2):
                bk = nb()
                for q in range(4):
                    kc = half * 4 + q
                    tr(bk.f[:, q * 128:(q + 1) * 128], xt[:, kc * 128:(kc + 1) * 128], id_f[:], [R_xt, R_const], [bk.res], signal=(q == 3))
                op("act", lambda: nc.scalar.copy(x1T_f[:, half * 4:(half + 1) * 4, :], bk.f[:].rearrange("p (q t) -> p q t", q=4)), [bk.res], [R_x1T])
                op("dve", lambda: nc.vector.tensor_copy(x1T_b[:, half * 4:(half + 1) * 4, :], bk.f[:].rearrange("p (q t) -> p q t", q=4)), [bk.res], [R_x1T])
            T.dma("sp", "sXT%d" % (i % 2), XT[:, :, i * 128:(i + 1) * 128], x1T_b[:], reads=[R_x1T], writes=[R_XTd[i]])
            bk = nb()
            for kc in range(8):
                mm(bk.f[:, 0:16], x1T_f[:, kc, :], wr_f[:, kc, :], kc == 0, kc == 7, [R_x1T, R_const], [bk.res], signal=(kc == 7))
            aff, sel, m1, eq, m2, sc, gm = rt
            v3 = lambda a: a.rearrange("p (g e) -> p g e", g=4)
            bc3 = lambda a: a.unsqueeze(2).broadcast_to([128, 4, 4])
            op("act", lambda: nc.scalar.activation(aff[:], bk.f[:, 0:16], AF.Sigmoid), [bk.res], [R_rt])
            op("dve", lambda: nc.vector.tensor_tensor(out=sel[:], in0=aff[:], in1=br_b[:], op=ALU.add), [R_rt, R_const], [R_rt])
            op("dve", lambda: nc.vector.tensor_reduce(out=m1[:], in_=v3(sel[:]), axis=AX.X, op=ALU.max), [R_rt], [R_rt])
            op("dve", lambda: nc.vector.tensor_tensor(out=v3(eq[:]), in0=v3(sel[:]), in1=bc3(m1[:]), op=ALU.is_equal), [R_rt], [R_rt])
            op("dve", lambda: nc.vector.scalar_tensor_tensor(out=eq[:], in0=eq[:], scalar=-1e9, in1=sel[:], op0=ALU.mult, op1=ALU.add), [R_rt], [R_rt])
            op("dve", lambda: nc.vector.tensor_reduce(out=m2[:], in_=v3(eq[:]), axis=AX.X, op=ALU.max), [R_rt], [R_rt])
            op("dve", lambda: nc.vector.tensor_tensor(out=sc[:], in0=m1[:], in1=m2[:], op=ALU.add), [R_rt], [R_rt])
            op("dve", lambda: nc.vector.tensor_reduce(out=gm[:, 0:1], in_=sc[:], axis=AX.X, op=ALU.max), [R_rt], [R_rt])
            op("dve", lambda: nc.vector.tensor_scalar(sc[:], sc[:], gm[:, 0:1], None, op0=ALU.is_ge), [R_rt], [R_rt])
            op("dve", lambda: nc.vector.tensor_tensor(out=v3(eq[:]), in0=v3(sel[:]), in1=bc3(m2[:]), op=ALU.is_ge), [R_rt], [R_rt])
            op("dve", lambda: nc.vector.tensor_tensor(out=v3(eq[:]), in0=v3(eq[:]), in1=bc3(sc[:]), op=ALU.mult), [R_rt], [R_rt])
            op("dve", lambda: nc.vector.tensor_tensor(out=eq[:], in0=eq[:], in1=aff[:], op=ALU.mult), [R_rt], [R_rt])
            op("dve", lambda: nc.vector.tensor_reduce(out=gm[:, 1:2], in_=eq[:], axis=AX.X, op=ALU.add), [R_rt], [R_rt])
            op("dve", lambda: nc.vector.reciprocal(gm[:, 2:3], gm[:, 1:2]), [R_rt], [R_rt])
            op("dve", lambda: nc.vector.tensor_scalar(gates_all[:, i, :], eq[:], gm[:, 2:3], None, op0=ALU.mult), [R_rt], [R_gates[i]])

        def alloc_out_tiles(es):
            catT = sbt(es, "catT", [128, 8, 128], BF16)
            st = sbt(es, "ln_st", [128, 12], F32)
            mv = sbt(es, "ln_mv", [128, 2], F32)
            rs = sbt(es, "ln_rs", [128, 2], F32)
            x1T_f = sbt(es, "x1T_f", [128, 8, 128], F32)
            x1T_b = sbt(es, "x1T_b", [128, 8, 128], BF16)
            rt = (sbt(es, "r_aff", [128, 16], F32), sbt(es, "r_sel", [128, 16], F32), sbt(es, "r_m1", [128, 4], F32),
                  sbt(es, "r_eq", [128, 16], F32), sbt(es, "r_m2", [128, 4], F32), sbt(es, "r_sc", [128, 4], F32),
                  sbt(es, "r_gm", [128, 4], F32))
            return (catT, Res("catT"), (st, mv, rs), Res("lnt"), x1T_f, x1T_b, Res("x1T"), rt, Res("rt"))

        def alloc_mem_tiles(es):
            return (sbt(es, "mqT", [128, 2, 128], BF16), sbt(es, "pexp_m", [128, 8, 128], BF16), sbt(es, "rd_m", [128, 4], F32), Res("memt"))

        def load_ln(es, l, gsrc, bsrc, key):
            g_b = sbt(es, "g_b" + key, [128, D], F32)
            b_b = sbt(es, "b_b" + key, [128, D], F32)
            R = Res("ln" + key)
            T.dma("sp", "lng" + key, g_b[:], gsrc[l:l + 1, :].broadcast_to([128, D]), writes=[R])
            T.dma("sp", "lnb" + key, b_b[:], bsrc[l:l + 1, :].broadcast_to([128, D]), writes=[R])
            return g_b, b_b, R

        def phase_A():
            with contextlib.ExitStack() as es:
                use_rot("A0", range(8))
                wina = sbt(es, "wina", [128, 8, A_IN], BF16)
                wout_b = sbt(es, "wout_b", [128, 8, D], BF16)
                wup17 = sbt(es, "wup17", [17, 384], F32)
                gn_b = sbt(es, "gn_b", [128, 192], F32)
                R_w = Res("wA")
                R_wina = [Res("wina%d" % kc) for kc in range(8)]
                R_wout = Res("woutA")
                wv = w_in_a.rearrange("(kc p) n -> p kc n", p=128)
                for kc in range(8):
                    T.dma("pool", "wA_%d" % kc, wina[:, kc, :], wv[:, kc, :], writes=[R_wina[kc]])
                T.dma("pool", "wA2", wout_b[:], w_out[0].rearrange("(kc p) n -> p kc n", p=128), writes=[R_wout])
                T.dma("sp", "wA3", wup17[0:16, :], w_gate_up_a, writes=[R_w])
                T.dma("sp", "wA4", wup17[16:17, :], b_gate_a, writes=[R_w])
                T.dma("sp", "wA5", gn_b[:], gla_norm_g.broadcast_to([128, 192]), writes=[R_w])
                g_b, b_b, R_lnp = load_ln(es, 0, ln_mix_g, ln_mix_b, "A")
                xt = [sbt(es, "xt%d" % p, [128, D], F32) for p in range(5)]
                xT = [sbt(es, "xT%d" % p, [128, 8, 128], BF16) for p in range(2)]
                xb = [sbt(es, "xb%d" % p, [128, D], BF16) for p in range(2)]
                R_xb = [Res("xb%d" % p) for p in range(2)]
                hs = [sbt(es, "hs%d" % p, [128, A_IN], F32) for p in range(2)]
                R_xt = [Res("xt%d" % p) for p in range(5)]
                R_xT = [Res("xT%d" % p) for p in range(2)]
                R_hs = [[Res("hs%d_%d" % (p, g)) for g in range(6)] for p in range(2)]
                gT17 = [sbt(es, "gT17_%d" % p, [17, 128], F32) for p in range(2)]
                R_gT = [Res("gT%d" % p) for p in range(2)]
                for p in range(2):
                    op("dve", lambda: nc.vector.memset(gT17[p][:], 1.0), [], [R_gT[p]])
                l_sb = sbt(es, "l_sb", [128, 384], F32)
                eb = sbt(es, "eb", [128, 384], F32)
                enb = sbt(es, "enb", [128, 384], F32)
                qd = sbt(es, "qd", [128, 384], BF16)
                ki2 = [sbt(es, "ki%d" % p, [128, 384], BF16) for p in range(2)]
                v_b2 = [sbt(es, "v_b%d" % p, [128, 768], BF16) for p in range(2)]
                dec2 = [sbt(es, "dec%d" % p, [96, 4], F32) for p in range(2)]
                qdT2 = [sbt(es, "qdT%d" % p, [96, 4, 128], BF16) for p in range(2)]
                kiT = sbt(es, "kiT", [96, 4, 128], BF16)
                attm2 = [sbt(es, "attm%d" % p, [128, 4, 128], BF16) for p in range(2)]
                sr2 = [sbt(es, "sr2_%d" % p, [128, 768], F32) for p in range(2)]
                R_ki2 = [Res("ki%d" % p) for p in range(2)]
                R_vb2 = [Res("vb%d" % p) for p in range(2)]
                R_dec2 = [Res("dec%d" % p) for p in range(2)]
                R_qdT2 = [Res("qdT%d" % p) for p in range(2)]
                R_attm2 = [Res("attm%d" % p) for p in range(2)]
                R_sr2 = [Res("sr%d" % p) for p in range(2)]
                Sst = sbt(es, "Sst", [96, 4, 192], F32)
                S_b = sbt(es, "S_b", [96, 4, 192], BF16)
                kvd = sbt(es, "kvd", [96, 4, 192], F32)
                sq = sbt(es, "sq", [128, 768], F32)
                ss = sbt(es, "ss", [128, 8], F32)
                on = sbt(es, "on", [128, 768], F32)
                cats = [sbt(es, "cat%d" % p, [128, D], BF16) for p in range(4)]
                mq_b = sbt(es, "mq_b", [128, 256], BF16)
                R_l, R_eb, R_qd, R_ki, R_vb, R_dec, R_qdT, R_kiT, R_attm = (Res(n) for n in ["l", "eb", "qd", "ki", "vb", "dec", "qdT", "kiT", "attm"])
                R_S = [Res("S%d" % h) for h in range(4)]
                R_Sb = [Res("Sb%d" % h) for h in range(4)]
                R_kvd = [Res("kvd%d" % h) for h in range(4)]
                R_sq, R_ss, R_on, R_sr, R_mqb = (Res(n) for n in ["sq", "ss", "on", "sr", "mqb"])
                R_cats = [Res("cat%d" % p) for p in range(4)]
                R_catm = [Res("catm%d" % p) for p in range(4)]
                op("dve", lambda: nc.vector.memset(Sst[:], 0.0), [], R_S)
                op("pool", lambda: nc.gpsimd.memset(S_b[:], 0.0), [], R_Sb)
                memt = alloc_mem_tiles(es)
                outt = alloc_out_tiles(es)
                GRP = [(0, 512), (512, 1024), (1024, 1536), (1536, 2048), (2048, 2560), (2560, 2576)]

                def grp_of(lo, hi):
                    return [g for g, (a, b) in enumerate(GRP) if a < hi and b > lo]

                def S1(i):
                    use_rot("A1", [0])
                    p = i % 2
                    p3 = i % 5
                    T.dma("sp", "ldx%d" % p3, xt[p3][:], x[i * 128:(i + 1) * 128, :], writes=[R_xt[p3]])
                    op("pool", lambda: nc.gpsimd.tensor_copy(xb[p][:], xt[p3][:]), [R_xt[p3]], [R_xb[p]])
                    bk = nb()
                    for kc in range(8):
                        tr(bk.b[:, kc * 128:(kc + 1) * 128], xb[p][:, kc * 128:(kc + 1) * 128], id_b[:], [R_xb[p], R_const], [bk.res], signal=(kc == 7))
                    op("act", lambda: nc.scalar.copy(xT[p][:], bk.b[:].rearrange("p (k t) -> p k t", k=8)), [bk.res], [R_xT[p]])
                    for g, (a, b) in enumerate(GRP):
                        bk = nb()
                        for kc in range(8):
                            mm(bk.f[:, 0:b - a], xT[p][:, kc, :], wina[:, kc, a:b], kc == 0, kc == 7, [R_xT[p], R_wina[kc]], [bk.res], signal=(kc == 7))
                        if g % 2 == 0:
                            op("dve", lambda: nc.vector.tensor_copy(hs[p][:, a:b], bk.f[:, 0:b - a]), [bk.res], [R_hs[p][g]])
                        else:
                            op("act", lambda: nc.scalar.copy(hs[p][:, a:b], bk.f[:, 0:b - a]), [bk.res], [R_hs[p][g]])

                def S2a(i):
                    use_rot("A2a", [1])
                    p = i % 2
                    h = hs[p]
                    Rh = lambda lo, hi: [R_hs[p][g] for g in grp_of(lo, hi)]
                    bk = nb()
                    tr(bk.f[0:16, 0:128], h[:, 1536:1552], id_f[:], Rh(1536, 1552) + [R_const], [bk.res])
                    op("act", lambda: nc.scalar.copy(gT17[p][0:16, :], bk.f[0:16, 0:128]), [bk.res], [R_gT[p]])
                    bk = nb()
                    mm(bk.f[:, 0:384], gT17[p][0:17, :], wup17[0:17, :], True, True, [R_gT[p], R_w], [bk.res])
                    op("act", lambda: nc.scalar.activation(l_sb[:], bk.f[:, 0:384], AF.Exp, scale=-1.0), [bk.res], [R_l])
                    op("act", lambda: nc.scalar.activation(l_sb[:], l_sb[:], AF.Ln, bias=ONE, scale=1.0), [R_l, R_const], [R_l])
                    bk = nb()
                    mm(bk.f[:, 0:384], triu[:], l_sb[:], True, True, [R_const, R_l], [bk.res])
                    op("act", lambda: nc.scalar.activation(eb[:], bk.f[:, 0:384], AF.Exp, scale=-1.0 / 16.0), [bk.res], [R_eb])
                    op("act", lambda: nc.scalar.activation(enb[:], bk.f[:, 0:384], AF.Exp, scale=1.0 / 16.0), [bk.res], [R_eb])
                    op("dve", lambda: nc.vector.scalar_tensor_tensor(out=qd[:], in0=h[:, 0:384], scalar=96.0 ** -0.5, in1=eb[:], op0=ALU.mult, op1=ALU.mult),
                       Rh(0, 384) + [R_eb], [R_qd])
                    op("dve", lambda: nc.vector.tensor_tensor(out=ki2[p][:], in0=h[:, 384:768], in1=enb[:], op=ALU.mult), Rh(384, 768) + [R_eb], [R_ki2[p]])
                    op("pool", lambda: nc.gpsimd.tensor_copy(v_b2[p][:], h[:, 768:1536]), Rh(768, 1536), [R_vb2[p]])
                    bk = nb()
                    for hd in range(4):
                        mm(bk.f[0:96, hd:hd + 1], l_sb[:, hd * 96:(hd + 1) * 96], ones_f[:, 0:1], True, True, [R_l, R_const], [bk.res], signal=(hd == 3))
                    op("act", lambda: nc.scalar.activation(dec2[p][:], bk.f[0:96, 0:4], AF.Exp, scale=-1.0 / 16.0), [bk.res], [R_dec2[p]])
                    bk = nb()
                    for hd in range(4):
                        tr(bk.b[0:96, hd * 128:(hd + 1) * 128], qd[:, hd * 96:(hd + 1) * 96], id_b[:], [R_qd, R_const], [bk.res], signal=(hd == 3))
                    op("act", lambda: nc.scalar.copy(qdT2[p][:], bk.b[0:96, 0:512].rearrange("p (h t) -> p h t", h=4)), [bk.res], [R_qdT2[p]])
                    bk = nb()
                    for hd in range(4):
                        tr(bk.b[0:96, hd * 128:(hd + 1) * 128], ki2[p][:, hd * 96:(hd + 1) * 96], id_b[:], [R_ki2[p], R_const], [bk.res], signal=(hd == 3))
                    op("dve", lambda: nc.vector.tensor_copy(kiT[:], bk.b[0:96, 0:512].rearrange("p (h t) -> p h t", h=4)), [bk.res], [R_kiT])
                    bk = nb()
                    for hd in range(4):
                        mm(bk.f[:, hd * 128:(hd + 1) * 128], kiT[:, hd, :], qdT2[p][:, hd, :], True, True, [R_kiT, R_qdT2[p]], [bk.res], signal=(hd == 3))
                    op("dve", lambda: nc.vector.tensor_tensor(out=attm2[p][:], in0=bk.f[:].rearrange("p (h t) -> p h t", h=4),
                                                              in1=triu[:].unsqueeze(1).broadcast_to([128, 4, 128]), op=ALU.mult),
                       [bk.res, R_const], [R_attm2[p]])
                    op("act", lambda: nc.scalar.activation(sr2[p][:], h[:, 1552:2320], AF.Silu), Rh(1552, 2320), [R_sr2[p]])

                def S2b(i):
                    use_rot("A2b", [2, 3, 4])
                    p = i % 2
                    cat, R_cat = cats[i % 4], R_cats[i % 4]
                    ki, v_b, dec, qdT, attm, sr = ki2[p], v_b2[p], dec2[p], qdT2[p], attm2[p], sr2[p]
                    R_ki, R_vb, R_dec, R_qdT, R_attm, R_sr = R_ki2[p], R_vb2[p], R_dec2[p], R_qdT2[p], R_attm2[p], R_sr2[p]
                    ob = [nb(), nb()]
                    for hd in range(4):
                        bo = ob[hd // 2]
                        oo = bo.f[:, (hd % 2) * 192:(hd % 2 + 1) * 192]
                        mm(oo, attm[:, hd, :], v_b[:, hd * 192:(hd + 1) * 192], True, False, [R_attm, R_vb], [bo.res], signal=False)
                        mm(oo, qdT[:, hd, :], S_b[:, hd, :], False, True, [R_qdT, R_Sb[hd]], [bo.res], signal=True)
                    for j in range(2):
                        op("act", lambda: nc.scalar.activation(sq[:, j * 384:(j + 1) * 384], ob[j].f[:, 0:384], AF.Square), [ob[j].res], [R_sq])
                    op("dve", lambda: nc.vector.tensor_reduce(out=ss[:, 0:4], in_=sq[:].rearrange("p (h e) -> p h e", h=4), axis=AX.X, op=ALU.add), [R_sq], [R_ss])
                    op("act", lambda: nc.scalar.activation(ss[:, 0:4], ss[:, 0:4], AF.Sqrt, bias=EPS_RMS, scale=1.0 / 192.0), [R_ss, R_const], [R_ss])
                    op("dve", lambda: nc.vector.reciprocal(ss[:, 4:8], ss[:, 0:4]), [R_ss], [R_ss])
                    for hd in range(4):
                        op("dve", lambda: nc.vector.scalar_tensor_tensor(out=on[:, hd * 192:(hd + 1) * 192], in0=ob[hd // 2].f[:, (hd % 2) * 192:(hd % 2 + 1) * 192],
                                                                         scalar=ss[:, 4 + hd:5 + hd], in1=gn_b[:], op0=ALU.mult, op1=ALU.mult),
                           [ob[hd // 2].res, R_ss, R_w], [R_on])
                    op("dve", lambda: nc.vector.tensor_tensor(out=cat[:, 0:768], in0=on[:], in1=sr[:], op=ALU.mult), [R_on, R_sr], [R_cat])
                    kb = [nb(), nb()]
                    for hd in range(4):
                        bkv = kb[hd // 2]
                        kk = bkv.f[0:96, (hd % 2) * 192:(hd % 2 + 1) * 192]
                        mm(kk, ki[:, hd * 96:(hd + 1) * 96], v_b[:, hd * 192:(hd + 1) * 192], True, True, [R_ki, R_vb], [bkv.res])
                        op("dve", lambda: nc.vector.tensor_scalar(kvd[:, hd, :], kk, dec[:, hd:hd + 1], None, op0=ALU.mult), [bkv.res, R_dec], [R_kvd[hd]])
                        op("dve", lambda: nc.vector.scalar_tensor_tensor(out=Sst[:, hd, :], in0=Sst[:, hd, :], scalar=dec[:, hd:hd + 1], in1=kvd[:, hd, :],
                                                                         op0=ALU.mult, op1=ALU.add), [R_S[hd], R_dec, R_kvd[hd]], [R_S[hd]])
                        op("pool", lambda: nc.gpsimd.tensor_copy(S_b[:, hd, :], Sst[:, hd, :]), [R_S[hd]], [R_Sb[hd]])

                def S2m(i):
                    p = i % 2
                    op("pool", lambda: nc.gpsimd.tensor_copy(mq_b[:], hs[p][:, 2320:2576]), [R_hs[p][g] for g in grp_of(2320, 2576)], [R_mqb])
                    mem_attn(0, memt, mq_b, R_mqb, cats[i % 4][:, 768:1024], R_catm[i % 4])

                def S3a(i):
                    use_rot("A3a", [5])
                    out_ln_router(0, i, outt, cats[i % 4], [R_cats[i % 4], R_catm[i % 4]], xt[i % 5], R_xt[i % 5], wout_b, g_b, b_b, R_wout, R_lnp, part="a")

                def SMB(step):
                    use_rot("A3b", [6, 7])
                    if 0 <= step - 1 < NTILE:
                        S2m(step - 1)
                    if 0 <= step - 4 < NTILE:
                        out_ln_router(0, step - 4, outt, None, None, xt[(step - 4) % 5], R_xt[(step - 4) % 5], wout_b, g_b, b_b, R_wout, R_lnp, part="b")

                for step in range(NTILE + 4):
                    chains = []
                    if step < NTILE:
                        chains.append(lambda: S1(step))
                    if 0 <= step - 1 < NTILE:
                        chains.append(lambda: S2a(step - 1))
                    if 0 <= step - 2 < NTILE:
                        chains.append(lambda: S2b(step - 2))
                    if 0 <= step - 3 < NTILE:
                        chains.append(lambda: S3a(step - 3))
                    if 0 <= step - 1 < NTILE or 0 <= step - 4 < NTILE:
                        chains.append(lambda: SMB(step))
                    COOP.run(chains)

        def phase_M(l, src, R_src, dst, R_dst, gsrc, bsrc):
            with contextlib.ExitStack() as es:
                set_rot(range(8))
                xTs = sbt(es, "xTs", [128, 8, ST_TOK], BF16)
                acc = sbt(es, "acc", [128, ST_TILES, D], F32)
                NTB = ST_TOK // 512
                R_xTs = [Res("xTs%d" % tb) for tb in range(NTB)]
                R_acc = [Res("acc%d" % t) for t in range(ST_TILES)]
                wg = [sbt(es, "wg%d" % p, [128, 8, 512], BF16) for p in range(2)]
                wu = [sbt(es, "wu%d" % p, [128, 8, 512], BF16) for p in range(2)]
                wd = [sbt(es, "wd%d" % p, [128, 4, D], BF16) for p in range(2)]
                R_wg = [Res("wg%d" % p) for p in range(2)]
                R_wu = [Res("wu%d" % p) for p in range(2)]
                R_wd = [Res("wd%d" % p) for p in range(2)]
                HT = [sbt(es, "HT%d" % p, [128, 4, 512], BF16) for p in range(2)]
                R_HT = [Res("HT%d" % p) for p in range(2)]
                sg = [sbt(es, "sg%d" % p, [128, 512], F32) for p in range(2)]
                R_sg = [Res("sg%d" % p) for p in range(2)]
                g_b, b_b, R_lnp = load_ln(es, l, gsrc, bsrc, "M%d" % l)
                NXR = 4
                xr = [sbt(es, "xr%d" % p, [128, D], F32) for p in range(NXR)]
                R_xr = [Res("xr%d" % p) for p in range(NXR)]
                lnt = (sbt(es, "m_st", [128, 12], F32), sbt(es, "m_mv", [128, 2], F32), sbt(es, "m_rs", [128, 2], F32))
                R_lnt = Res("mlnt")
                NST = S // ST_TOK
                steps = [(st, e) for st in range(NST) for e in range(NEXP)]

                def load_w(k):
                    st, e = steps[k]
                    p = k % 2
                    T.dma("pool", "lwg%d" % p, wg[p][:], w_exp_gate[l, e].rearrange("(kc p) f -> p kc f", p=128), writes=[R_wg[p]])
                    T.dma("pool", "lwu%d" % p, wu[p][:], w_exp_up[l, e].rearrange("(kc p) f -> p kc f", p=128), writes=[R_wu[p]])
                    T.dma("pool", "lwd%d" % p, wd[p][:], w_exp_down[l, e].rearrange("(fc p) d -> p fc d", p=128), writes=[R_wd[p]])

                def load_x(st):
                    for tb in range(NTB):
                        t0 = st * ST_TOK + tb * 512
                        T.dma("sp", "ldxTs%d" % tb, xTs[:, :, tb * 512:(tb + 1) * 512], XT[:, :, t0:t0 + 512],
                              reads=R_XTd[t0 // 128:t0 // 128 + 4], writes=[R_xTs[tb]])

                load_w(0)
                load_x(0)
                blk = 0
                for k, (st, e) in enumerate(steps):
                    p = k % 2
                    if k + 1 < len(steps):
                        load_w(k + 1)
                    last = (e == NEXP - 1)
                    for tb in range(NTB):
                        hp = blk % 2
                        blk += 1
                        if last:
                            for tt in range(4):
                                gi = st * ST_TILES + tb * 4 + tt
                                T.dma("sp", "ldxr%d" % (gi % NXR), xr[gi % NXR][:], src[gi * 128:(gi + 1) * 128, :], reads=[R_src[gi]], writes=[R_xr[gi % NXR]])
                        for fc in range(4):
                            bg = nb()
                            for kc in range(8):
                                mm(bg.f[:], wg[p][:, kc, fc * 128:(fc + 1) * 128], xTs[:, kc, tb * 512:(tb + 1) * 512], kc == 0, kc == 7,
                                   [R_wg[p], R_xTs[tb]], [bg.res], signal=(kc == 7))
                            bu = nb()
                            for kc in range(8):
                                mm(bu.f[:], wu[p][:, kc, fc * 128:(fc + 1) * 128], xTs[:, kc, tb * 512:(tb + 1) * 512], kc == 0, kc == 7,
                                   [R_wu[p], R_xTs[tb]], [bu.res], signal=(kc == 7))
                            sp_ = fc % 2
                            op("act", lambda: nc.scalar.activation(sg[sp_][:], bg.f[:], AF.Silu), [bg.res], [R_sg[sp_]])
                            op("dve", lambda: nc.vector.tensor_tensor(out=HT[hp][:, fc, :], in0=bu.f[:], in1=sg[sp_][:], op=ALU.mult),
                               [bu.res, R_sg[sp_]], [R_HT[hp]])
                        if last and st + 1 < NST:
                            t0 = (st + 1) * ST_TOK + tb * 512
                            T.dma("sp", "ldxTs%d" % tb, xTs[:, :, tb * 512:(tb + 1) * 512], XT[:, :, t0:t0 + 512],
                                  reads=R_XTd[t0 // 128:t0 // 128 + 4], writes=[R_xTs[tb]])
                        for tt in range(4):
                            ti = tb * 4 + tt
                            gi = st * ST_TILES + ti
                            for dh in range(2):
                                bo = nb()
                                for fc in range(4):
                                    mm(bo.f[:], HT[hp][:, fc, tt * 128:(tt + 1) * 128], wd[p][:, fc, dh * 512:(dh + 1) * 512], fc == 0, fc == 3,
                                       [R_HT[hp], R_wd[p]], [bo.res], signal=(fc == 3))
                                a_ap = acc[:, ti, dh * 512:(dh + 1) * 512]
                                if e == 0:
                                    op("dve", lambda: nc.vector.tensor_scalar(a_ap, bo.f[:], gates_all[:, gi, e:e + 1], None, op0=ALU.mult),
                                       [bo.res, R_gates[gi]], [R_acc[ti]])
                                else:
                                    op("dve", lambda: nc.vector.scalar_tensor_tensor(out=a_ap, in0=bo.f[:], scalar=gates_all[:, gi, e:e + 1], in1=a_ap,
                                                                                     op0=ALU.mult, op1=ALU.add),
                                       [bo.res, R_gates[gi], R_acc[ti]], [R_acc[ti]])
                            if last:
                                q = gi % NXR
                                op("dve", lambda: nc.vector.scalar_tensor_tensor(out=xr[q][:], in0=xr[q][:], scalar=ALPHA, in1=acc[:, ti, :], op0=ALU.mult, op1=ALU.add),
                                   [R_xr[q], R_acc[ti]], [R_xr[q]])
                                layer_norm(lnt, xr[q][:], R_xr[q], g_b[:], b_b[:], R_lnp, R_lnt, xr[q][:], R_xr[q])
                                T.dma("sp", "stM%d" % q, dst[gi * 128:(gi + 1) * 128, :], xr[q][:], reads=[R_xr[q]], writes=[R_dst[gi]])

        if stop_after != "P0":
            phase_A()
            _barrier(T)
        if dbg:
            T.dma("sp", "dg", DG[:, 0, :, :], gates_all[:], reads=R_gates)
        R_out = [Res("out%d" % i) for i in range(NTILE)]

        def phase_B(v_all, R_vall, kbias, R_kb):
            with contextlib.ExitStack() as es:
                use_rot("B0", range(8))
                wkv = sbt(es, "wkv", [128, 8, KV_IN], BF16)
                winb = sbt(es, "winb", [128, 8, B_IN], BF16)
                R_wkv = [Res("wkv%d" % kc) for kc in range(8)]
                R_winb = [Res("winb%d" % kc) for kc in range(8)]
                wkv_v = w_kv_shared.rearrange("(kc p) n -> p kc n", p=128)
                winb_v = w_in_b.rearrange("(kc p) n -> p kc n", p=128)
                for kc in range(8):
                    T.dma("pool", "wB_%d" % kc, wkv[:, kc, :], wkv_v[:, kc, :], writes=[R_wkv[kc]])
                    T.dma("pool", "wBb_%d" % kc, winb[:, kc, :], winb_v[:, kc, :], writes=[R_winb[kc]])
                gk_b = sbt(es, "gk_b", [128, 64], F32)
                gq_b = sbt(es, "gq_b", [128, 64], F32)
                bf_b = sbt(es, "bf_b", [128, 12], F32)
                R_pb = Res("parB")
                T.dma("sp", "pB0", gk_b[:], k_norm_g.broadcast_to([128, 64]), writes=[R_pb])
                T.dma("sp", "pB1", gq_b[:], q_norm_g.broadcast_to([128, 64]), writes=[R_pb])
                T.dma("sp", "pB2", bf_b[:], b_forget.broadcast_to([128, 12]), writes=[R_pb])
                xt = [sbt(es, "bxt%d" % p, [128, D], F32) for p in range(2)]
                xT = [sbt(es, "bxT%d" % p, [128, 8, 128], BF16) for p in range(2)]
                xb = [sbt(es, "bxb%d" % p, [128, D], BF16) for p in range(2)]
                R_xb = [Res("bxb%d" % p) for p in range(2)]
                hk = [sbt(es, "hk%d" % p, [128, KV_IN], F32) for p in range(2)]
                hb = [sbt(es, "hb%d" % p, [128, B_IN], F32) for p in range(2)]
                R_xt = [Res("bxt%d" % p) for p in range(2)]
                R_xT = [Res("bxT%d" % p) for p in range(2)]
                GK = [(0, 512), (512, 1024), (1024, 1536), (1536, KV_IN)]
                GB = [(0, 512), (512, 1024), (1024, 1536), (1536, B_IN)]
                R_hk = [[Res("hk%d_%d" % (p, g)) for g in range(4)] for p in range(2)]
                R_hb = [[Res("hb%d_%d" % (p, g)) for g in range(4)] for p in range(2)]
                sqt = sbt(es, "b_sq", [128, 768], F32)
                nrm = sbt(es, "b_nrm", [128, 768], F32)
                ssn = sbt(es, "b_ss", [128, 24], F32)
                k_augs = [sbt(es, "k_aug%d" % p, [128, 12, 72], BF16) for p in range(2)]
                q_augs = [sbt(es, "q_aug%d" % p, [128, 12, 72], BF16) for p in range(2)]
                lf = sbt(es, "b_lf", [128, 12], F32)
                carry = sbt(es, "b_carry", [128, 12], F32)
                t1 = sbt(es, "b_t1", [128, 12], F32)
                ogq = sbt(es, "b_ogq", [128, D], BF16)
                qT_t = sbt(es, "qT_t", [66, 12, 128], BF16)
                kT_t = sbt(es, "kT_t", [66, 12, 128], BF16)
                R_sq, R_nrm, R_ssn, R_lf, R_carry, R_t1, R_ogq, R_qTt, R_kTt = (
                    Res(n) for n in ["bsq", "bnrm", "bss", "lf", "carry", "t1", "ogq", "qTt", "kTt"])
                R_kas = [Res("ka%d" % p) for p in range(2)]
                R_qas = [Res("qa%d" % p) for p in range(2)]
                for p in range(2):
                    op("dve", lambda: nc.vector.memset(k_augs[p][:], 1.0), [], [R_kas[p]])
                    op("dve", lambda: nc.vector.memset(q_augs[p][:], 0.0), [], [R_qas[p]])
                op("dve", lambda: nc.vector.memset(carry[:], 0.0), [], [R_carry])
                for c4 in range(4):
                    op("pool", lambda: nc.gpsimd.memset(v_all[:, c4 * 8:(c4 + 1) * 8, :, :], 1.0), [], [R_vall[c4 * 8 + j] for j in range(8)])

                def grp_of(G, lo, hi):
                    return [g for g, (a, b) in enumerate(G) if a < hi and b > lo]

                def S1(i):
                    use_rot("B1", [0, 1, 2])
                    p = i % 2
                    T.dma("sp", "bldx%d" % p, xt[p][:], XB[i * 128:(i + 1) * 128, :], reads=[R_XB[i]], writes=[R_xt[p]])
                    op("pool", lambda: nc.gpsimd.tensor_copy(xb[p][:], xt[p][:]), [R_xt[p]], [R_xb[p]])
                    bk = nb()
                    for kc in range(8):
                        tr(bk.b[:, kc * 128:(kc + 1) * 128], xb[p][:, kc * 128:(kc + 1) * 128], id_b[:], [R_xb[p], R_const], [bk.res], signal=(kc == 7))
                    op("act", lambda: nc.scalar.copy(xT[p][:], bk.b[:].rearrange("p (k t) -> p k t", k=8)), [bk.res], [R_xT[p]])
                    n = 0
                    for (G, w, Rw, dst, Rd) in ((GK, wkv, R_wkv, hk[p], R_hk[p]), (GB, winb, R_winb, hb[p], R_hb[p])):
                        for g, (a, b) in enumerate(G):
                            bk = nb()
                            for kc in range(8):
                                mm(bk.f[:, 0:b - a], xT[p][:, kc, :], w[:, kc, a:b], kc == 0, kc == 7, [R_xT[p], Rw[kc]], [bk.res], signal=(kc == 7))
                            if n % 2 == 0:
                                op("dve", lambda: nc.vector.tensor_copy(dst[:, a:b], bk.f[:, 0:b - a]), [bk.res], [Rd[g]])
                            else:
                                op("act", lambda: nc.scalar.copy(dst[:, a:b], bk.f[:, 0:b - a]), [bk.res], [Rd[g]])
                            n += 1

                def rmsn(src, Rsrc, gb, aug, R_aug, off):
                    v3 = lambda a: a.rearrange("p (h e) -> p h e", h=12)
                    op("act", lambda: nc.scalar.activation(sqt[:], src, AF.Square), Rsrc, [R_sq])
                    op("dve", lambda: nc.vector.tensor_reduce(out=ssn[:, off:off + 12], in_=v3(sqt[:]), axis=AX.X, op=ALU.add), [R_sq], [R_ssn])
                    op("act", lambda: nc.scalar.activation(ssn[:, off:off + 12], ssn[:, off:off + 12], AF.Sqrt, bias=EPS_RMS, scale=1.0 / 64.0), [R_ssn, R_const], [R_ssn])
                    op("dve", lambda: nc.vector.reciprocal(ssn[:, off:off + 12], ssn[:, off:off + 12]), [R_ssn], [R_ssn])
                    op("dve", lambda: nc.vector.tensor_tensor(out=v3(nrm[:]), in0=v3(src), in1=ssn[:, off:off + 12].unsqueeze(2).broadcast_to([128, 12, 64]), op=ALU.mult),
                       Rsrc + [R_ssn], [R_nrm])
                    op("dve", lambda: nc.vector.tensor_tensor(out=aug[:, :, 0:64], in0=v3(nrm[:]), in1=gb[:].unsqueeze(1).broadcast_to([128, 12, 64]), op=ALU.mult),
                       [R_nrm, R_pb], [R_aug])

                def S2(i):
                    use_rot("B2", [3])
                    p = i % 2
                    k_aug, q_aug, R_ka, R_qa = k_augs[p], q_augs[p], R_kas[p], R_qas[p]
                    Rk = lambda lo, hi: [R_hk[p][g] for g in grp_of(GK, lo, hi)]
                    Rb = lambda lo, hi: [R_hb[p][g] for g in grp_of(GB, lo, hi)]
                    rmsn(hk[p][:, 0:768], Rk(0, 768), gk_b, k_aug, R_ka, 0)
                    rmsn(hb[p][:, 0:768], Rb(0, 768), gq_b, q_aug, R_qa, 12)
                    op("dve", lambda: nc.vector.tensor_tensor(out=lf[:], in0=hk[p][:, 1536:1548], in1=bf_b[:], op=ALU.add), Rk(1536, 1548) + [R_pb], [R_lf])
                    op("act", lambda: nc.scalar.activation(lf[:], lf[:], AF.Exp, scale=-1.0), [R_lf], [R_lf])
                    op("act", lambda: nc.scalar.activation(lf[:], lf[:], AF.Ln, bias=ONE, scale=1.0), [R_lf, R_const], [R_lf])
                    bk = nb()
                    mm(bk.f[:, 0:12], triu[:], lf[:], True, True, [R_const, R_lf], [bk.res], signal=False)
                    mm(bk.f[:, 16:28], ones_f[:], lf[:], True, True, [R_const, R_lf], [bk.res], signal=True)
                    op("dve", lambda: nc.vector.tensor_tensor(out=kbias[:, i, :], in0=bk.f[:, 0:12], in1=carry[:], op=ALU.add), [bk.res, R_carry], [R_kb[i]])
                    op("dve", lambda: nc.vector.tensor_tensor(out=carry[:], in0=bk.f[:, 16:28], in1=carry[:], op=ALU.add), [bk.res, R_carry], [R_carry])
                    op("dve", lambda: nc.vector.tensor_scalar(t1[:], kbias[:, i, :], -8.0, None, op0=ALU.mult), [R_kb[i]], [R_t1])
                    op("dve", lambda: nc.vector.tensor_copy(q_aug[:, :, 64], t1[:]), [R_t1], [R_qa])
                    op("dve", lambda: nc.vector.tensor_tensor(out=q_aug[:, :, 65], in0=t1[:], in1=q_aug[:, :, 64], op=ALU.subtract), [R_t1, R_qa], [R_qa])
                    op("pool", lambda: nc.gpsimd.tensor_copy(v_all[:, i, :, 0:64], hk[p][:, 768:1536].rearrange("p (h e) -> p h e", h=12)), Rk(768, 1536), [R_vall[i]])
                    op("act", lambda: nc.scalar.activation(ogq[:, 0:768], hb[p][:, 768:1536], AF.Sigmoid), Rb(768, 1536), [R_ogq])
                    op("pool", lambda: nc.gpsimd.tensor_copy(ogq[:, 768:1024], hb[p][:, 1536:1792]), Rb(1536, 1792), [R_ogq])
                    T.dma("sp", "sOGQ", OGQ[i * 128:(i + 1) * 128, :], ogq[:], reads=[R_ogq], writes=[R_OGQ[i]])

                def S2b(i):
                    use_rot("B2b", [4, 5, 6, 7])
                    p = i % 2
                    k_aug, q_aug, R_ka, R_qa = k_augs[p], q_augs[p], R_kas[p], R_qas[p]
                    for (aug, R_aug, tt, R_tt, dram, R_d, key, eng) in ((q_aug, R_qa, qT_t, R_qTt, QT, R_QTd, "sQT", "act"), (k_aug, R_ka, kT_t, R_kTt, KT, R_KTd, "sKT", "dve")):
                        for (h0, h1) in ((0, 8), (8, 12)):
                            bk = nb()
                            for h in range(h0, h1):
                                tr(bk.b[0:66, (h - h0) * 128:(h - h0 + 1) * 128], aug[:, h, 0:66], id_b[:], [R_aug, R_const], [bk.res], signal=(h == h1 - 1))
                            src = bk.b[0:66, 0:(h1 - h0) * 128].rearrange("p (h t) -> p h t", h=h1 - h0)
                            if eng == "act":
                                op("act", lambda: nc.scalar.copy(tt[:, h0:h1, :], src), [bk.res], [R_tt])
                            else:
                                op("dve", lambda: nc.vector.tensor_copy(tt[:, h0:h1, :], src), [bk.res], [R_tt])
                        T.dma("sp", key, dram.rearrange("h p t -> p h t")[:, :, i * 128:(i + 1) * 128], tt[:], reads=[R_tt], writes=[R_d[i]])

                for step in range(NTILE + 2):
                    chains = []
                    if step < NTILE:
                        chains.append(lambda: S1(step))
                    if 0 <= step - 1 < NTILE:
                        chains.append(lambda: S2(step - 1))
                    if 0 <= step - 2 < NTILE:
                        chains.append(lambda: S2b(step - 2))
                    COOP.run(chains)

        def phase_C(v_all, R_vall, kbias, R_kb, o_all, R_oall):
            with contextlib.ExitStack() as es:
                kTh = [sbt(es, "kTh%d" % p, [66, S], BF16) for p in range(2)]
                qTh = [sbt(es, "qTh%d" % p, [66, S], BF16) for p in range(2)]
                R_kTh = [Res("kTh%d" % p) for p in range(2)]
                R_qTh = [Res("qTh%d" % p) for p in range(2)]
                NPB = 6
                pex = [sbt(es, "pex%d" % j, [128, 512], BF16) for j in range(NPB)]
                R_pex = [Res("pex%d" % j) for j in range(NPB)]
                rdc = sbt(es, "rdc", [128, 4], F32)
                R_rdc = Res("rdc")
                set_rot([4, 5, 6, 7])
                acc_b = [banks[j] for j in range(4)]

                def load_h(h):
                    p = h % 2
                    T.dma("sp", "ldk%d" % p, kTh[p][:], KT[h], reads=R_KTd, writes=[R_kTh[p]])
                    T.dma("sp", "ldq%d" % p, qTh[p][:], QT[h], reads=R_QTd, writes=[R_qTh[p]])

                load_h(0)
                units = [(h, Q, c) for h in range(12) for Q in range(8) for c in range(4 * Q + 4)]
                sb_of = {}
                DEPTH = 3

                def qk(u):
                    h, Q, c = units[u]
                    p = h % 2
                    if Q == 0 and c == 0 and h + 1 < 12:
                        load_h(h + 1)
                    lo = 128 * max(0, c - 4 * Q)
                    bs = nb()
                    mm(bs.f[:, lo:512], kTh[p][0:66, c * 128:(c + 1) * 128], qTh[p][0:66, Q * 512 + lo:(Q + 1) * 512], True, True,
                       [R_kTh[p], R_qTh[p]], [bs.res])
                    sb_of[u] = bs

                def rest(u):
                    h, Q, c = units[u]
                    j0 = max(0, c - 4 * Q)
                    lo = 128 * j0
                    bs = sb_of.pop(u)
                    pj = u % NPB
                    op("act", lambda: nc.scalar.activation(pex[pj][:, lo:512], bs.f[:, lo:512], AF.Exp, bias=kbias[:, c, h:h + 1], scale=0.125),
                       [bs.res, R_kb[c]], [R_pex[pj]])
                    if c >= 4 * Q:
                        op("dve", lambda: nc.vector.tensor_tensor(out=pex[pj][:, lo:lo + 128], in0=pex[pj][:, lo:lo + 128], in1=triu_b[:], op=ALU.mult),
                           [R_pex[pj], R_const], [R_pex[pj]])
                    for j in range(j0, 4):
                        mm(acc_b[j].f[:, 0:65], pex[pj][:, j * 128:(j + 1) * 128], v_all[:, c, h, 0:65], c == 0, c == 4 * Q + j,
                           [R_pex[pj], R_vall[c]], [acc_b[j].res], signal=(j == 3))
                    if c == 4 * Q + 3:
                        for j in range(4):
                            ti = 4 * Q + j
                            op("dve", lambda: nc.vector.reciprocal(rdc[:, j:j + 1], acc_b[j].f[:, 64:65]), [acc_b[j].res], [R_rdc])
                            op("dve", lambda: nc.vector.tensor_scalar(o_all[:, ti, h * 64:(h + 1) * 64], acc_b[j].f[:, 0:64], rdc[:, j:j + 1], None, op0=ALU.mult),
                               [acc_b[j].res, R_rdc], [R_oall[ti]])

                for u in range(min(DEPTH, len(units))):
                    qk(u)
                for u in range(len(units)):
                    if u + DEPTH < len(units):
                        qk(u + DEPTH)
                    rest(u)

        def phase_D(o_all, R_oall):
            with contextlib.ExitStack() as es:
                use_rot("D0", range(8))
                wout_b = sbt(es, "wout_d", [128, 8, D], BF16)
                R_wout = Res("woutD")
                T.dma("pool", "wD", wout_b[:], w_out[1].rearrange("(kc p) n -> p kc n", p=128), writes=[R_wout])
                g_b, b_b, R_lnp = load_ln(es, 1, ln_mix_g, ln_mix_b, "D")
                xt = [sbt(es, "dxt%d" % p, [128, D], F32) for p in range(4)]
                og = [sbt(es, "dog%d" % p, [128, D], BF16) for p in range(2)]
                R_xt = [Res("dxt%d" % p) for p in range(4)]
                R_og = [Res("dog%d" % p) for p in range(2)]
                cats = [sbt(es, "dcat%d" % p, [128, D], BF16) for p in range(2)]
                R_cats = [Res("dcat%d" % p) for p in range(2)]
                memt = alloc_mem_tiles(es)
                outt = alloc_out_tiles(es)

                def S1(i):
                    p = i % 2
                    T.dma("sp", "dldx%d" % (i % 4), xt[i % 4][:], XB[i * 128:(i + 1) * 128, :], reads=[R_XB[i]], writes=[R_xt[i % 4]])
                    T.dma("sp", "dldo%d" % p, og[p][:], OGQ[i * 128:(i + 1) * 128, :], reads=[R_OGQ[i]], writes=[R_og[p]])

                def S2(i):
                    use_rot("D2", [0, 1, 2])
                    p = i % 2
                    cat, R_cat = cats[p], R_cats[p]
                    op("dve", lambda: nc.vector.tensor_tensor(out=cat[:, 0:768], in0=o_all[:, i, :], in1=og[p][:, 0:768], op=ALU.mult), [R_oall[i], R_og[p]], [R_cat])
                    mem_attn(1, memt, og[p][:, 768:1024], R_og[p], cat[:, 768:1024], R_cat)
                    if dbg:
                        T.dma("sp", "dcat", DCAT[1, i * 128:(i + 1) * 128, :], cat[:], reads=[R_cat])

                def S3a(i):
                    use_rot("D3a", [3, 4, 5])
                    out_ln_router(1, i, outt, cats[i % 2], R_cats[i % 2], xt[i % 4], R_xt[i % 4], wout_b, g_b, b_b, R_wout, R_lnp, part="a")

                def S3b(i):
                    use_rot("D3b", [6, 7])
                    out_ln_router(1, i, outt, cats[i % 2], R_cats[i % 2], xt[i % 4], R_xt[i % 4], wout_b, g_b, b_b, R_wout, R_lnp, part="b")

                for step in range(NTILE + 3):
                    chains = []
                    if step < NTILE:
                        chains.append(lambda: S1(step))
                    if 0 <= step - 1 < NTILE:
                        chains.append(lambda: S2(step - 1))
                    if 0 <= step - 2 < NTILE:
                        chains.append(lambda: S3a(step - 2))
                    if 0 <= step - 3 < NTILE:
                        chains.append(lambda: S3b(step - 3))
                    COOP.run(chains)

        if stop_after in ("A", "P0"):
            pass
        else:
            phase_M(0, XA, R_XA, XB, R_XB, ln_ffn_g, ln_ffn_b)
            _barrier(T)
            if stop_after != "M0":
                with contextlib.ExitStack() as esL:
                    v_all = sbt(esL, "v_all", [128, NTILE, 12, 72], BF16)
                    kbias = sbt(esL, "kbias", [128, NTILE, 12], F32)
                    R_vall = [Res("vall%d" % i) for i in range(NTILE)]
                    R_kb = [Res("kb%d" % i) for i in range(NTILE)]
                    phase_B(v_all, R_vall, kbias, R_kb)
                    _barrier(T)
                    if stop_after != "B":
                        with contextlib.ExitStack() as esO:
                            o_all = sbt(esO, "o_all", [128, NTILE, 768], BF16)
                            R_oall = [Res("oall%d" % i) for i in range(NTILE)]
                            phase_C(v_all, R_vall, kbias, R_kb, o_all, R_oall)
                            _barrier(T)
                            phase_D(o_all, R_oall)
                            _barrier(T)
                            if dbg:
                                T.dma("sp", "dg", DG[:, 1, :, :], gates_all[:], reads=R_gates)
                _barrier(T)
                if stop_after not in ("B", "D"):
                    phase_M(1, XA, R_XA, out, R_out, ln_ffn_g, ln_ffn_b)

        _barrier(T)
    print("build: ninst=%d nwait=%d" % (T.ninst, T.nwait))
    T.close()
    return nc


_CONSTS = None


def _consts():
    global _CONSTS
    if _CONSTS is None:
        ident = np.eye(128, dtype=np.float32)
        triu = np.triu(np.ones((128, 128), dtype=np.float32))
        _CONSTS = {"c_ident": ident, "c_triu": triu}
    return _CONSTS


def make_in_map(inputs, b):
    f = lambda a: np.ascontiguousarray(np.asarray(a, dtype=np.float32))
    m = {
        "x": f(inputs["x"][b]), "mem": f(inputs["mem"][b]),
        "w_in_a": f(inputs["w_in_a"][0]), "w_gate_up_a": f(inputs["w_gate_up_a"][0]),
        "b_gate_a": f(inputs["b_gate_a"]).reshape(1, 384), "gla_norm_g": f(inputs["gla_norm_g"]).reshape(1, 192),
        "w_in_b": f(inputs["w_in_b"][0]), "q_norm_g": f(inputs["q_norm_g"]).reshape(1, 64),
        "w_kv_shared": f(inputs["w_kv_shared"]), "b_forget": f(inputs["b_forget"]).reshape(1, 12),
        "k_norm_g": f(inputs["k_norm_g"]).reshape(1, 64), "w_mem_kv": f(inputs["w_mem_kv"]),
        "w_out": f(inputs["w_out"]), "ln_mix_g": f(inputs["ln_mix_g"]), "ln_mix_b": f(inputs["ln_mix_b"]),
        "ln_ffn_g": f(inputs["ln_ffn_g"]), "ln_ffn_b": f(inputs["ln_ffn_b"]),
        "w_router": f(inputs["w_router"]), "b_router": f(inputs["b_router"]).reshape(1, 16),
        "w_exp_gate": f(inputs["w_exp_gate"]), "w_exp_up": f(inputs["w_exp_up"]), "w_exp_down": f(inputs["w_exp_down"]),
    }
    m.update(_consts())
    return m


def kernel(**inputs):
    nc = build()
    in_maps = [make_in_map(inputs, b) for b in range(N_CORES)]
    res = run_bass_kernel_spmd(nc, in_maps, core_ids=list(range(N_CORES)))
    return np.stack([np.asarray(r["out"], dtype=np.float32) for r in res.results], axis=0)
```

```python
import contextlib
import os
import threading
import numpy as np
import concourse.bass as bass
import concourse.mybir as mybir
from concourse.bass_utils import run_bass_kernel_spmd

F32 = mybir.dt.float32
BF16 = mybir.dt.bfloat16
AF = mybir.ActivationFunctionType
ALU = mybir.AluOpType
AX = mybir.AxisListType

S = 4096
D = 1024
NTILE = S // 128
N_CORES = 8
ALPHA = (2.0 * 2) ** 0.25
LN_EPS = 1e-5
RMS_EPS = 1e-6
A_IN = 2576
B_IN = 1792
KV_IN = 1548
NEXP = 16
ST_TOK = 2048
ST_TILES = ST_TOK // 128


class Coop:
    def __init__(self):
        self.tl = threading.local()

    def switch(self):
        st = getattr(self.tl, "st", None)
        if st is None:
            return
        i, sems, done, n = st
        for k in range(1, n):
            j = (i + k) % n
            if not done[j]:
                sems[j].release()
                sems[i].acquire()
                return

    def run(self, fns):
        n = len(fns)
        if n == 1:
            fns[0]()
            return
        sems = [threading.Semaphore(0) for _ in range(n)]
        done = [False] * n
        main = threading.Semaphore(0)
        errs = []

        def worker(i):
            sems[i].acquire()
            self.tl.st = (i, sems, done, n)
            try:
                fns[i]()
            except BaseException as e:
                errs.append(e)
            done[i] = True
            for k in range(1, n):
                j = (i + k) % n
                if not done[j]:
                    sems[j].release()
                    return
            main.release()

        ths = [threading.Thread(target=worker, args=(i,)) for i in range(n)]
        for t in ths:
            t.start()
        sems[0].release()
        main.acquire()
        for t in ths:
            t.join()
        if errs:
            raise errs[0]


COOP = Coop()


class Res:
    __slots__ = ("name", "w", "r", "excl")

    def __init__(self, name, excl=False):
        self.name = name
        self.w = None
        self.r = []
        self.excl = excl


class Trk:
    def __init__(self, nc):
        self.nc = nc
        self.eng = {"pe": nc.tensor, "act": nc.scalar, "dve": nc.vector, "pool": nc.gpsimd, "sp": nc.sync}
        self.sem = {}
        self.cnt = {}
        self.known = {e: {} for e in self.eng}
        self._stack = []
        for e in self.eng:
            cm = nc.semaphore("s_" + e)
            self.sem[e] = cm.__enter__()
            self._stack.append(cm)
            self.cnt[e] = 0
        self.dsem = {}
        self.dcnt = {}
        self.nwait = 0
        self.ninst = 0

    def close(self):
        for cm in reversed(self._stack):
            cm.__exit__(None, None, None)

    def _dma_sem(self, key):
        if key not in self.dsem:
            cm = self.nc.semaphore("d_" + key)
            self.dsem[key] = cm.__enter__()
            self._stack.append(cm)
            self.dcnt[key] = 0
        return self.dsem[key]

    def _need(self, e, toks):
        best = {}
        for t in toks:
            if t is None:
                continue
            name, sem, val, src = t
            if src == "pe" and e == "pe":
                continue
            if self.known[e].get(name, 0) >= val:
                continue
            if best.get(name, (None, 0))[1] < val:
                best[name] = (sem, val)
        for name, (sem, val) in best.items():
            self.eng[e].wait_ge(sem, val)
            self.known[e][name] = val
            self.nwait += 1

    @staticmethod
    def _compact(lst):
        best = {}
        for t in lst:
            if t[0] not in best or best[t[0]][2] < t[2]:
                best[t[0]] = t
        return list(best.values())

    def op(self, e, fn, reads=(), writes=(), signal=True):
        toks = []
        for r in reads:
            toks.append(r.w)
            if r.excl and e != "pe":
                toks.extend(r.r)
        for w in writes:
            toks.append(w.w)
            toks.extend(w.r)
        self._need(e, toks)
        ins = fn()
        self.ninst += 1
        if signal:
            self.cnt[e] += 1
            ins.then_inc(self.sem[e], 1)
            tok = ("E" + e, self.sem[e], self.cnt[e], e)
        else:
            tok = ("E" + e, self.sem[e], self.cnt[e] + 1, e)
        for r in reads:
            if r in writes:
                continue
            r.r.append(tok)
            if len(r.r) > 16:
                r.r = self._compact(r.r)
        for w in writes:
            w.w = tok
            w.r = []
        COOP.switch()
        return ins

    def dma(self, q, key, out, in_, reads=(), writes=()):
        toks = []
        for r in reads:
            toks.append(r.w)
        for w in writes:
            toks.append(w.w)
            toks.extend(w.r)
        self._need(q, toks)
        sem = self._dma_sem(key)
        self.dcnt[key] += 16
        self.eng[q].dma_start(out=out, in_=in_).then_inc(sem, 16)
        self.ninst += 1
        tok = ("D" + key, sem, self.dcnt[key], "dma")
        for r in reads:
            r.r.append(tok)
            if len(r.r) > 16:
                r.r = self._compact(r.r)
        for w in writes:
            w.w = tok
            w.r = []
        COOP.switch()
        return tok


def _barrier(T):
    for e in T.eng:
        for f in T.eng:
            if f != e and T.cnt[f] > 0 and T.known[e].get("E" + f, 0) < T.cnt[f]:
                T.eng[e].wait_ge(T.sem[f], T.cnt[f])
                T.known[e]["E" + f] = T.cnt[f]
                T.nwait += 1
        for key in list(T.dsem.keys()):
            if T.known[e].get("D" + key, 0) < T.dcnt[key]:
                T.eng[e].wait_ge(T.dsem[key], T.dcnt[key])
                T.known[e]["D" + key] = T.dcnt[key]
                T.nwait += 1


class Bank:
    def __init__(self, f, res):
        self.f = f
        self.b = f.bitcast(BF16)
        self.res = res


def build(dbg=False, stop_after=None):
    nc = bass.Bass("TRN2", target_bir_lowering=False)

    def din(name, shape):
        return nc.dram_tensor(name, list(shape), F32, kind="ExternalInput").ap()

    x = din("x", [S, D])
    mem = din("mem", [256, D])
    w_in_a = din("w_in_a", [D, A_IN])
    w_gate_up_a = din("w_gate_up_a", [16, 384])
    b_gate_a = din("b_gate_a", [1, 384])
    gla_norm_g = din("gla_norm_g", [1, 192])
    w_in_b = din("w_in_b", [D, B_IN])
    q_norm_g = din("q_norm_g", [1, 64])
    w_kv_shared = din("w_kv_shared", [D, KV_IN])
    b_forget = din("b_forget", [1, 12])
    k_norm_g = din("k_norm_g", [1, 64])
    w_mem_kv = din("w_mem_kv", [2, D, 512])
    w_out = din("w_out", [2, D, D])
    ln_mix_g = din("ln_mix_g", [2, D])
    ln_mix_b = din("ln_mix_b", [2, D])
    ln_ffn_g = din("ln_ffn_g", [2, D])
    ln_ffn_b = din("ln_ffn_b", [2, D])
    w_router = din("w_router", [D, 16])
    b_router = din("b_router", [1, 16])
    w_exp_gate = din("w_exp_gate", [2, NEXP, D, 512])
    w_exp_up = din("w_exp_up", [2, NEXP, D, 512])
    w_exp_down = din("w_exp_down", [2, NEXP, 512, D])
    c_ident = din("c_ident", [128, 128])
    c_triu = din("c_triu", [128, 128])

    out = nc.dram_tensor("out", [S, D], F32, kind="ExternalOutput").ap()
    kscr = "ExternalOutput" if dbg else "Internal"
    XA = nc.dram_tensor("XA", [S, D], F32, kind=kscr).ap()
    XB = nc.dram_tensor("XB", [S, D], F32, kind=kscr).ap()
    XT = nc.dram_tensor("XT", [128, 8, S], BF16, kind="Internal").ap()
    QT = nc.dram_tensor("QT", [12, 66, S], BF16, kind="Internal").ap()
    KT = nc.dram_tensor("KT", [12, 66, S], BF16, kind="Internal").ap()
    OGQ = nc.dram_tensor("OGQ", [S, D], BF16, kind="Internal").ap()
    if dbg:
        DG = nc.dram_tensor("DG", [128, 2, NTILE, 16], F32, kind="ExternalOutput").ap()
        DCAT = nc.dram_tensor("DCAT", [2, S, D], BF16, kind="ExternalOutput").ap()

    T = Trk(nc)
    out_toks = []
    R_XA = [Res('XA%d' % i) for i in range(NTILE)]
    R_XB = [Res('XB%d' % i) for i in range(NTILE)]
    R_XTd = [Res('XTd%d' % i) for i in range(NTILE)]
    R_OGQ = [Res('OGQ%d' % i) for i in range(NTILE)]
    R_QTd = [Res('QTd%d' % i) for i in range(NTILE)]
    R_KTd = [Res('KTd%d' % i) for i in range(NTILE)]

    with contextlib.ExitStack() as ges:
        uid = [0]

        def sbt(es, name, shape, dt):
            uid[0] += 1
            return es.enter_context(nc.sbuf_tensor("%s_%d" % (name, uid[0]), list(shape), dt))

        banks = []
        for i in range(8):
            pt = ges.enter_context(nc.psum_tensor("pb%d" % i, [128, 512], F32))
            banks.append(Bank(pt[:], Res("pb%d" % i, excl=True)))
        rots = {}
        rtl = threading.local()

        def use_rot(name, lst):
            if name not in rots:
                rots[name] = {"lst": list(lst), "i": 0}
            rtl.cur = rots[name]

        def set_rot(lst):
            use_rot("main%s" % (tuple(lst),), lst)

        def nb():
            rot = rtl.cur
            b = banks[rot["lst"][rot["i"] % len(rot["lst"])]]
            rot["i"] += 1
            return b

        def op(e, fn, reads=(), writes=(), signal=True):
            return T.op(e, fn, reads, writes, signal)

        def mm(o, lhsT, rhs, start, stop, reads, writes, signal=True):
            return T.op("pe", lambda: nc.tensor.matmul(o, lhsT, rhs, start=start, stop=stop), reads, writes, signal)

        def tr(o, in_, ident, reads, writes, signal=True):
            return T.op("pe", lambda: nc.tensor.transpose(o, in_, ident), reads, writes, signal)

        id_f = sbt(ges, "id_f", [128, 128], F32)
        id_b = sbt(ges, "id_b", [128, 128], BF16)
        triu = sbt(ges, "triu", [128, 128], F32)
        triu_b = sbt(ges, "triu_b", [128, 128], BF16)
        ones_f = sbt(ges, "ones_f", [128, 128], F32)
        cst = sbt(ges, "cst", [128, 4], F32)
        gates_all = sbt(ges, "gates_all", [128, NTILE, 16], F32)
        mkT = sbt(ges, "mkT", [128, 2, 2, 256], BF16)
        mv_aug = sbt(ges, "mv_aug", [128, 2, 2, 4, 72], BF16)
        wr_f = sbt(ges, "wr_f", [128, 8, 16], F32)
        br_b = sbt(ges, "br_b", [128, 16], F32)
        R_const = Res("const")
        R_gates = [Res("gates%d" % i) for i in range(NTILE)]
        R_mk = Res("mk")
        T.dma("sp", "c0", id_f[:], c_ident, writes=[R_const])
        T.dma("sp", "c1", triu[:], c_triu, writes=[R_const])
        T.dma("pool", "c2", id_b[:], c_ident, writes=[R_const])
        T.dma("pool", "c3", triu_b[:], c_triu, writes=[R_const])
        T.dma("sp", "c4", wr_f[:], w_router.rearrange("(kc p) e -> p kc e", p=128), writes=[R_const])
        T.dma("sp", "c5", br_b[:], b_router.broadcast_to([128, 16]), writes=[R_const])
        op("dve", lambda: nc.vector.memset(ones_f[:], 1.0), writes=[R_const])
        op("dve", lambda: nc.vector.memset(cst[:, 0:1], LN_EPS), writes=[R_const])
        op("dve", lambda: nc.vector.memset(cst[:, 1:2], RMS_EPS), writes=[R_const])
        op("dve", lambda: nc.vector.memset(cst[:, 2:3], 1.0), writes=[R_const])
        op("dve", lambda: nc.vector.memset(mv_aug[:], 1.0), writes=[R_mk])
        EPS_LN = cst[:, 0:1]
        EPS_RMS = cst[:, 1:2]
        ONE = cst[:, 2:3]

        with contextlib.ExitStack() as es:
            set_rot(range(8))
            mem_f = sbt(es, "mem_f", [128, 2, D], F32)
            memT = sbt(es, "memT", [128, 8, 256], BF16)
            wm = sbt(es, "wm", [128, 8, 512], BF16)
            R_memf, R_memT, R_wm = Res("memf"), Res("memT"), Res("wm")
            T.dma("sp", "p0a", mem_f[:], mem.rearrange("(mc p) d -> p mc d", p=128), writes=[R_memf])
            for mc in range(2):
                for half in range(2):
                    bk = nb()
                    for q in range(4):
                        kc = half * 4 + q
                        tr(bk.f[:, q * 128:(q + 1) * 128], mem_f[:, mc, kc * 128:(kc + 1) * 128], id_f[:],
                           [R_memf, R_const], [bk.res], signal=(q == 3))
                    op("act", lambda: nc.scalar.copy(memT[:, half * 4:(half + 1) * 4, mc * 128:(mc + 1) * 128],
                                                     bk.f[:].rearrange("p (q t) -> p q t", q=4)),
                       [bk.res], [R_memT])
            for l in range(2):
                T.dma("pool", "p0b", wm[:], w_mem_kv[l].rearrange("(kc p) n -> p kc n", p=128), writes=[R_wm])
                for j in range(2):
                    bk = nb()
                    for kc in range(8):
                        mm(bk.f[:, 0:256], wm[:, kc, j * 128:(j + 1) * 128], memT[:, kc, :], kc == 0, kc == 7,
                           [R_wm, R_memT], [bk.res], signal=(kc == 7))
                    op("act", lambda: nc.scalar.copy(mkT[:, l, j, :], bk.f[:, 0:256]), [bk.res], [R_mk])
                for mc in range(2):
                    bk = nb()
                    for kc in range(8):
                        mm(bk.f[:, 0:256], memT[:, kc, mc * 128:(mc + 1) * 128], wm[:, kc, 256:512], kc == 0, kc == 7,
                           [R_wm, R_memT], [bk.res], signal=(kc == 7))
                    op("dve", lambda: nc.vector.tensor_copy(mv_aug[:, l, mc, :, 0:64],
                                                            bk.f[:, 0:256].rearrange("p (h e) -> p h e", h=4)),
                       [bk.res], [R_mk])

        _barrier(T)
        def layer_norm(es_tiles, zt, R_zt, g_b, b_b, R_gb, R_ln, outap, R_out):
            st, mv, rs = es_tiles
            op("dve", lambda: nc.vector.bn_stats(st[:, 0:6], zt[:, 0:512]), [R_zt], [R_ln])
            op("dve", lambda: nc.vector.bn_stats(st[:, 6:12], zt[:, 512:1024]), [R_zt], [R_ln])
            op("dve", lambda: nc.vector.bn_aggr(mv[:, 0:2], st[:, 0:12]), [R_ln], [R_ln])
            op("act", lambda: nc.scalar.activation(rs[:, 0:1], mv[:, 1:2], AF.Sqrt, bias=EPS_LN, scale=1.0), [R_ln, R_const], [R_ln])
            op("dve", lambda: nc.vector.reciprocal(rs[:, 1:2], rs[:, 0:1]), [R_ln], [R_ln])
            op("dve", lambda: nc.vector.scalar_tensor_tensor(out=zt, in0=zt, scalar=mv[:, 0:1], in1=g_b, op0=ALU.subtract, op1=ALU.mult), [R_zt, R_ln, R_gb], [R_zt])
            op("dve", lambda: nc.vector.scalar_tensor_tensor(out=outap, in0=zt, scalar=rs[:, 1:2], in1=b_b, op0=ALU.mult, op1=ALU.add), [R_zt, R_ln, R_gb], [R_out])

        def mem_attn(l, tl, mq_b, R_mq, cat_mem, R_cat):
            mqT, pexp, rd, R_t = tl
            bk = nb()
            for j in range(2):
                tr(bk.b[:, j * 128:(j + 1) * 128], mq_b[:, j * 128:(j + 1) * 128], id_b[:], [R_mq, R_const], [bk.res], signal=(j == 1))
            op("act", lambda: nc.scalar.copy(mqT[:], bk.b[:, 0:256].rearrange("p (j t) -> p j t", j=2)), [bk.res], [R_t])
            MA = int(os.environ.get("MA", 99))
            if MA < 2:
                return
            bkh = [nb(), nb()]
            for hp in range(2):
                for hh in range(2):
                    pb = hh * 64
                    for mc in range(2):
                        mm(bkh[hh].f[:, (hp * 2 + mc) * 128:(hp * 2 + mc + 1) * 128], mkT[pb:pb + 64, l, hp, mc * 128:(mc + 1) * 128],
                           mqT[pb:pb + 64, hp, :], True, True, [R_mk, R_t], [bkh[hh].res], signal=(hp == 1 and mc == 1))
            for hh in range(2):
                op("act", lambda: nc.scalar.activation(pexp[:, hh * 4:(hh + 1) * 4, :], bkh[hh].f[:].rearrange("p (a t) -> p a t", a=4),
                                                       AF.Exp, scale=0.125), [bkh[hh].res], [R_t])
            if MA < 3:
                return
            bk = nb()
            for h in range(4):
                for mc in range(2):
                    mm(bk.f[:, h * 128:h * 128 + 65], pexp[:, (h % 2) * 4 + (h // 2) * 2 + mc, :], mv_aug[:, l, mc, h, 0:65], mc == 0, mc == 1,
                       [R_t, R_mk], [bk.res], signal=(h == 3 and mc == 1))
            pv = bk.f[:].rearrange("p (h e) -> p h e", e=128)
            if MA < 4:
                return
            op("dve", lambda: nc.vector.reciprocal(rd[:, 0:4], pv[:, :, 64]), [bk.res], [R_t])
            op("dve", lambda: nc.vector.tensor_tensor(out=cat_mem.rearrange("p (h e) -> p h e", h=4), in0=pv[:, :, 0:64],
                                                      in1=rd[:, 0:4].unsqueeze(2).broadcast_to([128, 4, 64]), op=ALU.mult),
               [bk.res, R_t], [R_cat])

        def out_ln_router(l, i, tl, cat, R_cat, xt, R_xt, wout_b, g_b, b_b, R_w, R_gb, part="ab"):
            catT, R_catT, lnt, R_lnt, x1T_f, x1T_b, R_x1T, rt, R_rt = tl
            if "a" in part:
                out_ln_a(l, i, tl, cat, R_cat, xt, R_xt, wout_b, g_b, b_b, R_w, R_gb)
            if "b" in part:
                out_ln_b(l, i, tl, xt, R_xt)

        def out_ln_a(l, i, tl, cat, R_cat, xt, R_xt, wout_b, g_b, b_b, R_w, R_gb):
            catT, R_catT, lnt, R_lnt, x1T_f, x1T_b, R_x1T, rt, R_rt = tl
            rc = list(R_cat) if isinstance(R_cat, (list, tuple)) else [R_cat]
            if dbg:
                T.dma("sp", "dcat", DCAT[l, i * 128:(i + 1) * 128, :], cat[:], reads=rc)
            bk = nb()
            for kc in range(8):
                tr(bk.b[:, kc * 128:(kc + 1) * 128], cat[:, kc * 128:(kc + 1) * 128], id_b[:], rc + [R_const], [bk.res], signal=(kc == 7))
            op("act", lambda: nc.scalar.copy(catT[:], bk.b[:].rearrange("p (k t) -> p k t", k=8)), [bk.res], [R_catT])
            for half in range(2):
                bk = nb()
                for kc in range(8):
                    mm(bk.f[:], catT[:, kc, :], wout_b[:, kc, half * 512:(half + 1) * 512], kc == 0, kc == 7,
                       [R_catT, R_w], [bk.res], signal=(kc == 7))
                op("dve", lambda: nc.vector.scalar_tensor_tensor(out=xt[:, half * 512:(half + 1) * 512], in0=xt[:, half * 512:(half + 1) * 512],
                                                                 scalar=ALPHA, in1=bk.f[:], op0=ALU.mult, op1=ALU.add),
                   [R_xt, bk.res], [R_xt])
            layer_norm(lnt, xt[:], R_xt, g_b[:], b_b[:], R_gb, R_lnt, xt[:], R_xt)
            T.dma("sp", "sXA%d" % (i % 2), XA[i * 128:(i + 1) * 128, :], xt[:], reads=[R_xt], writes=[R_XA[i]])

        def out_ln_b(l, i, tl, xt, R_xt):
            catT, R_catT, lnt, R_lnt, x1T_f, x1T_b, R_x1T, rt, R_rt = tl
            for half in range(2):
                bk = nb()
                for q in range(4):
                    kc = half * 4 + q
                    tr(bk.f[:, q * 128:(q + 1) * 128], xt[:, kc * 128:(kc + 1) * 128], id_f[:], [R_xt, R_const], [bk.res], signal=(q == 3))
                op("act", lambda: nc.scalar.copy(x1T_f[:, half * 4:(half + 1) * 4, :], bk.f[:].rearrange("p (q t) -> p q t", q=4)), [bk.res], [R_x1T])
                op("dve", lambda: nc.vector.tensor_copy(x1T_b[:, half * 4:(half + 1) * 4, :], bk.f[:].rearrange("p (q t) -> p q t", q=4)), [bk.res], [R_x1T])
            T.dma("sp", "sXT%d" % (i % 2), XT[:, :, i * 128:(i + 1) * 128], x1T_b[:], reads=[R_x1T], writes=[R_XTd[i]])
            bk = nb()
            for kc in range(8):
                mm(bk.f[:, 0:16], x1T_f[:, kc, :], wr_f[:, kc, :], kc == 0, kc == 7, [R_x1T, R_const], [bk.res], signal=(kc == 7))
            aff, sel, m1, eq, m2, sc, gm = rt
            v3 = lambda a: a.rearrange("p (g e) -> p g e", g=4)
            bc3 = lambda a: a.unsqueeze(2).broadcast_to([128, 4, 4])
            op("act", lambda: nc.scalar.activation(aff[:], bk.f[:, 0:16], AF.Sigmoid), [bk.res], [R_rt])
            op("dve", lambda: nc.vector.tensor_tensor(out=sel[:], in0=aff[:], in1=br_b[:], op=ALU.add), [R_rt, R_const], [R_rt])
            op("dve", lambda: nc.vector.tensor_reduce(out=m1[:], in_=v3(sel[:]), axis=AX.X, op=ALU.max), [R_rt], [R_rt])
            op("dve", lambda: nc.vector.tensor_tensor(out=v3(eq[:]), in0=v3(sel[:]), in1=bc3(m1[:]), op=ALU.is_equal), [R_rt], [R_rt])
            op("dve", lambda: nc.vector.scalar_tensor_tensor(out=eq[:], in0=eq[:], scalar=-1e9, in1=sel[:], op0=ALU.mult, op1=ALU.add), [R_rt], [R_rt])
            op("dve", lambda: nc.vector.tensor_reduce(out=m2[:], in_=v3(eq[:]), axis=AX.X, op=ALU.max), [R_rt], [R_rt])
            op("dve", lambda: nc.vector.tensor_tensor(out=sc[:], in0=m1[:], in1=m2[:], op=ALU.add), [R_rt], [R_rt])
            op("dve", lambda: nc.vector.tensor_reduce(out=gm[:, 0:1], in_=sc[:], axis=AX.X, op=ALU.max), [R_rt], [R_rt])
            op("dve", lambda: nc.vector.tensor_scalar(sc[:], sc[:], gm[:, 0:1], None, op0=ALU.is_ge), [R_rt], [R_rt])
            op("dve", lambda: nc.vector.tensor_tensor(out=v3(eq[:]), in0=v3(sel[:]), in1=bc3(m2[:]), op=ALU.is_ge), [R_rt], [R_rt])
            op("dve", lambda: nc.vector.tensor_tensor(out=v3(eq[:]), in0=v3(eq[:]), in1=bc3(sc[:]), op=ALU.mult), [R_rt], [R_rt])
            op("dve", lambda: nc.vector.tensor_tensor(out=eq[:], in0=eq[:], in1=aff[:], op=ALU.mult), [R_rt], [R_rt])
            op("dve", lambda: nc.vector.tensor_reduce(out=gm[:, 1:2], in_=eq[:], axis=AX.X, op=ALU.add), [R_rt], [R_rt])
            op("dve", lambda: nc.vector.reciprocal(gm[:, 2:3], gm[:, 1:2]), [R_rt], [R_rt])
            op("dve", lambda: nc.vector.tensor_scalar(gates_all[:, i, :], eq[:], gm[:, 2:3], None, op0=ALU.mult), [R_rt], [R_gates[i]])

        def alloc_out_tiles(es):
            catT = sbt(es, "catT", [128, 8, 128], BF16)
            st = sbt(es, "ln_st", [128, 12], F32)
            mv = sbt(es, "ln_mv", [128, 2], F32)
            rs = sbt(es, "ln_rs", [128, 2], F32)
            x1T_f = sbt(es, "x1T_f", [128, 8, 128], F32)
            x1T_b = sbt(es, "x1T_b", [128, 8, 128], BF16)
            rt = (sbt(es, "r_aff", [128, 16], F32), sbt(es, "r_sel", [128, 16], F32), sbt(es, "r_m1", [128, 4], F32),
                  sbt(es, "r_eq", [128, 16], F32), sbt(es, "r_m2", [128, 4], F32), sbt(es, "r_sc", [128, 4], F32),
                  sbt(es, "r_gm", [128, 4], F32))
            return (catT, Res("catT"), (st, mv, rs), Res("lnt"), x1T_f, x1T_b, Res("x1T"), rt, Res("rt"))

        def alloc_mem_tiles(es):
            return (sbt(es, "mqT", [128, 2, 128], BF16), sbt(es, "pexp_m", [128, 8, 128], BF16), sbt(es, "rd_m", [128, 4], F32), Res("memt"))

        def load_ln(es, l, gsrc, bsrc, key):
            g_b = sbt(es, "g_b" + key, [128, D], F32)
            b_b = sbt(es, "b_b" + key, [128, D], F32)
            R = Res("ln" + key)
            T.dma("sp", "lng" + key, g_b[:], gsrc[l:l + 1, :].broadcast_to([128, D]), writes=[R])
            T.dma("sp", "lnb" + key, b_b[:], bsrc[l:l + 1, :].broadcast_to([128, D]), writes=[R])
            return g_b, b_b, R

        def phase_A():
            with contextlib.ExitStack() as es:
                use_rot("A0", range(8))
                wina = sbt(es, "wina", [128, 8, A_IN], BF16)
                wout_b = sbt(es, "wout_b", [128, 8, D], BF16)
                wup17 = sbt(es, "wup17", [17, 384], F32)
                gn_b = sbt(es, "gn_b", [128, 192], F32)
                R_w = Res("wA")
                R_wina = [Res("wina%d" % kc) for kc in range(8)]
                R_wout = Res("woutA")
                wv = w_in_a.rearrange("(kc p) n -> p kc n", p=128)
                for kc in range(8):
                    T.dma("pool", "wA_%d" % kc, wina[:, kc, :], wv[:, kc, :], writes=[R_wina[kc]])
                T.dma("pool", "wA2", wout_b[:], w_out[0].rearrange("(kc p) n -> p kc n", p=128), writes=[R_wout])
                T.dma("sp", "wA3", wup17[0:16, :], w_gate_up_a, writes=[R_w])
                T.dma("sp", "wA4", wup17[16:17, :], b_gate_a, writes=[R_w])
                T.dma("sp", "wA5", gn_b[:], gla_norm_g.broadcast_to([128, 192]), writes=[R_w])
                g_b, b_b, R_lnp = load_ln(es, 0, ln_mix_g, ln_mix_b, "A")
                xt = [sbt(es, "xt%d" % p, [128, D], F32) for p in range(4)]
                xT = [sbt(es, "xT%d" % p, [128, 8, 128], BF16) for p in range(2)]
                xb = [sbt(es, "xb%d" % p, [128, D], BF16) for p in range(2)]
                R_xb = [Res("xb%d" % p) for p in range(2)]
                hs = [sbt(es, "hs%d" % p, [128, A_IN], F32) for p in range(2)]
                R_xt = [Res("xt%d" % p) for p in range(4)]
                R_xT = [Res("xT%d" % p) for p in range(2)]
                R_hs = [[Res("hs%d_%d" % (p, g)) for g in range(6)] for p in range(2)]
                gT17 = [sbt(es, "gT17_%d" % p, [17, 128], F32) for p in range(2)]
                R_gT = [Res("gT%d" % p) for p in range(2)]
                for p in range(2):
                    op("dve", lambda: nc.vector.memset(gT17[p][:], 1.0), [], [R_gT[p]])
                l_sb = sbt(es, "l_sb", [128, 384], F32)
                eb = sbt(es, "eb", [128, 384], F32)
                enb = sbt(es, "enb", [128, 384], F32)
                qd = sbt(es, "qd", [128, 384], BF16)
                ki = sbt(es, "ki", [128, 384], BF16)
                v_b = sbt(es, "v_b", [128, 768], BF16)
                dec = sbt(es, "dec", [96, 4], F32)
                qdT = sbt(es, "qdT", [96, 4, 128], BF16)
                kiT = sbt(es, "kiT", [96, 4, 128], BF16)
                attm = sbt(es, "attm", [128, 4, 128], BF16)
                Sst = sbt(es, "Sst", [96, 4, 192], F32)
                S_b = sbt(es, "S_b", [96, 4, 192], BF16)
                kvd = sbt(es, "kvd", [96, 4, 192], F32)
                sq = sbt(es, "sq", [128, 768], F32)
                ss = sbt(es, "ss", [128, 8], F32)
                on = sbt(es, "on", [128, 768], F32)
                sr = sbt(es, "sr", [128, 768], F32)
                cats = [sbt(es, "cat%d" % p, [128, D], BF16) for p in range(2)]
                mq_b = sbt(es, "mq_b", [128, 256], BF16)
                R_l, R_eb, R_qd, R_ki, R_vb, R_dec, R_qdT, R_kiT, R_attm = (Res(n) for n in ["l", "eb", "qd", "ki", "vb", "dec", "qdT", "kiT", "attm"])
                R_S = [Res("S%d" % h) for h in range(4)]
                R_Sb = [Res("Sb%d" % h) for h in range(4)]
                R_kvd = [Res("kvd%d" % h) for h in range(4)]
                R_sq, R_ss, R_on, R_sr, R_mqb = (Res(n) for n in ["sq", "ss", "on", "sr", "mqb"])
                R_cats = [Res("cat%d" % p) for p in range(2)]
                R_catm = [Res("catm%d" % p) for p in range(2)]
                op("dve", lambda: nc.vector.memset(Sst[:], 0.0), [], R_S)
                op("pool", lambda: nc.gpsimd.memset(S_b[:], 0.0), [], R_Sb)
                memt = alloc_mem_tiles(es)
                outt = alloc_out_tiles(es)
                GRP = [(0, 512), (512, 1024), (1024, 1536), (1536, 2048), (2048, 2560), (2560, 2576)]

                def grp_of(lo, hi):
                    return [g for g, (a, b) in enumerate(GRP) if a < hi and b > lo]

                def S1(i):
                    use_rot("A1", [0])
                    p = i % 2
                    p3 = i % 4
                    T.dma("sp", "ldx%d" % p3, xt[p3][:], x[i * 128:(i + 1) * 128, :], writes=[R_xt[p3]])
                    op("pool", lambda: nc.gpsimd.tensor_copy(xb[p][:], xt[p3][:]), [R_xt[p3]], [R_xb[p]])
                    bk = nb()
                    for kc in range(8):
                        tr(bk.b[:, kc * 128:(kc + 1) * 128], xb[p][:, kc * 128:(kc + 1) * 128], id_b[:], [R_xb[p], R_const], [bk.res], signal=(kc == 7))
                    op("act", lambda: nc.scalar.copy(xT[p][:], bk.b[:].rearrange("p (k t) -> p k t", k=8)), [bk.res], [R_xT[p]])
                    for g, (a, b) in enumerate(GRP):
                        bk = nb()
                        for kc in range(8):
                            mm(bk.f[:, 0:b - a], xT[p][:, kc, :], wina[:, kc, a:b], kc == 0, kc == 7, [R_xT[p], R_wina[kc]], [bk.res], signal=(kc == 7))
                        if g % 2 == 0:
                            op("dve", lambda: nc.vector.tensor_copy(hs[p][:, a:b], bk.f[:, 0:b - a]), [bk.res], [R_hs[p][g]])
                        else:
                            op("act", lambda: nc.scalar.copy(hs[p][:, a:b], bk.f[:, 0:b - a]), [bk.res], [R_hs[p][g]])

                S2L = int(os.environ.get("S2_LIMIT", 99))

                def S2(i):
                    use_rot("A2", [1, 2, 3, 4])
                    p = i % 2
                    cat = cats[p]
                    R_cat = R_cats[p]
                    h = hs[p]
                    Rh = lambda lo, hi: [R_hs[p][g] for g in grp_of(lo, hi)]
                    bk = nb()
                    tr(bk.f[0:16, 0:128], h[:, 1536:1552], id_f[:], Rh(1536, 1552) + [R_const], [bk.res])
                    op("act", lambda: nc.scalar.copy(gT17[p][0:16, :], bk.f[0:16, 0:128]), [bk.res], [R_gT[p]])
                    bk = nb()
                    mm(bk.f[:, 0:384], gT17[p][0:17, :], wup17[0:17, :], True, True, [R_gT[p], R_w], [bk.res])
                    op("act", lambda: nc.scalar.activation(l_sb[:], bk.f[:, 0:384], AF.Exp, scale=-1.0), [bk.res], [R_l])
                    op("act", lambda: nc.scalar.activation(l_sb[:], l_sb[:], AF.Ln, bias=ONE, scale=1.0), [R_l, R_const], [R_l])
                    if S2L < 1:
                        return
                    bk = nb()
                    mm(bk.f[:, 0:384], triu[:], l_sb[:], True, True, [R_const, R_l], [bk.res])
                    op("act", lambda: nc.scalar.activation(eb[:], bk.f[:, 0:384], AF.Exp, scale=-1.0 / 16.0), [bk.res], [R_eb])
                    op("act", lambda: nc.scalar.activation(enb[:], bk.f[:, 0:384], AF.Exp, scale=1.0 / 16.0), [bk.res], [R_eb])
                    op("dve", lambda: nc.vector.scalar_tensor_tensor(out=qd[:], in0=h[:, 0:384], scalar=96.0 ** -0.5, in1=eb[:], op0=ALU.mult, op1=ALU.mult),
                       Rh(0, 384) + [R_eb], [R_qd])
                    op("dve", lambda: nc.vector.tensor_tensor(out=ki[:], in0=h[:, 384:768], in1=enb[:], op=ALU.mult), Rh(384, 768) + [R_eb], [R_ki])
                    op("pool", lambda: nc.gpsimd.tensor_copy(v_b[:], h[:, 768:1536]), Rh(768, 1536), [R_vb])
                    if S2L < 2:
                        return
                    bk = nb()
                    for hd in range(4):
                        mm(bk.f[0:96, hd:hd + 1], l_sb[:, hd * 96:(hd + 1) * 96], ones_f[:, 0:1], True, True, [R_l, R_const], [bk.res], signal=(hd == 3))
                    op("act", lambda: nc.scalar.activation(dec[:], bk.f[0:96, 0:4], AF.Exp, scale=-1.0 / 16.0), [bk.res], [R_dec])
                    if S2L < 3:
                        return
                    bk = nb()
                    for hd in range(4):
                        tr(bk.b[0:96, hd * 128:(hd + 1) * 128], qd[:, hd * 96:(hd + 1) * 96], id_b[:], [R_qd, R_const], [bk.res], signal=(hd == 3))
                    op("act", lambda: nc.scalar.copy(qdT[:], bk.b[0:96, 0:512].rearrange("p (h t) -> p h t", h=4)), [bk.res], [R_qdT])
                    bk = nb()
                    for hd in range(4):
                        tr(bk.b[0:96, hd * 128:(hd + 1) * 128], ki[:, hd * 96:(hd + 1) * 96], id_b[:], [R_ki, R_const], [bk.res], signal=(hd == 3))
                    op("dve", lambda: nc.vector.tensor_copy(kiT[:], bk.b[0:96, 0:512].rearrange("p (h t) -> p h t", h=4)), [bk.res], [R_kiT])
                    if S2L < 4:
                        return
                    bk = nb()
                    for hd in range(4):
                        mm(bk.f[:, hd * 128:(hd + 1) * 128], kiT[:, hd, :], qdT[:, hd, :], True, True, [R_kiT, R_qdT], [bk.res], signal=(hd == 3))
                    op("dve", lambda: nc.vector.tensor_tensor(out=attm[:], in0=bk.f[:].rearrange("p (h t) -> p h t", h=4),
                                                              in1=triu[:].unsqueeze(1).broadcast_to([128, 4, 128]), op=ALU.mult),
                       [bk.res, R_const], [R_attm])
                    if S2L < 5:
                        return
                    ob = [nb(), nb()]
                    for hd in range(4):
                        bo = ob[hd // 2]
                        oo = bo.f[:, (hd % 2) * 192:(hd % 2 + 1) * 192]
                        mm(oo, attm[:, hd, :], v_b[:, hd * 192:(hd + 1) * 192], True, False, [R_attm, R_vb], [bo.res], signal=False)
                        mm(oo, qdT[:, hd, :], S_b[:, hd, :], False, True, [R_qdT, R_Sb[hd]], [bo.res], signal=True)
                    if S2L < 6:
                        return
                    kb = [nb(), nb()]
                    for hd in range(4):
                        bkv = kb[hd // 2]
                        kk = bkv.f[0:96, (hd % 2) * 192:(hd % 2 + 1) * 192]
                        mm(kk, ki[:, hd * 96:(hd + 1) * 96], v_b[:, hd * 192:(hd + 1) * 192], True, True, [R_ki, R_vb], [bkv.res])
                        op("dve", lambda: nc.vector.tensor_scalar(kvd[:, hd, :], kk, dec[:, hd:hd + 1], None, op0=ALU.mult), [bkv.res, R_dec], [R_kvd[hd]])
                        op("dve", lambda: nc.vector.scalar_tensor_tensor(out=Sst[:, hd, :], in0=Sst[:, hd, :], scalar=dec[:, hd:hd + 1], in1=kvd[:, hd, :],
                                                                         op0=ALU.mult, op1=ALU.add), [R_S[hd], R_dec, R_kvd[hd]], [R_S[hd]])
                        op("pool", lambda: nc.gpsimd.tensor_copy(S_b[:, hd, :], Sst[:, hd, :]), [R_S[hd]], [R_Sb[hd]])
                    if S2L < 7:
                        return
                    for j in range(2):
                        op("act", lambda: nc.scalar.activation(sq[:, j * 384:(j + 1) * 384], ob[j].f[:, 0:384], AF.Square), [ob[j].res], [R_sq])
                    S7 = int(os.environ.get("S7", 99))
                    if S7 < 2:
                        return
                    op("dve", lambda: nc.vector.tensor_reduce(out=ss[:, 0:4], in_=sq[:].rearrange("p (h e) -> p h e", h=4), axis=AX.X, op=ALU.add), [R_sq], [R_ss])
                    if S7 < 3:
                        return
                    op("act", lambda: nc.scalar.activation(ss[:, 0:4], ss[:, 0:4], AF.Sqrt, bias=EPS_RMS, scale=1.0 / 192.0), [R_ss, R_const], [R_ss])
                    if S7 < 4:
                        return
                    op("dve", lambda: nc.vector.reciprocal(ss[:, 4:8], ss[:, 0:4]), [R_ss], [R_ss])
                    if S7 < 5:
                        return
                    for hd in range(4):
                        op("dve", lambda: nc.vector.scalar_tensor_tensor(out=on[:, hd * 192:(hd + 1) * 192], in0=ob[hd // 2].f[:, (hd % 2) * 192:(hd % 2 + 1) * 192],
                                                                         scalar=ss[:, 4 + hd:5 + hd], in1=gn_b[:], op0=ALU.mult, op1=ALU.mult),
                           [ob[hd // 2].res, R_ss, R_w], [R_on])
                    if S7 < 6:
                        return
                    op("act", lambda: nc.scalar.activation(sr[:], h[:, 1552:2320], AF.Silu), Rh(1552, 2320), [R_sr])
                    if S7 < 7:
                        return
                    VV = os.environ.get("VV", "0")
                    if VV == "1":
                        op("dve", lambda: nc.vector.tensor_tensor(out=sq[:], in0=on[:], in1=sr[:], op=ALU.mult), [R_on, R_sr], [R_sq])
                    elif VV == "2":
                        op("dve", lambda: nc.vector.tensor_copy(cat[:, 0:768], on[:]), [R_on], [R_cat])
                    elif VV == "3":
                        op("dve", lambda: nc.vector.tensor_copy(cat[:, 0:768], sr[:]), [R_sr], [R_cat])
                    else:
                        op("dve", lambda: nc.vector.tensor_tensor(out=cat[:, 0:768], in0=on[:], in1=sr[:], op=ALU.mult), [R_on, R_sr], [R_cat])

                def S2m(i):
                    p = i % 2
                    op("pool", lambda: nc.gpsimd.tensor_copy(mq_b[:], hs[p][:, 2320:2576]), [R_hs[p][g] for g in grp_of(2320, 2576)], [R_mqb])
                    mem_attn(0, memt, mq_b, R_mqb, cats[p][:, 768:1024], R_catm[p])

                def S3a(i):
                    use_rot("A3a", [5])
                    out_ln_router(0, i, outt, cats[i % 2], [R_cats[i % 2], R_catm[i % 2]], xt[i % 4], R_xt[i % 4], wout_b, g_b, b_b, R_wout, R_lnp, part="a")

                def SMB(step):
                    use_rot("A3b", [6, 7])
                    if 0 <= step - 1 < NTILE:
                        S2m(step - 1)
                    if 0 <= step - 3 < NTILE:
                        out_ln_router(0, step - 3, outt, None, None, xt[(step - 3) % 4], R_xt[(step - 3) % 4], wout_b, g_b, b_b, R_wout, R_lnp, part="b")

                for step in range(NTILE + 3):
                    chains = []
                    if step < NTILE:
                        chains.append(lambda: S1(step))
                    if 0 <= step - 1 < NTILE:
                        chains.append(lambda: S2(step - 1))
                    if 0 <= step - 2 < NTILE:
                        chains.append(lambda: S3a(step - 2))
                    if 0 <= step - 1 < NTILE or 0 <= step - 3 < NTILE:
                        chains.append(lambda: SMB(step))
                    COOP.run(chains)

        def phase_M(l, src, R_src, dst, R_dst, gsrc, bsrc):
            with contextlib.ExitStack() as es:
                set_rot(range(8))
                xTs = sbt(es, "xTs", [128, 8, ST_TOK], BF16)
                acc = sbt(es, "acc", [128, ST_TILES, D], F32)
                NTB = ST_TOK // 512
                R_xTs = [Res("xTs%d" % tb) for tb in range(NTB)]
                R_acc = [Res("acc%d" % t) for t in range(ST_TILES)]
                wg = [sbt(es, "wg%d" % p, [128, 8, 512], BF16) for p in range(2)]
                wu = [sbt(es, "wu%d" % p, [128, 8, 512], BF16) for p in range(2)]
                wd = [sbt(es, "wd%d" % p, [128, 4, D], BF16) for p in range(2)]
                R_wg = [Res("wg%d" % p) for p in range(2)]
                R_wu = [Res("wu%d" % p) for p in range(2)]
                R_wd = [Res("wd%d" % p) for p in range(2)]
                HT = [sbt(es, "HT%d" % p, [128, 4, 512], BF16) for p in range(2)]
                R_HT = [Res("HT%d" % p) for p in range(2)]
                sg = [sbt(es, "sg%d" % p, [128, 512], F32) for p in range(2)]
                R_sg = [Res("sg%d" % p) for p in range(2)]
                g_b, b_b, R_lnp = load_ln(es, l, gsrc, bsrc, "M%d" % l)
                NXR = 4
                xr = [sbt(es, "xr%d" % p, [128, D], F32) for p in range(NXR)]
                R_xr = [Res("xr%d" % p) for p in range(NXR)]
                lnt = (sbt(es, "m_st", [128, 12], F32), sbt(es, "m_mv", [128, 2], F32), sbt(es, "m_rs", [128, 2], F32))
                R_lnt = Res("mlnt")
                NST = S // ST_TOK
                steps = [(st, e) for st in range(NST) for e in range(NEXP)]

                def load_w(k):
                    st, e = steps[k]
                    p = k % 2
                    T.dma("pool", "lwg%d" % p, wg[p][:], w_exp_gate[l, e].rearrange("(kc p) f -> p kc f", p=128), writes=[R_wg[p]])
                    T.dma("pool", "lwu%d" % p, wu[p][:], w_exp_up[l, e].rearrange("(kc p) f -> p kc f", p=128), writes=[R_wu[p]])
                    T.dma("pool", "lwd%d" % p, wd[p][:], w_exp_down[l, e].rearrange("(fc p) d -> p fc d", p=128), writes=[R_wd[p]])

                def load_x(st):
                    for tb in range(NTB):
                        t0 = st * ST_TOK + tb * 512
                        T.dma("sp", "ldxTs%d" % tb, xTs[:, :, tb * 512:(tb + 1) * 512], XT[:, :, t0:t0 + 512],
                              reads=R_XTd[t0 // 128:t0 // 128 + 4], writes=[R_xTs[tb]])

                blocks = [(k, tb) for k in range(len(steps)) for tb in range(NTB)]

                def GU(bi):
                    k, tb = blocks[bi]
                    st, e = steps[k]
                    p = k % 2
                    hp = bi % 2
                    last = (e == NEXP - 1)
                    for fc in range(4):
                        bg = nb()
                        for kc in range(8):
                            mm(bg.f[:], wg[p][:, kc, fc * 128:(fc + 1) * 128], xTs[:, kc, tb * 512:(tb + 1) * 512], kc == 0, kc == 7,
                               [R_wg[p], R_xTs[tb]], [bg.res], signal=(kc == 7))
                        bu = nb()
                        for kc in range(8):
                            mm(bu.f[:], wu[p][:, kc, fc * 128:(fc + 1) * 128], xTs[:, kc, tb * 512:(tb + 1) * 512], kc == 0, kc == 7,
                               [R_wu[p], R_xTs[tb]], [bu.res], signal=(kc == 7))
                        sp_ = fc % 2
                        op("act", lambda: nc.scalar.activation(sg[sp_][:], bg.f[:], AF.Silu), [bg.res], [R_sg[sp_]])
                        op("dve", lambda: nc.vector.tensor_tensor(out=HT[hp][:, fc, :], in0=bu.f[:], in1=sg[sp_][:], op=ALU.mult),
                           [bu.res, R_sg[sp_]], [R_HT[hp]])
                    if last and st + 1 < NST:
                        t0 = (st + 1) * ST_TOK + tb * 512
                        T.dma("sp", "ldxTs%d" % tb, xTs[:, :, tb * 512:(tb + 1) * 512], XT[:, :, t0:t0 + 512],
                              reads=R_XTd[t0 // 128:t0 // 128 + 4], writes=[R_xTs[tb]])

                def DOWN(bi):
                    k, tb = blocks[bi]
                    st, e = steps[k]
                    p = k % 2
                    hp = bi % 2
                    last = (e == NEXP - 1)
                    if last:
                        for tt in range(4):
                            gi = st * ST_TILES + tb * 4 + tt
                            T.dma("sp", "ldxr%d" % (gi % NXR), xr[gi % NXR][:], src[gi * 128:(gi + 1) * 128, :], reads=[R_src[gi]], writes=[R_xr[gi % NXR]])
                    for tt in range(4):
                        ti = tb * 4 + tt
                        gi = st * ST_TILES + ti
                        for dh in range(2):
                            bo = nb()
                            for fc in range(4):
                                mm(bo.f[:], HT[hp][:, fc, tt * 128:(tt + 1) * 128], wd[p][:, fc, dh * 512:(dh + 1) * 512], fc == 0, fc == 3,
                                   [R_HT[hp], R_wd[p]], [bo.res], signal=(fc == 3))
                            a_ap = acc[:, ti, dh * 512:(dh + 1) * 512]
                            if e == 0:
                                op("dve", lambda: nc.vector.tensor_scalar(a_ap, bo.f[:], gates_all[:, gi, e:e + 1], None, op0=ALU.mult),
                                   [bo.res, R_gates[gi]], [R_acc[ti]])
                            else:
                                op("dve", lambda: nc.vector.scalar_tensor_tensor(out=a_ap, in0=bo.f[:], scalar=gates_all[:, gi, e:e + 1], in1=a_ap,
                                                                                 op0=ALU.mult, op1=ALU.add),
                                   [bo.res, R_gates[gi], R_acc[ti]], [R_acc[ti]])
                        if last:
                            q = gi % NXR
                            op("dve", lambda: nc.vector.scalar_tensor_tensor(out=xr[q][:], in0=xr[q][:], scalar=ALPHA, in1=acc[:, ti, :], op0=ALU.mult, op1=ALU.add),
                               [R_xr[q], R_acc[ti]], [R_xr[q]])
                            layer_norm(lnt, xr[q][:], R_xr[q], g_b[:], b_b[:], R_lnp, R_lnt, xr[q][:], R_xr[q])
                            T.dma("sp", "stM%d" % q, dst[gi * 128:(gi + 1) * 128, :], xr[q][:], reads=[R_xr[q]], writes=[R_dst[gi]])
                    if tb == NTB - 1 and k + 2 < len(steps):
                        load_w(k + 2)

                load_w(0)
                load_w(1)
                load_x(0)
                GU(0)
                for bi in range(len(blocks)):
                    if bi + 1 < len(blocks):
                        GU(bi + 1)
                    DOWN(bi)

        if stop_after != "P0":
            phase_A()
            _barrier(T)
        if dbg:
            T.dma("sp", "dg", DG[:, 0, :, :], gates_all[:], reads=R_gates)
        R_out = [Res("out%d" % i) for i in range(NTILE)]

        def phase_B(v_all, R_vall, kbias, R_kb):
            with contextlib.ExitStack() as es:
                use_rot("B0", range(8))
                wkv = sbt(es, "wkv", [128, 8, KV_IN], BF16)
                winb = sbt(es, "winb", [128, 8, B_IN], BF16)
                R_wkv = [Res("wkv%d" % kc) for kc in range(8)]
                R_winb = [Res("winb%d" % kc) for kc in range(8)]
                wkv_v = w_kv_shared.rearrange("(kc p) n -> p kc n", p=128)
                winb_v = w_in_b.rearrange("(kc p) n -> p kc n", p=128)
                for kc in range(8):
                    T.dma("pool", "wB_%d" % kc, wkv[:, kc, :], wkv_v[:, kc, :], writes=[R_wkv[kc]])
                    T.dma("pool", "wBb_%d" % kc, winb[:, kc, :], winb_v[:, kc, :], writes=[R_winb[kc]])
                gk_b = sbt(es, "gk_b", [128, 64], F32)
                gq_b = sbt(es, "gq_b", [128, 64], F32)
                bf_b = sbt(es, "bf_b", [128, 12], F32)
                R_pb = Res("parB")
                T.dma("sp", "pB0", gk_b[:], k_norm_g.broadcast_to([128, 64]), writes=[R_pb])
                T.dma("sp", "pB1", gq_b[:], q_norm_g.broadcast_to([128, 64]), writes=[R_pb])
                T.dma("sp", "pB2", bf_b[:], b_forget.broadcast_to([128, 12]), writes=[R_pb])
                xt = [sbt(es, "bxt%d" % p, [128, D], F32) for p in range(2)]
                xT = [sbt(es, "bxT%d" % p, [128, 8, 128], BF16) for p in range(2)]
                xb = [sbt(es, "bxb%d" % p, [128, D], BF16) for p in range(2)]
                R_xb = [Res("bxb%d" % p) for p in range(2)]
                hk = [sbt(es, "hk%d" % p, [128, KV_IN], F32) for p in range(2)]
                hb = [sbt(es, "hb%d" % p, [128, B_IN], F32) for p in range(2)]
                R_xt = [Res("bxt%d" % p) for p in range(2)]
                R_xT = [Res("bxT%d" % p) for p in range(2)]
                GK = [(0, 512), (512, 1024), (1024, 1536), (1536, KV_IN)]
                GB = [(0, 512), (512, 1024), (1024, 1536), (1536, B_IN)]
                R_hk = [[Res("hk%d_%d" % (p, g)) for g in range(4)] for p in range(2)]
                R_hb = [[Res("hb%d_%d" % (p, g)) for g in range(4)] for p in range(2)]
                sqt = sbt(es, "b_sq", [128, 768], F32)
                nrm = sbt(es, "b_nrm", [128, 768], F32)
                ssn = sbt(es, "b_ss", [128, 24], F32)
                k_augs = [sbt(es, "k_aug%d" % p, [128, 12, 72], BF16) for p in range(2)]
                q_augs = [sbt(es, "q_aug%d" % p, [128, 12, 72], BF16) for p in range(2)]
                lf = sbt(es, "b_lf", [128, 12], F32)
                carry = sbt(es, "b_carry", [128, 12], F32)
                t1 = sbt(es, "b_t1", [128, 12], F32)
                ogq = sbt(es, "b_ogq", [128, D], BF16)
                qT_t = sbt(es, "qT_t", [66, 12, 128], BF16)
                kT_t = sbt(es, "kT_t", [66, 12, 128], BF16)
                R_sq, R_nrm, R_ssn, R_lf, R_carry, R_t1, R_ogq, R_qTt, R_kTt = (
                    Res(n) for n in ["bsq", "bnrm", "bss", "lf", "carry", "t1", "ogq", "qTt", "kTt"])
                R_kas = [Res("ka%d" % p) for p in range(2)]
                R_qas = [Res("qa%d" % p) for p in range(2)]
                for p in range(2):
                    op("dve", lambda: nc.vector.memset(k_augs[p][:], 1.0), [], [R_kas[p]])
                    op("dve", lambda: nc.vector.memset(q_augs[p][:], 0.0), [], [R_qas[p]])
                op("dve", lambda: nc.vector.memset(carry[:], 0.0), [], [R_carry])
                for c4 in range(4):
                    op("pool", lambda: nc.gpsimd.memset(v_all[:, c4 * 8:(c4 + 1) * 8, :, :], 1.0), [], [R_vall[c4 * 8 + j] for j in range(8)])

                def grp_of(G, lo, hi):
                    return [g for g, (a, b) in enumerate(G) if a < hi and b > lo]

                def S1(i):
                    use_rot("B1", [0, 1, 2])
                    p = i % 2
                    T.dma("sp", "bldx%d" % p, xt[p][:], XB[i * 128:(i + 1) * 128, :], reads=[R_XB[i]], writes=[R_xt[p]])
                    op("pool", lambda: nc.gpsimd.tensor_copy(xb[p][:], xt[p][:]), [R_xt[p]], [R_xb[p]])
                    bk = nb()
                    for kc in range(8):
                        tr(bk.b[:, kc * 128:(kc + 1) * 128], xb[p][:, kc * 128:(kc + 1) * 128], id_b[:], [R_xb[p], R_const], [bk.res], signal=(kc == 7))
                    op("act", lambda: nc.scalar.copy(xT[p][:], bk.b[:].rearrange("p (k t) -> p k t", k=8)), [bk.res], [R_xT[p]])
                    n = 0
                    for (G, w, Rw, dst, Rd) in ((GK, wkv, R_wkv, hk[p], R_hk[p]), (GB, winb, R_winb, hb[p], R_hb[p])):
                        for g, (a, b) in enumerate(G):
                            bk = nb()
                            for kc in range(8):
                                mm(bk.f[:, 0:b - a], xT[p][:, kc, :], w[:, kc, a:b], kc == 0, kc == 7, [R_xT[p], Rw[kc]], [bk.res], signal=(kc == 7))
                            if n % 2 == 0:
                                op("dve", lambda: nc.vector.tensor_copy(dst[:, a:b], bk.f[:, 0:b - a]), [bk.res], [Rd[g]])
                            else:
                                op("act", lambda: nc.scalar.copy(dst[:, a:b], bk.f[:, 0:b - a]), [bk.res], [Rd[g]])
                            n += 1

                def rmsn(src, Rsrc, gb, aug, R_aug, off):
                    v3 = lambda a: a.rearrange("p (h e) -> p h e", h=12)
                    op("act", lambda: nc.scalar.activation(sqt[:], src, AF.Square), Rsrc, [R_sq])
                    op("dve", lambda: nc.vector.tensor_reduce(out=ssn[:, off:off + 12], in_=v3(sqt[:]), axis=AX.X, op=ALU.add), [R_sq], [R_ssn])
                    op("act", lambda: nc.scalar.activation(ssn[:, off:off + 12], ssn[:, off:off + 12], AF.Sqrt, bias=EPS_RMS, scale=1.0 / 64.0), [R_ssn, R_const], [R_ssn])
                    op("dve", lambda: nc.vector.reciprocal(ssn[:, off:off + 12], ssn[:, off:off + 12]), [R_ssn], [R_ssn])
                    op("dve", lambda: nc.vector.tensor_tensor(out=v3(nrm[:]), in0=v3(src), in1=ssn[:, off:off + 12].unsqueeze(2).broadcast_to([128, 12, 64]), op=ALU.mult),
                       Rsrc + [R_ssn], [R_nrm])
                    op("dve", lambda: nc.vector.tensor_tensor(out=aug[:, :, 0:64], in0=v3(nrm[:]), in1=gb[:].unsqueeze(1).broadcast_to([128, 12, 64]), op=ALU.mult),
                       [R_nrm, R_pb], [R_aug])

                def S2(i):
                    use_rot("B2", [3])
                    p = i % 2
                    k_aug, q_aug, R_ka, R_qa = k_augs[p], q_augs[p], R_kas[p], R_qas[p]
                    Rk = lambda lo, hi: [R_hk[p][g] for g in grp_of(GK, lo, hi)]
                    Rb = lambda lo, hi: [R_hb[p][g] for g in grp_of(GB, lo, hi)]
                    rmsn(hk[p][:, 0:768], Rk(0, 768), gk_b, k_aug, R_ka, 0)
                    rmsn(hb[p][:, 0:768], Rb(0, 768), gq_b, q_aug, R_qa, 12)
                    op("dve", lambda: nc.vector.tensor_tensor(out=lf[:], in0=hk[p][:, 1536:1548], in1=bf_b[:], op=ALU.add), Rk(1536, 1548) + [R_pb], [R_lf])
                    op("act", lambda: nc.scalar.activation(lf[:], lf[:], AF.Exp, scale=-1.0), [R_lf], [R_lf])
                    op("act", lambda: nc.scalar.activation(lf[:], lf[:], AF.Ln, bias=ONE, scale=1.0), [R_lf, R_const], [R_lf])
                    bk = nb()
                    mm(bk.f[:, 0:12], triu[:], lf[:], True, True, [R_const, R_lf], [bk.res], signal=False)
                    mm(bk.f[:, 16:28], ones_f[:], lf[:], True, True, [R_const, R_lf], [bk.res], signal=True)
                    op("dve", lambda: nc.vector.tensor_tensor(out=kbias[:, i, :], in0=bk.f[:, 0:12], in1=carry[:], op=ALU.add), [bk.res, R_carry], [R_kb[i]])
                    op("dve", lambda: nc.vector.tensor_tensor(out=carry[:], in0=bk.f[:, 16:28], in1=carry[:], op=ALU.add), [bk.res, R_carry], [R_carry])
                    op("dve", lambda: nc.vector.tensor_scalar(t1[:], kbias[:, i, :], -8.0, None, op0=ALU.mult), [R_kb[i]], [R_t1])
                    op("dve", lambda: nc.vector.tensor_copy(q_aug[:, :, 64], t1[:]), [R_t1], [R_qa])
                    op("dve", lambda: nc.vector.tensor_tensor(out=q_aug[:, :, 65], in0=t1[:], in1=q_aug[:, :, 64], op=ALU.subtract), [R_t1, R_qa], [R_qa])
                    op("pool", lambda: nc.gpsimd.tensor_copy(v_all[:, i, :, 0:64], hk[p][:, 768:1536].rearrange("p (h e) -> p h e", h=12)), Rk(768, 1536), [R_vall[i]])
                    op("act", lambda: nc.scalar.activation(ogq[:, 0:768], hb[p][:, 768:1536], AF.Sigmoid), Rb(768, 1536), [R_ogq])
                    op("pool", lambda: nc.gpsimd.tensor_copy(ogq[:, 768:1024], hb[p][:, 1536:1792]), Rb(1536, 1792), [R_ogq])
                    T.dma("sp", "sOGQ", OGQ[i * 128:(i + 1) * 128, :], ogq[:], reads=[R_ogq], writes=[R_OGQ[i]])

                def S2b(i):
                    use_rot("B2b", [4, 5, 6, 7])
                    p = i % 2
                    k_aug, q_aug, R_ka, R_qa = k_augs[p], q_augs[p], R_kas[p], R_qas[p]
                    for (aug, R_aug, tt, R_tt, dram, R_d, key, eng) in ((q_aug, R_qa, qT_t, R_qTt, QT, R_QTd, "sQT", "act"), (k_aug, R_ka, kT_t, R_kTt, KT, R_KTd, "sKT", "dve")):
                        for (h0, h1) in ((0, 8), (8, 12)):
                            bk = nb()
                            for h in range(h0, h1):
                                tr(bk.b[0:66, (h - h0) * 128:(h - h0 + 1) * 128], aug[:, h, 0:66], id_b[:], [R_aug, R_const], [bk.res], signal=(h == h1 - 1))
                            src = bk.b[0:66, 0:(h1 - h0) * 128].rearrange("p (h t) -> p h t", h=h1 - h0)
                            if eng == "act":
                                op("act", lambda: nc.scalar.copy(tt[:, h0:h1, :], src), [bk.res], [R_tt])
                            else:
                                op("dve", lambda: nc.vector.tensor_copy(tt[:, h0:h1, :], src), [bk.res], [R_tt])
                        T.dma("sp", key, dram.rearrange("h p t -> p h t")[:, :, i * 128:(i + 1) * 128], tt[:], reads=[R_tt], writes=[R_d[i]])

                for step in range(NTILE + 2):
                    chains = []
                    if step < NTILE:
                        chains.append(lambda: S1(step))
                    if 0 <= step - 1 < NTILE:
                        chains.append(lambda: S2(step - 1))
                    if 0 <= step - 2 < NTILE:
                        chains.append(lambda: S2b(step - 2))
                    COOP.run(chains)

        def phase_C(v_all, R_vall, kbias, R_kb, o_all, R_oall):
            with contextlib.ExitStack() as es:
                kTh = [sbt(es, "kTh%d" % p, [66, S], BF16) for p in range(2)]
                qTh = [sbt(es, "qTh%d" % p, [66, S], BF16) for p in range(2)]
                R_kTh = [Res("kTh%d" % p) for p in range(2)]
                R_qTh = [Res("qTh%d" % p) for p in range(2)]
                NPB = 6
                pex = [sbt(es, "pex%d" % j, [128, 512], BF16) for j in range(NPB)]
                R_pex = [Res("pex%d" % j) for j in range(NPB)]
                rdc = sbt(es, "rdc", [128, 4], F32)
                R_rdc = Res("rdc")
                set_rot([4, 5, 6, 7])
                acc_b = [banks[j] for j in range(4)]

                def load_h(h):
                    p = h % 2
                    T.dma("sp", "ldk%d" % p, kTh[p][:], KT[h], reads=R_KTd, writes=[R_kTh[p]])
                    T.dma("sp", "ldq%d" % p, qTh[p][:], QT[h], reads=R_QTd, writes=[R_qTh[p]])

                load_h(0)
                units = [(h, Q, c) for h in range(12) for Q in range(8) for c in range(4 * Q + 4)]
                sb_of = {}
                DEPTH = 3

                def qk(u):
                    h, Q, c = units[u]
                    p = h % 2
                    if Q == 0 and c == 0 and h + 1 < 12:
                        load_h(h + 1)
                    lo = 128 * max(0, c - 4 * Q)
                    bs = nb()
                    mm(bs.f[:, lo:512], kTh[p][0:66, c * 128:(c + 1) * 128], qTh[p][0:66, Q * 512 + lo:(Q + 1) * 512], True, True,
                       [R_kTh[p], R_qTh[p]], [bs.res])
                    sb_of[u] = bs

                def rest(u):
                    h, Q, c = units[u]
                    j0 = max(0, c - 4 * Q)
                    lo = 128 * j0
                    bs = sb_of.pop(u)
                    pj = u % NPB
                    op("act", lambda: nc.scalar.activation(pex[pj][:, lo:512], bs.f[:, lo:512], AF.Exp, bias=kbias[:, c, h:h + 1], scale=0.125),
                       [bs.res, R_kb[c]], [R_pex[pj]])
                    if c >= 4 * Q:
                        op("dve", lambda: nc.vector.tensor_tensor(out=pex[pj][:, lo:lo + 128], in0=pex[pj][:, lo:lo + 128], in1=triu_b[:], op=ALU.mult),
                           [R_pex[pj], R_const], [R_pex[pj]])
                    for j in range(j0, 4):
                        mm(acc_b[j].f[:, 0:65], pex[pj][:, j * 128:(j + 1) * 128], v_all[:, c, h, 0:65], c == 0, c == 4 * Q + j,
                           [R_pex[pj], R_vall[c]], [acc_b[j].res], signal=(j == 3))
                    if c == 4 * Q + 3:
                        for j in range(4):
                            ti = 4 * Q + j
                            op("dve", lambda: nc.vector.reciprocal(rdc[:, j:j + 1], acc_b[j].f[:, 64:65]), [acc_b[j].res], [R_rdc])
                            op("dve", lambda: nc.vector.tensor_scalar(o_all[:, ti, h * 64:(h + 1) * 64], acc_b[j].f[:, 0:64], rdc[:, j:j + 1], None, op0=ALU.mult),
                               [acc_b[j].res, R_rdc], [R_oall[ti]])

                for u in range(min(DEPTH, len(units))):
                    qk(u)
                for u in range(len(units)):
                    if u + DEPTH < len(units):
                        qk(u + DEPTH)
                    rest(u)

        def phase_D(o_all, R_oall):
            with contextlib.ExitStack() as es:
                use_rot("D0", range(8))
                wout_b = sbt(es, "wout_d", [128, 8, D], BF16)
                R_wout = Res("woutD")
                T.dma("pool", "wD", wout_b[:], w_out[1].rearrange("(kc p) n -> p kc n", p=128), writes=[R_wout])
                g_b, b_b, R_lnp = load_ln(es, 1, ln_mix_g, ln_mix_b, "D")
                xt = [sbt(es, "dxt%d" % p, [128, D], F32) for p in range(4)]
                og = [sbt(es, "dog%d" % p, [128, D], BF16) for p in range(2)]
                R_xt = [Res("dxt%d" % p) for p in range(4)]
                R_og = [Res("dog%d" % p) for p in range(2)]
                cats = [sbt(es, "dcat%d" % p, [128, D], BF16) for p in range(2)]
                R_cats = [Res("dcat%d" % p) for p in range(2)]
                memt = alloc_mem_tiles(es)
                outt = alloc_out_tiles(es)

                def S1(i):
                    p = i % 2
                    T.dma("sp", "dldx%d" % (i % 4), xt[i % 4][:], XB[i * 128:(i + 1) * 128, :], reads=[R_XB[i]], writes=[R_xt[i % 4]])
                    T.dma("sp", "dldo%d" % p, og[p][:], OGQ[i * 128:(i + 1) * 128, :], reads=[R_OGQ[i]], writes=[R_og[p]])

                def S2(i):
                    use_rot("D2", [0, 1, 2])
                    p = i % 2
                    cat, R_cat = cats[p], R_cats[p]
                    op("dve", lambda: nc.vector.tensor_tensor(out=cat[:, 0:768], in0=o_all[:, i, :], in1=og[p][:, 0:768], op=ALU.mult), [R_oall[i], R_og[p]], [R_cat])
                    mem_attn(1, memt, og[p][:, 768:1024], R_og[p], cat[:, 768:1024], R_cat)
                    if dbg:
                        T.dma("sp", "dcat", DCAT[1, i * 128:(i + 1) * 128, :], cat[:], reads=[R_cat])

                def S3a(i):
                    use_rot("D3a", [3, 4, 5])
                    out_ln_router(1, i, outt, cats[i % 2], R_cats[i % 2], xt[i % 4], R_xt[i % 4], wout_b, g_b, b_b, R_wout, R_lnp, part="a")

                def S3b(i):
                    use_rot("D3b", [6, 7])
                    out_ln_router(1, i, outt, cats[i % 2], R_cats[i % 2], xt[i % 4], R_xt[i % 4], wout_b, g_b, b_b, R_wout, R_lnp, part="b")

                for step in range(NTILE + 3):
                    chains = []
                    if step < NTILE:
                        chains.append(lambda: S1(step))
                    if 0 <= step - 1 < NTILE:
                        chains.append(lambda: S2(step - 1))
                    if 0 <= step - 2 < NTILE:
                        chains.append(lambda: S3a(step - 2))
                    if 0 <= step - 3 < NTILE:
                        chains.append(lambda: S3b(step - 3))
                    COOP.run(chains)

        if stop_after in ("A", "P0"):
            pass
        else:
            phase_M(0, XA, R_XA, XB, R_XB, ln_ffn_g, ln_ffn_b)
            _barrier(T)
            if stop_after != "M0":
                with contextlib.ExitStack() as esL:
                    v_all = sbt(esL, "v_all", [128, NTILE, 12, 72], BF16)
                    kbias = sbt(esL, "kbias", [128, NTILE, 12], F32)
                    R_vall = [Res("vall%d" % i) for i in range(NTILE)]
                    R_kb = [Res("kb%d" % i) for i in range(NTILE)]
                    phase_B(v_all, R_vall, kbias, R_kb)
                    _barrier(T)
                    if stop_after != "B":
                        with contextlib.ExitStack() as esO:
                            o_all = sbt(esO, "o_all", [128, NTILE, 768], BF16)
                            R_oall = [Res("oall%d" % i) for i in range(NTILE)]
                            phase_C(v_all, R_vall, kbias, R_kb, o_all, R_oall)
                            _barrier(T)
                            phase_D(o_all, R_oall)
                            _barrier(T)
                            if dbg:
                                T.dma("sp", "dg", DG[:, 1, :, :], gates_all[:], reads=R_gates)
                _barrier(T)
                if stop_after not in ("B", "D"):
                    phase_M(1, XA, R_XA, out, R_out, ln_ffn_g, ln_ffn_b)

        _barrier(T)
    print("build: ninst=%d nwait=%d" % (T.ninst, T.nwait))
    T.close()
    return nc


_CONSTS = None


def _consts():
    global _CONSTS
    if _CONSTS is None:
        ident = np.eye(128, dtype=np.float32)
        triu = np.triu(np.ones((128, 128), dtype=np.float32))
        _CONSTS = {"c_ident": ident, "c_triu": triu}
    return _CONSTS


def make_in_map(inputs, b):
    f = lambda a: np.ascontiguousarray(np.asarray(a, dtype=np.float32))
    m = {
        "x": f(inputs["x"][b]), "mem": f(inputs["mem"][b]),
        "w_in_a": f(inputs["w_in_a"][0]), "w_gate_up_a": f(inputs["w_gate_up_a"][0]),
        "b_gate_a": f(inputs["b_gate_a"]).reshape(1, 384), "gla_norm_g": f(inputs["gla_norm_g"]).reshape(1, 192),
        "w_in_b": f(inputs["w_in_b"][0]), "q_norm_g": f(inputs["q_norm_g"]).reshape(1, 64),
        "w_kv_shared": f(inputs["w_kv_shared"]), "b_forget": f(inputs["b_forget"]).reshape(1, 12),
        "k_norm_g": f(inputs["k_norm_g"]).reshape(1, 64), "w_mem_kv": f(inputs["w_mem_kv"]),
        "w_out": f(inputs["w_out"]), "ln_mix_g": f(inputs["ln_mix_g"]), "ln_mix_b": f(inputs["ln_mix_b"]),
        "ln_ffn_g": f(inputs["ln_ffn_g"]), "ln_ffn_b": f(inputs["ln_ffn_b"]),
        "w_router": f(inputs["w_router"]), "b_router": f(inputs["b_router"]).reshape(1, 16),
        "w_exp_gate": f(inputs["w_exp_gate"]), "w_exp_up": f(inputs["w_exp_up"]), "w_exp_down": f(inputs["w_exp_down"]),
    }
    m.update(_consts())
    return m


def kernel(**inputs):
    nc = build()
    in_maps = [make_in_map(inputs, b) for b in range(N_CORES)]
    res = run_bass_kernel_spmd(nc, in_maps, core_ids=list(range(N_CORES)))
    return np.stack([np.asarray(r["out"], dtype=np.float32) for r in res.results], axis=0)
```

```python
import contextlib
import os
import threading
import numpy as np
import concourse.bass as bass
import concourse.mybir as mybir
from concourse.bass_utils import run_bass_kernel_spmd

F32 = mybir.dt.float32
BF16 = mybir.dt.bfloat16
AF = mybir.ActivationFunctionType
ALU = mybir.AluOpType
AX = mybir.AxisListType

S = 4096
D = 1024
NTILE = S // 128
N_CORES = 8
ALPHA = (2.0 * 2) ** 0.25
LN_EPS = 1e-5
RMS_EPS = 1e-6
A_IN = 2576
B_IN = 1792
KV_IN = 1548
NEXP = 16
ST_TOK = 2048
ST_TILES = ST_TOK // 128


class Coop:
    def __init__(self):
        self.tl = threading.local()

    def switch(self):
        st = getattr(self.tl, "st", None)
        if st is None:
            return
        i, sems, done, n = st
        for k in range(1, n):
            j = (i + k) % n
            if not done[j]:
                sems[j].release()
                sems[i].acquire()
                return

    def run(self, fns):
        n = len(fns)
        if n == 1:
            fns[0]()
            return
        sems = [threading.Semaphore(0) for _ in range(n)]
        done = [False] * n
        main = threading.Semaphore(0)
        errs = []

        def worker(i):
            sems[i].acquire()
            self.tl.st = (i, sems, done, n)
            try:
                fns[i]()
            except BaseException as e:
                errs.append(e)
            done[i] = True
            for k in range(1, n):
                j = (i + k) % n
                if not done[j]:
                    sems[j].release()
                    return
            main.release()

        ths = [threading.Thread(target=worker, args=(i,)) for i in range(n)]
        for t in ths:
            t.start()
        sems[0].release()
        main.acquire()
        for t in ths:
            t.join()
        if errs:
            raise errs[0]


COOP = Coop()


class Res:
    __slots__ = ("name", "w", "r", "excl")

    def __init__(self, name, excl=False):
        self.name = name
        self.w = None
        self.r = []
        self.excl = excl


class Trk:
    def __init__(self, nc):
        self.nc = nc
        self.eng = {"pe": nc.tensor, "act": nc.scalar, "dve": nc.vector, "pool": nc.gpsimd, "sp": nc.sync}
        self.sem = {}
        self.cnt = {}
        self.known = {e: {} for e in self.eng}
        self._stack = []
        for e in self.eng:
            cm = nc.semaphore("s_" + e)
            self.sem[e] = cm.__enter__()
            self._stack.append(cm)
            self.cnt[e] = 0
        self.dsem = {}
        self.dcnt = {}
        self.nwait = 0
        self.ninst = 0

    def close(self):
        for cm in reversed(self._stack):
            cm.__exit__(None, None, None)

    def _dma_sem(self, key):
        if key not in self.dsem:
            cm = self.nc.semaphore("d_" + key)
            self.dsem[key] = cm.__enter__()
            self._stack.append(cm)
            self.dcnt[key] = 0
        return self.dsem[key]

    def _need(self, e, toks):
        best = {}
        for t in toks:
            if t is None:
                continue
            name, sem, val, src = t
            if src == "pe" and e == "pe":
                continue
            if self.known[e].get(name, 0) >= val:
                continue
            if best.get(name, (None, 0))[1] < val:
                best[name] = (sem, val)
        for name, (sem, val) in best.items():
            self.eng[e].wait_ge(sem, val)
            self.known[e][name] = val
            self.nwait += 1

    @staticmethod
    def _compact(lst):
        best = {}
        for t in lst:
            if t[0] not in best or best[t[0]][2] < t[2]:
                best[t[0]] = t
        return list(best.values())

    def op(self, e, fn, reads=(), writes=(), signal=True):
        toks = []
        for r in reads:
            toks.append(r.w)
            if r.excl and e != "pe":
                toks.extend(r.r)
        for w in writes:
            toks.append(w.w)
            toks.extend(w.r)
        self._need(e, toks)
        ins = fn()
        self.ninst += 1
        if signal:
            self.cnt[e] += 1
            ins.then_inc(self.sem[e], 1)
            tok = ("E" + e, self.sem[e], self.cnt[e], e)
        else:
            tok = ("E" + e, self.sem[e], self.cnt[e] + 1, e)
        for r in reads:
            if r in writes:
                continue
            r.r.append(tok)
            if len(r.r) > 16:
                r.r = self._compact(r.r)
        for w in writes:
            w.w = tok
            w.r = []
        COOP.switch()
        return ins

    def dma(self, q, key, out, in_, reads=(), writes=()):
        toks = []
        for r in reads:
            toks.append(r.w)
        for w in writes:
            toks.append(w.w)
            toks.extend(w.r)
        self._need(q, toks)
        sem = self._dma_sem(key)
        self.dcnt[key] += 16
        self.eng[q].dma_start(out=out, in_=in_).then_inc(sem, 16)
        self.ninst += 1
        tok = ("D" + key, sem, self.dcnt[key], "dma")
        for r in reads:
            r.r.append(tok)
            if len(r.r) > 16:
                r.r = self._compact(r.r)
        for w in writes:
            w.w = tok
            w.r = []
        COOP.switch()
        return tok


def _barrier(T):
    for e in T.eng:
        for f in T.eng:
            if f != e and T.cnt[f] > 0 and T.known[e].get("E" + f, 0) < T.cnt[f]:
                T.eng[e].wait_ge(T.sem[f], T.cnt[f])
                T.known[e]["E" + f] = T.cnt[f]
                T.nwait += 1
        for key in list(T.dsem.keys()):
            if T.known[e].get("D" + key, 0) < T.dcnt[key]:
                T.eng[e].wait_ge(T.dsem[key], T.dcnt[key])
                T.known[e]["D" + key] = T.dcnt[key]
                T.nwait += 1


class Bank:
    def __init__(self, f, res):
        self.f = f
        self.b = f.bitcast(BF16)
        self.res = res


def build(dbg=False, stop_after=None):
    nc = bass.Bass("TRN2", target_bir_lowering=False)

    def din(name, shape):
        return nc.dram_tensor(name, list(shape), F32, kind="ExternalInput").ap()

    x = din("x", [S, D])
    mem = din("mem", [256, D])
    w_in_a = din("w_in_a", [D, A_IN])
    w_gate_up_a = din("w_gate_up_a", [16, 384])
    b_gate_a = din("b_gate_a", [1, 384])
    gla_norm_g = din("gla_norm_g", [1, 192])
    w_in_b = din("w_in_b", [D, B_IN])
    q_norm_g = din("q_norm_g", [1, 64])
    w_kv_shared = din("w_kv_shared", [D, KV_IN])
    b_forget = din("b_forget", [1, 12])
    k_norm_g = din("k_norm_g", [1, 64])
    w_mem_kv = din("w_mem_kv", [2, D, 512])
    w_out = din("w_out", [2, D, D])
    ln_mix_g = din("ln_mix_g", [2, D])
    ln_mix_b = din("ln_mix_b", [2, D])
    ln_ffn_g = din("ln_ffn_g", [2, D])
    ln_ffn_b = din("ln_ffn_b", [2, D])
    w_router = din("w_router", [D, 16])
    b_router = din("b_router", [1, 16])
    w_exp_gate = din("w_exp_gate", [2, NEXP, D, 512])
    w_exp_up = din("w_exp_up", [2, NEXP, D, 512])
    w_exp_down = din("w_exp_down", [2, NEXP, 512, D])
    c_ident = din("c_ident", [128, 128])
    c_triu = din("c_triu", [128, 128])

    out = nc.dram_tensor("out", [S, D], F32, kind="ExternalOutput").ap()
    kscr = "ExternalOutput" if dbg else "Internal"
    XA = nc.dram_tensor("XA", [S, D], F32, kind=kscr).ap()
    XB = nc.dram_tensor("XB", [S, D], F32, kind=kscr).ap()
    XT = nc.dram_tensor("XT", [128, 8, S], BF16, kind="Internal").ap()
    QT = nc.dram_tensor("QT", [12, 66, S], BF16, kind="Internal").ap()
    KT = nc.dram_tensor("KT", [12, 66, S], BF16, kind="Internal").ap()
    OGQ = nc.dram_tensor("OGQ", [S, D], BF16, kind="Internal").ap()
    if dbg:
        DG = nc.dram_tensor("DG", [128, 2, NTILE, 16], F32, kind="ExternalOutput").ap()
        DCAT = nc.dram_tensor("DCAT", [2, S, D], BF16, kind="ExternalOutput").ap()

    T = Trk(nc)
    out_toks = []
    R_XA = [Res('XA%d' % i) for i in range(NTILE)]
    R_XB = [Res('XB%d' % i) for i in range(NTILE)]
    R_XTd = [Res('XTd%d' % i) for i in range(NTILE)]
    R_OGQ = [Res('OGQ%d' % i) for i in range(NTILE)]
    R_QTd = [Res('QTd%d' % i) for i in range(NTILE)]
    R_KTd = [Res('KTd%d' % i) for i in range(NTILE)]

    with contextlib.ExitStack() as ges:
        uid = [0]

        def sbt(es, name, shape, dt):
            uid[0] += 1
            return es.enter_context(nc.sbuf_tensor("%s_%d" % (name, uid[0]), list(shape), dt))

        banks = []
        for i in range(8):
            pt = ges.enter_context(nc.psum_tensor("pb%d" % i, [128, 512], F32))
            banks.append(Bank(pt[:], Res("pb%d" % i, excl=True)))
        rots = {}
        rtl = threading.local()

        def use_rot(name, lst):
            if name not in rots:
                rots[name] = {"lst": list(lst), "i": 0}
            rtl.cur = rots[name]

        def set_rot(lst):
            use_rot("main%s" % (tuple(lst),), lst)

        def nb():
            rot = rtl.cur
            b = banks[rot["lst"][rot["i"] % len(rot["lst"])]]
            rot["i"] += 1
            return b

        def op(e, fn, reads=(), writes=(), signal=True):
            return T.op(e, fn, reads, writes, signal)

        def mm(o, lhsT, rhs, start, stop, reads, writes, signal=True):
            return T.op("pe", lambda: nc.tensor.matmul(o, lhsT, rhs, start=start, stop=stop), reads, writes, signal)

        def tr(o, in_, ident, reads, writes, signal=True):
            return T.op("pe", lambda: nc.tensor.transpose(o, in_, ident), reads, writes, signal)

        id_f = sbt(ges, "id_f", [128, 128], F32)
        id_b = sbt(ges, "id_b", [128, 128], BF16)
        triu = sbt(ges, "triu", [128, 128], F32)
        triu_b = sbt(ges, "triu_b", [128, 128], BF16)
        ones_f = sbt(ges, "ones_f", [128, 128], F32)
        cst = sbt(ges, "cst", [128, 4], F32)
        gates_all = sbt(ges, "gates_all", [128, NTILE, 16], F32)
        mkT = sbt(ges, "mkT", [128, 2, 2, 256], BF16)
        mv_aug = sbt(ges, "mv_aug", [128, 2, 2, 4, 72], BF16)
        wr_f = sbt(ges, "wr_f", [128, 8, 16], F32)
        br_b = sbt(ges, "br_b", [128, 16], F32)
        R_const = Res("const")
        R_gates = [Res("gates%d" % i) for i in range(NTILE)]
        R_mk = Res("mk")
        T.dma("sp", "c0", id_f[:], c_ident, writes=[R_const])
        T.dma("sp", "c1", triu[:], c_triu, writes=[R_const])
        T.dma("pool", "c2", id_b[:], c_ident, writes=[R_const])
        T.dma("pool", "c3", triu_b[:], c_triu, writes=[R_const])
        T.dma("sp", "c4", wr_f[:], w_router.rearrange("(kc p) e -> p kc e", p=128), writes=[R_const])
        T.dma("sp", "c5", br_b[:], b_router.broadcast_to([128, 16]), writes=[R_const])
        op("dve", lambda: nc.vector.memset(ones_f[:], 1.0), writes=[R_const])
        op("dve", lambda: nc.vector.memset(cst[:, 0:1], LN_EPS), writes=[R_const])
        op("dve", lambda: nc.vector.memset(cst[:, 1:2], RMS_EPS), writes=[R_const])
        op("dve", lambda: nc.vector.memset(cst[:, 2:3], 1.0), writes=[R_const])
        op("dve", lambda: nc.vector.memset(mv_aug[:], 1.0), writes=[R_mk])
        EPS_LN = cst[:, 0:1]
        EPS_RMS = cst[:, 1:2]
        ONE = cst[:, 2:3]

        with contextlib.ExitStack() as es:
            set_rot(range(8))
            mem_f = sbt(es, "mem_f", [128, 2, D], F32)
            memT = sbt(es, "memT", [128, 8, 256], BF16)
            wm = sbt(es, "wm", [128, 8, 512], BF16)
            R_memf, R_memT, R_wm = Res("memf"), Res("memT"), Res("wm")
            T.dma("sp", "p0a", mem_f[:], mem.rearrange("(mc p) d -> p mc d", p=128), writes=[R_memf])
            for mc in range(2):
                for half in range(2):
                    bk = nb()
                    for q in range(4):
                        kc = half * 4 + q
                        tr(bk.f[:, q * 128:(q + 1) * 128], mem_f[:, mc, kc * 128:(kc + 1) * 128], id_f[:],
                           [R_memf, R_const], [bk.res], signal=(q == 3))
                    op("act", lambda: nc.scalar.copy(memT[:, half * 4:(half + 1) * 4, mc * 128:(mc + 1) * 128],
                                                     bk.f[:].rearrange("p (q t) -> p q t", q=4)),
                       [bk.res], [R_memT])
            for l in range(2):
                T.dma("pool", "p0b", wm[:], w_mem_kv[l].rearrange("(kc p) n -> p kc n", p=128), writes=[R_wm])
                for j in range(2):
                    bk = nb()
                    for kc in range(8):
                        mm(bk.f[:, 0:256], wm[:, kc, j * 128:(j + 1) * 128], memT[:, kc, :], kc == 0, kc == 7,
                           [R_wm, R_memT], [bk.res], signal=(kc == 7))
                    op("act", lambda: nc.scalar.copy(mkT[:, l, j, :], bk.f[:, 0:256]), [bk.res], [R_mk])
                for mc in range(2):
                    bk = nb()
                    for kc in range(8):
                        mm(bk.f[:, 0:256], memT[:, kc, mc * 128:(mc + 1) * 128], wm[:, kc, 256:512], kc == 0, kc == 7,
                           [R_wm, R_memT], [bk.res], signal=(kc == 7))
                    op("dve", lambda: nc.vector.tensor_copy(mv_aug[:, l, mc, :, 0:64],
                                                            bk.f[:, 0:256].rearrange("p (h e) -> p h e", h=4)),
                       [bk.res], [R_mk])

        _barrier(T)
        def layer_norm(es_tiles, zt, R_zt, g_b, b_b, R_gb, R_ln, outap, R_out):
            st, mv, rs = es_tiles
            op("dve", lambda: nc.vector.bn_stats(st[:, 0:6], zt[:, 0:512]), [R_zt], [R_ln])
            op("dve", lambda: nc.vector.bn_stats(st[:, 6:12], zt[:, 512:1024]), [R_zt], [R_ln])
            op("dve", lambda: nc.vector.bn_aggr(mv[:, 0:2], st[:, 0:12]), [R_ln], [R_ln])
            op("act", lambda: nc.scalar.activation(rs[:, 0:1], mv[:, 1:2], AF.Sqrt, bias=EPS_LN, scale=1.0), [R_ln, R_const], [R_ln])
            op("dve", lambda: nc.vector.reciprocal(rs[:, 1:2], rs[:, 0:1]), [R_ln], [R_ln])
            op("dve", lambda: nc.vector.scalar_tensor_tensor(out=zt, in0=zt, scalar=mv[:, 0:1], in1=g_b, op0=ALU.subtract, op1=ALU.mult), [R_zt, R_ln, R_gb], [R_zt])
            op("dve", lambda: nc.vector.scalar_tensor_tensor(out=outap, in0=zt, scalar=rs[:, 1:2], in1=b_b, op0=ALU.mult, op1=ALU.add), [R_zt, R_ln, R_gb], [R_out])

        def mem_attn(l, tl, mq_b, R_mq, cat_mem, R_cat):
            mqT, pexp, rd, R_t = tl
            bk = nb()
            for j in range(2):
                tr(bk.b[:, j * 128:(j + 1) * 128], mq_b[:, j * 128:(j + 1) * 128], id_b[:], [R_mq, R_const], [bk.res], signal=(j == 1))
            op("act", lambda: nc.scalar.copy(mqT[:], bk.b[:, 0:256].rearrange("p (j t) -> p j t", j=2)), [bk.res], [R_t])
            MA = int(os.environ.get("MA", 99))
            if MA < 2:
                return
            bkh = [nb(), nb()]
            for hp in range(2):
                for hh in range(2):
                    pb = hh * 64
                    for mc in range(2):
                        mm(bkh[hh].f[:, (hp * 2 + mc) * 128:(hp * 2 + mc + 1) * 128], mkT[pb:pb + 64, l, hp, mc * 128:(mc + 1) * 128],
                           mqT[pb:pb + 64, hp, :], True, True, [R_mk, R_t], [bkh[hh].res], signal=(hp == 1 and mc == 1))
            for hh in range(2):
                op("act", lambda: nc.scalar.activation(pexp[:, hh * 4:(hh + 1) * 4, :], bkh[hh].f[:].rearrange("p (a t) -> p a t", a=4),
                                                       AF.Exp, scale=0.125), [bkh[hh].res], [R_t])
            if MA < 3:
                return
            bk = nb()
            for h in range(4):
                for mc in range(2):
                    mm(bk.f[:, h * 128:h * 128 + 65], pexp[:, (h % 2) * 4 + (h // 2) * 2 + mc, :], mv_aug[:, l, mc, h, 0:65], mc == 0, mc == 1,
                       [R_t, R_mk], [bk.res], signal=(h == 3 and mc == 1))
            pv = bk.f[:].rearrange("p (h e) -> p h e", e=128)
            if MA < 4:
                return
            op("dve", lambda: nc.vector.reciprocal(rd[:, 0:4], pv[:, :, 64]), [bk.res], [R_t])
            op("dve", lambda: nc.vector.tensor_tensor(out=cat_mem.rearrange("p (h e) -> p h e", h=4), in0=pv[:, :, 0:64],
                                                      in1=rd[:, 0:4].unsqueeze(2).broadcast_to([128, 4, 64]), op=ALU.mult),
               [bk.res, R_t], [R_cat])

        def out_ln_router(l, i, tl, cat, R_cat, xt, R_xt, wout_b, g_b, b_b, R_w, R_gb, part="ab"):
            catT, R_catT, lnt, R_lnt, x1T_f, x1T_b, R_x1T, rt, R_rt = tl
            if "a" in part:
                out_ln_a(l, i, tl, cat, R_cat, xt, R_xt, wout_b, g_b, b_b, R_w, R_gb)
            if "b" in part:
                out_ln_b(l, i, tl, xt, R_xt)

        def out_ln_a(l, i, tl, cat, R_cat, xt, R_xt, wout_b, g_b, b_b, R_w, R_gb):
            catT, R_catT, lnt, R_lnt, x1T_f, x1T_b, R_x1T, rt, R_rt = tl
            rc = list(R_cat) if isinstance(R_cat, (list, tuple)) else [R_cat]
            if dbg:
                T.dma("sp", "dcat", DCAT[l, i * 128:(i + 1) * 128, :], cat[:], reads=rc)
            bk = nb()
            for kc in range(8):
                tr(bk.b[:, kc * 128:(kc + 1) * 128], cat[:, kc * 128:(kc + 1) * 128], id_b[:], rc + [R_const], [bk.res], signal=(kc == 7))
            op("act", lambda: nc.scalar.copy(catT[:], bk.b[:].rearrange("p (k t) -> p k t", k=8)), [bk.res], [R_catT])
            for half in range(2):
                bk = nb()
                for kc in range(8):
                    mm(bk.f[:], catT[:, kc, :], wout_b[:, kc, half * 512:(half + 1) * 512], kc == 0, kc == 7,
                       [R_catT, R_w], [bk.res], signal=(kc == 7))
                op("dve", lambda: nc.vector.scalar_tensor_tensor(out=xt[:, half * 512:(half + 1) * 512], in0=xt[:, half * 512:(half + 1) * 512],
                                                                 scalar=ALPHA, in1=bk.f[:], op0=ALU.mult, op1=ALU.add),
                   [R_xt, bk.res], [R_xt])
            layer_norm(lnt, xt[:], R_xt, g_b[:], b_b[:], R_gb, R_lnt, xt[:], R_xt)
            T.dma("sp", "sXA%d" % (i % 2), XA[i * 128:(i + 1) * 128, :], xt[:], reads=[R_xt], writes=[R_XA[i]])

        def out_ln_b(l, i, tl, xt, R_xt):
            catT, R_catT, lnt, R_lnt, x1T_f, x1T_b, R_x1T, rt, R_rt = tl
            for half in range(2):
                bk = nb()
                for q in range(4):
                    kc = half * 4 + q
                    tr(bk.f[:, q * 128:(q + 1) * 128], xt[:, kc * 128:(kc + 1) * 128], id_f[:], [R_xt, R_const], [bk.res], signal=(q == 3))
                op("act", lambda: nc.scalar.copy(x1T_f[:, half * 4:(half + 1) * 4, :], bk.f[:].rearrange("p (q t) -> p q t", q=4)), [bk.res], [R_x1T])
                op("dve", lambda: nc.vector.tensor_copy(x1T_b[:, half * 4:(half + 1) * 4, :], bk.f[:].rearrange("p (q t) -> p q t", q=4)), [bk.res], [R_x1T])
            T.dma("sp", "sXT%d" % (i % 2), XT[:, :, i * 128:(i + 1) * 128], x1T_b[:], reads=[R_x1T], writes=[R_XTd[i]])
            bk = nb()
            for kc in range(8):
                mm(bk.f[:, 0:16], x1T_f[:, kc, :], wr_f[:, kc, :], kc == 0, kc == 7, [R_x1T, R_const], [bk.res], signal=(kc == 7))
            aff, sel, m1, eq, m2, sc, gm = rt
            v3 = lambda a: a.rearrange("p (g e) -> p g e", g=4)
            bc3 = lambda a: a.unsqueeze(2).broadcast_to([128, 4, 4])
            op("act", lambda: nc.scalar.activation(aff[:], bk.f[:, 0:16], AF.Sigmoid), [bk.res], [R_rt])
            op("dve", lambda: nc.vector.tensor_tensor(out=sel[:], in0=aff[:], in1=br_b[:], op=ALU.add), [R_rt, R_const], [R_rt])
            op("dve", lambda: nc.vector.tensor_reduce(out=m1[:], in_=v3(sel[:]), axis=AX.X, op=ALU.max), [R_rt], [R_rt])
            op("dve", lambda: nc.vector.tensor_tensor(out=v3(eq[:]), in0=v3(sel[:]), in1=bc3(m1[:]), op=ALU.is_equal), [R_rt], [R_rt])
            op("dve", lambda: nc.vector.scalar_tensor_tensor(out=eq[:], in0=eq[:], scalar=-1e9, in1=sel[:], op0=ALU.mult, op1=ALU.add), [R_rt], [R_rt])
            op("dve", lambda: nc.vector.tensor_reduce(out=m2[:], in_=v3(eq[:]), axis=AX.X, op=ALU.max), [R_rt], [R_rt])
            op("dve", lambda: nc.vector.tensor_tensor(out=sc[:], in0=m1[:], in1=m2[:], op=ALU.add), [R_rt], [R_rt])
            op("dve", lambda: nc.vector.tensor_reduce(out=gm[:, 0:1], in_=sc[:], axis=AX.X, op=ALU.max), [R_rt], [R_rt])
            op("dve", lambda: nc.vector.tensor_scalar(sc[:], sc[:], gm[:, 0:1], None, op0=ALU.is_ge), [R_rt], [R_rt])
            op("dve", lambda: nc.vector.tensor_tensor(out=v3(eq[:]), in0=v3(sel[:]), in1=bc3(m2[:]), op=ALU.is_ge), [R_rt], [R_rt])
            op("dve", lambda: nc.vector.tensor_tensor(out=v3(eq[:]), in0=v3(eq[:]), in1=bc3(sc[:]), op=ALU.mult), [R_rt], [R_rt])
            op("dve", lambda: nc.vector.tensor_tensor(out=eq[:], in0=eq[:], in1=aff[:], op=ALU.mult), [R_rt], [R_rt])
            op("dve", lambda: nc.vector.tensor_reduce(out=gm[:, 1:2], in_=eq[:], axis=AX.X, op=ALU.add), [R_rt], [R_rt])
            op("dve", lambda: nc.vector.reciprocal(gm[:, 2:3], gm[:, 1:2]), [R_rt], [R_rt])
            op("dve", lambda: nc.vector.tensor_scalar(gates_all[:, i, :], eq[:], gm[:, 2:3], None, op0=ALU.mult), [R_rt], [R_gates[i]])

        def alloc_out_tiles(es):
            catT = sbt(es, "catT", [128, 8, 128], BF16)
            st = sbt(es, "ln_st", [128, 12], F32)
            mv = sbt(es, "ln_mv", [128, 2], F32)
            rs = sbt(es, "ln_rs", [128, 2], F32)
            x1T_f = sbt(es, "x1T_f", [128, 8, 128], F32)
            x1T_b = sbt(es, "x1T_b", [128, 8, 128], BF16)
            rt = (sbt(es, "r_aff", [128, 16], F32), sbt(es, "r_sel", [128, 16], F32), sbt(es, "r_m1", [128, 4], F32),
                  sbt(es, "r_eq", [128, 16], F32), sbt(es, "r_m2", [128, 4], F32), sbt(es, "r_sc", [128, 4], F32),
                  sbt(es, "r_gm", [128, 4], F32))
            return (catT, Res("catT"), (st, mv, rs), Res("lnt"), x1T_f, x1T_b, Res("x1T"), rt, Res("rt"))

        def alloc_mem_tiles(es):
            return (sbt(es, "mqT", [128, 2, 128], BF16), sbt(es, "pexp_m", [128, 8, 128], BF16), sbt(es, "rd_m", [128, 4], F32), Res("memt"))

        def load_ln(es, l, gsrc, bsrc, key):
            g_b = sbt(es, "g_b" + key, [128, D], F32)
            b_b = sbt(es, "b_b" + key, [128, D], F32)
            R = Res("ln" + key)
            T.dma("sp", "lng" + key, g_b[:], gsrc[l:l + 1, :].broadcast_to([128, D]), writes=[R])
            T.dma("sp", "lnb" + key, b_b[:], bsrc[l:l + 1, :].broadcast_to([128, D]), writes=[R])
            return g_b, b_b, R

        def phase_A():
            with contextlib.ExitStack() as es:
                use_rot("A0", range(8))
                wina = sbt(es, "wina", [128, 8, A_IN], BF16)
                wout_b = sbt(es, "wout_b", [128, 8, D], BF16)
                wup17 = sbt(es, "wup17", [17, 384], F32)
                gn_b = sbt(es, "gn_b", [128, 192], F32)
                R_w = Res("wA")
                R_wina = [Res("wina%d" % kc) for kc in range(8)]
                R_wout = Res("woutA")
                wv = w_in_a.rearrange("(kc p) n -> p kc n", p=128)
                for kc in range(8):
                    T.dma("pool", "wA_%d" % kc, wina[:, kc, :], wv[:, kc, :], writes=[R_wina[kc]])
                T.dma("pool", "wA2", wout_b[:], w_out[0].rearrange("(kc p) n -> p kc n", p=128), writes=[R_wout])
                T.dma("sp", "wA3", wup17[0:16, :], w_gate_up_a, writes=[R_w])
                T.dma("sp", "wA4", wup17[16:17, :], b_gate_a, writes=[R_w])
                T.dma("sp", "wA5", gn_b[:], gla_norm_g.broadcast_to([128, 192]), writes=[R_w])
                g_b, b_b, R_lnp = load_ln(es, 0, ln_mix_g, ln_mix_b, "A")
                xt = [sbt(es, "xt%d" % p, [128, D], F32) for p in range(4)]
                xT = [sbt(es, "xT%d" % p, [128, 8, 128], BF16) for p in range(2)]
                xb = [sbt(es, "xb%d" % p, [128, D], BF16) for p in range(2)]
                R_xb = [Res("xb%d" % p) for p in range(2)]
                hs = [sbt(es, "hs%d" % p, [128, A_IN], F32) for p in range(2)]
                R_xt = [Res("xt%d" % p) for p in range(4)]
                R_xT = [Res("xT%d" % p) for p in range(2)]
                R_hs = [[Res("hs%d_%d" % (p, g)) for g in range(6)] for p in range(2)]
                gT17 = [sbt(es, "gT17_%d" % p, [17, 128], F32) for p in range(2)]
                R_gT = [Res("gT%d" % p) for p in range(2)]
                for p in range(2):
                    op("dve", lambda: nc.vector.memset(gT17[p][:], 1.0), [], [R_gT[p]])
                l_sb = sbt(es, "l_sb", [128, 384], F32)
                eb = sbt(es, "eb", [128, 384], F32)
                enb = sbt(es, "enb", [128, 384], F32)
                qd = sbt(es, "qd", [128, 384], BF16)
                ki = sbt(es, "ki", [128, 384], BF16)
                v_b = sbt(es, "v_b", [128, 768], BF16)
                dec = sbt(es, "dec", [96, 4], F32)
                qdT = sbt(es, "qdT", [96, 4, 128], BF16)
                kiT = sbt(es, "kiT", [96, 4, 128], BF16)
                attm = sbt(es, "attm", [128, 4, 128], BF16)
                Sst = sbt(es, "Sst", [96, 4, 192], F32)
                S_b = sbt(es, "S_b", [96, 4, 192], BF16)
                kvd = sbt(es, "kvd", [96, 4, 192], F32)
                sq = sbt(es, "sq", [128, 768], F32)
                ss = sbt(es, "ss", [128, 8], F32)
                on = sbt(es, "on", [128, 768], F32)
                sr = sbt(es, "sr", [128, 768], F32)
                cats = [sbt(es, "cat%d" % p, [128, D], BF16) for p in range(2)]
                mq_b = sbt(es, "mq_b", [128, 256], BF16)
                R_l, R_eb, R_qd, R_ki, R_vb, R_dec, R_qdT, R_kiT, R_attm = (Res(n) for n in ["l", "eb", "qd", "ki", "vb", "dec", "qdT", "kiT", "attm"])
                R_S = [Res("S%d" % h) for h in range(4)]
                R_Sb = [Res("Sb%d" % h) for h in range(4)]
                R_kvd = [Res("kvd%d" % h) for h in range(4)]
                R_sq, R_ss, R_on, R_sr, R_mqb = (Res(n) for n in ["sq", "ss", "on", "sr", "mqb"])
                R_cats = [Res("cat%d" % p) for p in range(2)]
                R_catm = [Res("catm%d" % p) for p in range(2)]
                op("dve", lambda: nc.vector.memset(Sst[:], 0.0), [], R_S)
                op("pool", lambda: nc.gpsimd.memset(S_b[:], 0.0), [], R_Sb)
                memt = alloc_mem_tiles(es)
                outt = alloc_out_tiles(es)
                GRP = [(0, 512), (512, 1024), (1024, 1536), (1536, 2048), (2048, 2560), (2560, 2576)]

                def grp_of(lo, hi):
                    return [g for g, (a, b) in enumerate(GRP) if a < hi and b > lo]

                def S1(i):
                    use_rot("A1", [0])
                    p = i % 2
                    p3 = i % 4
                    T.dma("sp", "ldx%d" % p3, xt[p3][:], x[i * 128:(i + 1) * 128, :], writes=[R_xt[p3]])
                    op("pool", lambda: nc.gpsimd.tensor_copy(xb[p][:], xt[p3][:]), [R_xt[p3]], [R_xb[p]])
                    bk = nb()
                    for kc in range(8):
                        tr(bk.b[:, kc * 128:(kc + 1) * 128], xb[p][:, kc * 128:(kc + 1) * 128], id_b[:], [R_xb[p], R_const], [bk.res], signal=(kc == 7))
                    op("act", lambda: nc.scalar.copy(xT[p][:], bk.b[:].rearrange("p (k t) -> p k t", k=8)), [bk.res], [R_xT[p]])
                    for g, (a, b) in enumerate(GRP):
                        bk = nb()
                        for kc in range(8):
                            mm(bk.f[:, 0:b - a], xT[p][:, kc, :], wina[:, kc, a:b], kc == 0, kc == 7, [R_xT[p], R_wina[kc]], [bk.res], signal=(kc == 7))
                        if g % 2 == 0:
                            op("dve", lambda: nc.vector.tensor_copy(hs[p][:, a:b], bk.f[:, 0:b - a]), [bk.res], [R_hs[p][g]])
                        else:
                            op("act", lambda: nc.scalar.copy(hs[p][:, a:b], bk.f[:, 0:b - a]), [bk.res], [R_hs[p][g]])

                S2L = int(os.environ.get("S2_LIMIT", 99))

                def S2(i):
                    use_rot("A2", [1, 2, 3, 4])
                    p = i % 2
                    cat = cats[p]
                    R_cat = R_cats[p]
                    h = hs[p]
                    Rh = lambda lo, hi: [R_hs[p][g] for g in grp_of(lo, hi)]
                    bk = nb()
                    tr(bk.f[0:16, 0:128], h[:, 1536:1552], id_f[:], Rh(1536, 1552) + [R_const], [bk.res])
                    op("act", lambda: nc.scalar.copy(gT17[p][0:16, :], bk.f[0:16, 0:128]), [bk.res], [R_gT[p]])
                    bk = nb()
                    mm(bk.f[:, 0:384], gT17[p][0:17, :], wup17[0:17, :], True, True, [R_gT[p], R_w], [bk.res])
                    op("act", lambda: nc.scalar.activation(l_sb[:], bk.f[:, 0:384], AF.Exp, scale=-1.0), [bk.res], [R_l])
                    op("act", lambda: nc.scalar.activation(l_sb[:], l_sb[:], AF.Ln, bias=ONE, scale=1.0), [R_l, R_const], [R_l])
                    if S2L < 1:
                        return
                    bk = nb()
                    mm(bk.f[:, 0:384], triu[:], l_sb[:], True, True, [R_const, R_l], [bk.res])
                    op("act", lambda: nc.scalar.activation(eb[:], bk.f[:, 0:384], AF.Exp, scale=-1.0 / 16.0), [bk.res], [R_eb])
                    op("act", lambda: nc.scalar.activation(enb[:], bk.f[:, 0:384], AF.Exp, scale=1.0 / 16.0), [bk.res], [R_eb])
                    op("dve", lambda: nc.vector.scalar_tensor_tensor(out=qd[:], in0=h[:, 0:384], scalar=96.0 ** -0.5, in1=eb[:], op0=ALU.mult, op1=ALU.mult),
                       Rh(0, 384) + [R_eb], [R_qd])
                    op("dve", lambda: nc.vector.tensor_tensor(out=ki[:], in0=h[:, 384:768], in1=enb[:], op=ALU.mult), Rh(384, 768) + [R_eb], [R_ki])
                    op("pool", lambda: nc.gpsimd.tensor_copy(v_b[:], h[:, 768:1536]), Rh(768, 1536), [R_vb])
                    if S2L < 2:
                        return
                    bk = nb()
                    for hd in range(4):
                        mm(bk.f[0:96, hd:hd + 1], l_sb[:, hd * 96:(hd + 1) * 96], ones_f[:, 0:1], True, True, [R_l, R_const], [bk.res], signal=(hd == 3))
                    op("act", lambda: nc.scalar.activation(dec[:], bk.f[0:96, 0:4], AF.Exp, scale=-1.0 / 16.0), [bk.res], [R_dec])
                    if S2L < 3:
                        return
                    bk = nb()
                    for hd in range(4):
                        tr(bk.b[0:96, hd * 128:(hd + 1) * 128], qd[:, hd * 96:(hd + 1) * 96], id_b[:], [R_qd, R_const], [bk.res], signal=(hd == 3))
                    op("act", lambda: nc.scalar.copy(qdT[:], bk.b[0:96, 0:512].rearrange("p (h t) -> p h t", h=4)), [bk.res], [R_qdT])
                    bk = nb()
                    for hd in range(4):
                        tr(bk.b[0:96, hd * 128:(hd + 1) * 128], ki[:, hd * 96:(hd + 1) * 96], id_b[:], [R_ki, R_const], [bk.res], signal=(hd == 3))
                    op("dve", lambda: nc.vector.tensor_copy(kiT[:], bk.b[0:96, 0:512].rearrange("p (h t) -> p h t", h=4)), [bk.res], [R_kiT])
                    if S2L < 4:
                        return
                    bk = nb()
                    for hd in range(4):
                        mm(bk.f[:, hd * 128:(hd + 1) * 128], kiT[:, hd, :], qdT[:, hd, :], True, True, [R_kiT, R_qdT], [bk.res], signal=(hd == 3))
                    op("dve", lambda: nc.vector.tensor_tensor(out=attm[:], in0=bk.f[:].rearrange("p (h t) -> p h t", h=4),
                                                              in1=triu[:].unsqueeze(1).broadcast_to([128, 4, 128]), op=ALU.mult),
                       [bk.res, R_const], [R_attm])
                    if S2L < 5:
                        return
                    ob = [nb(), nb()]
                    for hd in range(4):
                        bo = ob[hd // 2]
                        oo = bo.f[:, (hd % 2) * 192:(hd % 2 + 1) * 192]
                        mm(oo, attm[:, hd, :], v_b[:, hd * 192:(hd + 1) * 192], True, False, [R_attm, R_vb], [bo.res], signal=False)
                        mm(oo, qdT[:, hd, :], S_b[:, hd, :], False, True, [R_qdT, R_Sb[hd]], [bo.res], signal=True)
                    if S2L < 6:
                        return
                    kb = [nb(), nb()]
                    for hd in range(4):
                        bkv = kb[hd // 2]
                        kk = bkv.f[0:96, (hd % 2) * 192:(hd % 2 + 1) * 192]
                        mm(kk, ki[:, hd * 96:(hd + 1) * 96], v_b[:, hd * 192:(hd + 1) * 192], True, True, [R_ki, R_vb], [bkv.res])
                        op("dve", lambda: nc.vector.tensor_scalar(kvd[:, hd, :], kk, dec[:, hd:hd + 1], None, op0=ALU.mult), [bkv.res, R_dec], [R_kvd[hd]])
                        op("dve", lambda: nc.vector.scalar_tensor_tensor(out=Sst[:, hd, :], in0=Sst[:, hd, :], scalar=dec[:, hd:hd + 1], in1=kvd[:, hd, :],
                                                                         op0=ALU.mult, op1=ALU.add), [R_S[hd], R_dec, R_kvd[hd]], [R_S[hd]])
                        op("pool", lambda: nc.gpsimd.tensor_copy(S_b[:, hd, :], Sst[:, hd, :]), [R_S[hd]], [R_Sb[hd]])
                    if S2L < 7:
                        return
                    for j in range(2):
                        op("act", lambda: nc.scalar.activation(sq[:, j * 384:(j + 1) * 384], ob[j].f[:, 0:384], AF.Square), [ob[j].res], [R_sq])
                    S7 = int(os.environ.get("S7", 99))
                    if S7 < 2:
                        return
                    op("dve", lambda: nc.vector.tensor_reduce(out=ss[:, 0:4], in_=sq[:].rearrange("p (h e) -> p h e", h=4), axis=AX.X, op=ALU.add), [R_sq], [R_ss])
                    if S7 < 3:
                        return
                    op("act", lambda: nc.scalar.activation(ss[:, 0:4], ss[:, 0:4], AF.Sqrt, bias=EPS_RMS, scale=1.0 / 192.0), [R_ss, R_const], [R_ss])
                    if S7 < 4:
                        return
                    op("dve", lambda: nc.vector.reciprocal(ss[:, 4:8], ss[:, 0:4]), [R_ss], [R_ss])
                    if S7 < 5:
                        return
                    for hd in range(4):
                        op("dve", lambda: nc.vector.scalar_tensor_tensor(out=on[:, hd * 192:(hd + 1) * 192], in0=ob[hd // 2].f[:, (hd % 2) * 192:(hd % 2 + 1) * 192],
                                                                         scalar=ss[:, 4 + hd:5 + hd], in1=gn_b[:], op0=ALU.mult, op1=ALU.mult),
                           [ob[hd // 2].res, R_ss, R_w], [R_on])
                    if S7 < 6:
                        return
                    op("act", lambda: nc.scalar.activation(sr[:], h[:, 1552:2320], AF.Silu), Rh(1552, 2320), [R_sr])
                    if S7 < 7:
                        return
                    VV = os.environ.get("VV", "0")
                    if VV == "1":
                        op("dve", lambda: nc.vector.tensor_tensor(out=sq[:], in0=on[:], in1=sr[:], op=ALU.mult), [R_on, R_sr], [R_sq])
                    elif VV == "2":
                        op("dve", lambda: nc.vector.tensor_copy(cat[:, 0:768], on[:]), [R_on], [R_cat])
                    elif VV == "3":
                        op("dve", lambda: nc.vector.tensor_copy(cat[:, 0:768], sr[:]), [R_sr], [R_cat])
                    else:
                        op("dve", lambda: nc.vector.tensor_tensor(out=cat[:, 0:768], in0=on[:], in1=sr[:], op=ALU.mult), [R_on, R_sr], [R_cat])

                def S2m(i):
                    p = i % 2
                    op("pool", lambda: nc.gpsimd.tensor_copy(mq_b[:], hs[p][:, 2320:2576]), [R_hs[p][g] for g in grp_of(2320, 2576)], [R_mqb])
                    mem_attn(0, memt, mq_b, R_mqb, cats[p][:, 768:1024], R_catm[p])

                def S3a(i):
                    use_rot("A3a", [5])
                    out_ln_router(0, i, outt, cats[i % 2], [R_cats[i % 2], R_catm[i % 2]], xt[i % 4], R_xt[i % 4], wout_b, g_b, b_b, R_wout, R_lnp, part="a")

                def SMB(step):
                    use_rot("A3b", [6, 7])
                    if 0 <= step - 1 < NTILE:
                        S2m(step - 1)
                    if 0 <= step - 3 < NTILE:
                        out_ln_router(0, step - 3, outt, None, None, xt[(step - 3) % 4], R_xt[(step - 3) % 4], wout_b, g_b, b_b, R_wout, R_lnp, part="b")

                for step in range(NTILE + 3):
                    chains = []
                    if step < NTILE:
                        chains.append(lambda: S1(step))
                    if 0 <= step - 1 < NTILE:
                        chains.append(lambda: S2(step - 1))
                    if 0 <= step - 2 < NTILE:
                        chains.append(lambda: S3a(step - 2))
                    if 0 <= step - 1 < NTILE or 0 <= step - 3 < NTILE:
                        chains.append(lambda: SMB(step))
                    COOP.run(chains)

        def phase_M(l, src, R_src, dst, R_dst, gsrc, bsrc):
            with contextlib.ExitStack() as es:
                set_rot(range(8))
                xTs = sbt(es, "xTs", [128, 8, ST_TOK], BF16)
                acc = sbt(es, "acc", [128, ST_TILES, D], F32)
                NTB = ST_TOK // 512
                R_xTs = [Res("xTs%d" % tb) for tb in range(NTB)]
                R_acc = [Res("acc%d" % t) for t in range(ST_TILES)]
                wg = [sbt(es, "wg%d" % p, [128, 8, 512], BF16) for p in range(2)]
                wu = [sbt(es, "wu%d" % p, [128, 8, 512], BF16) for p in range(2)]
                wd = [sbt(es, "wd%d" % p, [128, 4, D], BF16) for p in range(2)]
                R_wg = [Res("wg%d" % p) for p in range(2)]
                R_wu = [Res("wu%d" % p) for p in range(2)]
                R_wd = [Res("wd%d" % p) for p in range(2)]
                HT = [sbt(es, "HT%d" % p, [128, 4, 512], BF16) for p in range(2)]
                R_HT = [Res("HT%d" % p) for p in range(2)]
                sg = [sbt(es, "sg%d" % p, [128, 512], F32) for p in range(2)]
                R_sg = [Res("sg%d" % p) for p in range(2)]
                g_b, b_b, R_lnp = load_ln(es, l, gsrc, bsrc, "M%d" % l)
                NXR = 4
                xr = [sbt(es, "xr%d" % p, [128, D], F32) for p in range(NXR)]
                R_xr = [Res("xr%d" % p) for p in range(NXR)]
                lnt = (sbt(es, "m_st", [128, 12], F32), sbt(es, "m_mv", [128, 2], F32), sbt(es, "m_rs", [128, 2], F32))
                R_lnt = Res("mlnt")
                NST = S // ST_TOK
                steps = [(st, e) for st in range(NST) for e in range(NEXP)]

                def load_w(k):
                    st, e = steps[k]
                    p = k % 2
                    T.dma("pool", "lwg%d" % p, wg[p][:], w_exp_gate[l, e].rearrange("(kc p) f -> p kc f", p=128), writes=[R_wg[p]])
                    T.dma("pool", "lwu%d" % p, wu[p][:], w_exp_up[l, e].rearrange("(kc p) f -> p kc f", p=128), writes=[R_wu[p]])
                    T.dma("pool", "lwd%d" % p, wd[p][:], w_exp_down[l, e].rearrange("(fc p) d -> p fc d", p=128), writes=[R_wd[p]])

                def load_x(st):
                    for tb in range(NTB):
                        t0 = st * ST_TOK + tb * 512
                        T.dma("sp", "ldxTs%d" % tb, xTs[:, :, tb * 512:(tb + 1) * 512], XT[:, :, t0:t0 + 512],
                              reads=R_XTd[t0 // 128:t0 // 128 + 4], writes=[R_xTs[tb]])

                blocks = [(k, tb) for k in range(len(steps)) for tb in range(NTB)]

                def GU(bi):
                    k, tb = blocks[bi]
                    st, e = steps[k]
                    p = k % 2
                    hp = bi % 2
                    last = (e == NEXP - 1)
                    for fc in range(4):
                        bg = nb()
                        for kc in range(8):
                            mm(bg.f[:], wg[p][:, kc, fc * 128:(fc + 1) * 128], xTs[:, kc, tb * 512:(tb + 1) * 512], kc == 0, kc == 7,
                               [R_wg[p], R_xTs[tb]], [bg.res], signal=(kc == 7))
                        bu = nb()
                        for kc in range(8):
                            mm(bu.f[:], wu[p][:, kc, fc * 128:(fc + 1) * 128], xTs[:, kc, tb * 512:(tb + 1) * 512], kc == 0, kc == 7,
                               [R_wu[p], R_xTs[tb]], [bu.res], signal=(kc == 7))
                        sp_ = fc % 2
                        op("act", lambda: nc.scalar.activation(sg[sp_][:], bg.f[:], AF.Silu), [bg.res], [R_sg[sp_]])
                        op("dve", lambda: nc.vector.tensor_tensor(out=HT[hp][:, fc, :], in0=bu.f[:], in1=sg[sp_][:], op=ALU.mult),
                           [bu.res, R_sg[sp_]], [R_HT[hp]])
                    if last and st + 1 < NST:
                        t0 = (st + 1) * ST_TOK + tb * 512
                        T.dma("sp", "ldxTs%d" % tb, xTs[:, :, tb * 512:(tb + 1) * 512], XT[:, :, t0:t0 + 512],
                              reads=R_XTd[t0 // 128:t0 // 128 + 4], writes=[R_xTs[tb]])

                def DOWN(bi):
                    k, tb = blocks[bi]
                    st, e = steps[k]
                    p = k % 2
                    hp = bi % 2
                    last = (e == NEXP - 1)
                    if last:
                        for tt in range(4):
                            gi = st * ST_TILES + tb * 4 + tt
                            T.dma("sp", "ldxr%d" % (gi % NXR), xr[gi % NXR][:], src[gi * 128:(gi + 1) * 128, :], reads=[R_src[gi]], writes=[R_xr[gi % NXR]])
                    for tt in range(4):
                        ti = tb * 4 + tt
                        gi = st * ST_TILES + ti
                        for dh in range(2):
                            bo = nb()
                            for fc in range(4):
                                mm(bo.f[:], HT[hp][:, fc, tt * 128:(tt + 1) * 128], wd[p][:, fc, dh * 512:(dh + 1) * 512], fc == 0, fc == 3,
                                   [R_HT[hp], R_wd[p]], [bo.res], signal=(fc == 3))
                            a_ap = acc[:, ti, dh * 512:(dh + 1) * 512]
                            if e == 0:
                                op("dve", lambda: nc.vector.tensor_scalar(a_ap, bo.f[:], gates_all[:, gi, e:e + 1], None, op0=ALU.mult),
                                   [bo.res, R_gates[gi]], [R_acc[ti]])
                            else:
                                op("dve", lambda: nc.vector.scalar_tensor_tensor(out=a_ap, in0=bo.f[:], scalar=gates_all[:, gi, e:e + 1], in1=a_ap,
                                                                                 op0=ALU.mult, op1=ALU.add),
                                   [bo.res, R_gates[gi], R_acc[ti]], [R_acc[ti]])
                        if last:
                            q = gi % NXR
                            op("dve", lambda: nc.vector.scalar_tensor_tensor(out=xr[q][:], in0=xr[q][:], scalar=ALPHA, in1=acc[:, ti, :], op0=ALU.mult, op1=ALU.add),
                               [R_xr[q], R_acc[ti]], [R_xr[q]])
                            layer_norm(lnt, xr[q][:], R_xr[q], g_b[:], b_b[:], R_lnp, R_lnt, xr[q][:], R_xr[q])
                            T.dma("sp", "stM%d" % q, dst[gi * 128:(gi + 1) * 128, :], xr[q][:], reads=[R_xr[q]], writes=[R_dst[gi]])
                    if tb == NTB - 1 and k + 2 < len(steps):
                        load_w(k + 2)

                load_w(0)
                load_w(1)
                load_x(0)
                GU(0)
                for bi in range(len(blocks)):
                    if bi + 1 < len(blocks):
                        GU(bi + 1)
                    DOWN(bi)

        if stop_after != "P0":
            phase_A()
            _barrier(T)
        if dbg:
            T.dma("sp", "dg", DG[:, 0, :, :], gates_all[:], reads=R_gates)
        R_out = [Res("out%d" % i) for i in range(NTILE)]

        def phase_B(v_all, R_vall, kbias, R_kb):
            with contextlib.ExitStack() as es:
                use_rot("B0", range(8))
                wkv = sbt(es, "wkv", [128, 8, KV_IN], BF16)
                winb = sbt(es, "winb", [128, 8, B_IN], BF16)
                R_wkv = [Res("wkv%d" % kc) for kc in range(8)]
                R_winb = [Res("winb%d" % kc) for kc in range(8)]
                wkv_v = w_kv_shared.rearrange("(kc p) n -> p kc n", p=128)
                winb_v = w_in_b.rearrange("(kc p) n -> p kc n", p=128)
                for kc in range(8):
                    T.dma("pool", "wB_%d" % kc, wkv[:, kc, :], wkv_v[:, kc, :], writes=[R_wkv[kc]])
                    T.dma("pool", "wBb_%d" % kc, winb[:, kc, :], winb_v[:, kc, :], writes=[R_winb[kc]])
                gk_b = sbt(es, "gk_b", [128, 64], F32)
                gq_b = sbt(es, "gq_b", [128, 64], F32)
                bf_b = sbt(es, "bf_b", [128, 12], F32)
                R_pb = Res("parB")
                T.dma("sp", "pB0", gk_b[:], k_norm_g.broadcast_to([128, 64]), writes=[R_pb])
                T.dma("sp", "pB1", gq_b[:], q_norm_g.broadcast_to([128, 64]), writes=[R_pb])
                T.dma("sp", "pB2", bf_b[:], b_forget.broadcast_to([128, 12]), writes=[R_pb])
                xt = [sbt(es, "bxt%d" % p, [128, D], F32) for p in range(2)]
                xT = [sbt(es, "bxT%d" % p, [128, 8, 128], BF16) for p in range(2)]
                xb = [sbt(es, "bxb%d" % p, [128, D], BF16) for p in range(2)]
                R_xb = [Res("bxb%d" % p) for p in range(2)]
                hk = [sbt(es, "hk%d" % p, [128, KV_IN], F32) for p in range(2)]
                hb = [sbt(es, "hb%d" % p, [128, B_IN], F32) for p in range(2)]
                R_xt = [Res("bxt%d" % p) for p in range(2)]
                R_xT = [Res("bxT%d" % p) for p in range(2)]
                GK = [(0, 512), (512, 1024), (1024, 1536), (1536, KV_IN)]
                GB = [(0, 512), (512, 1024), (1024, 1536), (1536, B_IN)]
                R_hk = [[Res("hk%d_%d" % (p, g)) for g in range(4)] for p in range(2)]
                R_hb = [[Res("hb%d_%d" % (p, g)) for g in range(4)] for p in range(2)]
                sqt = sbt(es, "b_sq", [128, 768], F32)
                nrm = sbt(es, "b_nrm", [128, 768], F32)
                ssn = sbt(es, "b_ss", [128, 24], F32)
                k_augs = [sbt(es, "k_aug%d" % p, [128, 12, 72], BF16) for p in range(2)]
                q_augs = [sbt(es, "q_aug%d" % p, [128, 12, 72], BF16) for p in range(2)]
                lf = sbt(es, "b_lf", [128, 12], F32)
                carry = sbt(es, "b_carry", [128, 12], F32)
                t1 = sbt(es, "b_t1", [128, 12], F32)
                ogq = sbt(es, "b_ogq", [128, D], BF16)
                qT_t = sbt(es, "qT_t", [66, 12, 128], BF16)
                kT_t = sbt(es, "kT_t", [66, 12, 128], BF16)
                R_sq, R_nrm, R_ssn, R_lf, R_carry, R_t1, R_ogq, R_qTt, R_kTt = (
                    Res(n) for n in ["bsq", "bnrm", "bss", "lf", "carry", "t1", "ogq", "qTt", "kTt"])
                R_kas = [Res("ka%d" % p) for p in range(2)]
                R_qas = [Res("qa%d" % p) for p in range(2)]
                for p in range(2):
                    op("dve", lambda: nc.vector.memset(k_augs[p][:], 1.0), [], [R_kas[p]])
                    op("dve", lambda: nc.vector.memset(q_augs[p][:], 0.0), [], [R_qas[p]])
                op("dve", lambda: nc.vector.memset(carry[:], 0.0), [], [R_carry])
                for c4 in range(4):
                    op("pool", lambda: nc.gpsimd.memset(v_all[:, c4 * 8:(c4 + 1) * 8, :, :], 1.0), [], [R_vall[c4 * 8 + j] for j in range(8)])

                def grp_of(G, lo, hi):
                    return [g for g, (a, b) in enumerate(G) if a < hi and b > lo]

                def S1(i):
                    use_rot("B1", [0, 1, 2])
                    p = i % 2
                    T.dma("sp", "bldx%d" % p, xt[p][:], XB[i * 128:(i + 1) * 128, :], reads=[R_XB[i]], writes=[R_xt[p]])
                    op("pool", lambda: nc.gpsimd.tensor_copy(xb[p][:], xt[p][:]), [R_xt[p]], [R_xb[p]])
                    bk = nb()
                    for kc in range(8):
                        tr(bk.b[:, kc * 128:(kc + 1) * 128], xb[p][:, kc * 128:(kc + 1) * 128], id_b[:], [R_xb[p], R_const], [bk.res], signal=(kc == 7))
                    op("act", lambda: nc.scalar.copy(xT[p][:], bk.b[:].rearrange("p (k t) -> p k t", k=8)), [bk.res], [R_xT[p]])
                    n = 0
                    for (G, w, Rw, dst, Rd) in ((GK, wkv, R_wkv, hk[p], R_hk[p]), (GB, winb, R_winb, hb[p], R_hb[p])):
                        for g, (a, b) in enumerate(G):
                            bk = nb()
                            for kc in range(8):
                                mm(bk.f[:, 0:b - a], xT[p][:, kc, :], w[:, kc, a:b], kc == 0, kc == 7, [R_xT[p], Rw[kc]], [bk.res], signal=(kc == 7))
                            if n % 2 == 0:
                                op("dve", lambda: nc.vector.tensor_copy(dst[:, a:b], bk.f[:, 0:b - a]), [bk.res], [Rd[g]])
                            else:
                                op("act", lambda: nc.scalar.copy(dst[:, a:b], bk.f[:, 0:b - a]), [bk.res], [Rd[g]])
                            n += 1

                def rmsn(src, Rsrc, gb, aug, R_aug, off):
                    v3 = lambda a: a.rearrange("p (h e) -> p h e", h=12)
                    op("act", lambda: nc.scalar.activation(sqt[:], src, AF.Square), Rsrc, [R_sq])
                    op("dve", lambda: nc.vector.tensor_reduce(out=ssn[:, off:off + 12], in_=v3(sqt[:]), axis=AX.X, op=ALU.add), [R_sq], [R_ssn])
                    op("act", lambda: nc.scalar.activation(ssn[:, off:off + 12], ssn[:, off:off + 12], AF.Sqrt, bias=EPS_RMS, scale=1.0 / 64.0), [R_ssn, R_const], [R_ssn])
                    op("dve", lambda: nc.vector.reciprocal(ssn[:, off:off + 12], ssn[:, off:off + 12]), [R_ssn], [R_ssn])
                    op("dve", lambda: nc.vector.tensor_tensor(out=v3(nrm[:]), in0=v3(src), in1=ssn[:, off:off + 12].unsqueeze(2).broadcast_to([128, 12, 64]), op=ALU.mult),
                       Rsrc + [R_ssn], [R_nrm])
                    op("dve", lambda: nc.vector.tensor_tensor(out=aug[:, :, 0:64], in0=v3(nrm[:]), in1=gb[:].unsqueeze(1).broadcast_to([128, 12, 64]), op=ALU.mult),
                       [R_nrm, R_pb], [R_aug])

                def S2(i):
                    use_rot("B2", [3])
                    p = i % 2
                    k_aug, q_aug, R_ka, R_qa = k_augs[p], q_augs[p], R_kas[p], R_qas[p]
                    Rk = lambda lo, hi: [R_hk[p][g] for g in grp_of(GK, lo, hi)]
                    Rb = lambda lo, hi: [R_hb[p][g] for g in grp_of(GB, lo, hi)]
                    rmsn(hk[p][:, 0:768], Rk(0, 768), gk_b, k_aug, R_ka, 0)
                    rmsn(hb[p][:, 0:768], Rb(0, 768), gq_b, q_aug, R_qa, 12)
                    op("dve", lambda: nc.vector.tensor_tensor(out=lf[:], in0=hk[p][:, 1536:1548], in1=bf_b[:], op=ALU.add), Rk(1536, 1548) + [R_pb], [R_lf])
                    op("act", lambda: nc.scalar.activation(lf[:], lf[:], AF.Exp, scale=-1.0), [R_lf], [R_lf])
                    op("act", lambda: nc.scalar.activation(lf[:], lf[:], AF.Ln, bias=ONE, scale=1.0), [R_lf, R_const], [R_lf])
                    bk = nb()
                    mm(bk.f[:, 0:12], triu[:], lf[:], True, True, [R_const, R_lf], [bk.res], signal=False)
                    mm(bk.f[:, 16:28], ones_f[:], lf[:], True, True, [R_const, R_lf], [bk.res], signal=True)
                    op("dve", lambda: nc.vector.tensor_tensor(out=kbias[:, i, :], in0=bk.f[:, 0:12], in1=carry[:], op=ALU.add), [bk.res, R_carry], [R_kb[i]])
                    op("dve", lambda: nc.vector.tensor_tensor(out=carry[:], in0=bk.f[:, 16:28], in1=carry[:], op=ALU.add), [bk.res, R_carry], [R_carry])
                    op("dve", lambda: nc.vector.tensor_scalar(t1[:], kbias[:, i, :], -8.0, None, op0=ALU.mult), [R_kb[i]], [R_t1])
                    op("dve", lambda: nc.vector.tensor_copy(q_aug[:, :, 64], t1[:]), [R_t1], [R_qa])
                    op("dve", lambda: nc.vector.tensor_tensor(out=q_aug[:, :, 65], in0=t1[:], in1=q_aug[:, :, 64], op=ALU.subtract), [R_t1, R_qa], [R_qa])
                    op("pool", lambda: nc.gpsimd.tensor_copy(v_all[:, i, :, 0:64], hk[p][:, 768:1536].rearrange("p (h e) -> p h e", h=12)), Rk(768, 1536), [R_vall[i]])
                    op("act", lambda: nc.scalar.activation(ogq[:, 0:768], hb[p][:, 768:1536], AF.Sigmoid), Rb(768, 1536), [R_ogq])
                    op("pool", lambda: nc.gpsimd.tensor_copy(ogq[:, 768:1024], hb[p][:, 1536:1792]), Rb(1536, 1792), [R_ogq])
                    T.dma("sp", "sOGQ", OGQ[i * 128:(i + 1) * 128, :], ogq[:], reads=[R_ogq], writes=[R_OGQ[i]])

                def S2b(i):
                    use_rot("B2b", [4, 5, 6, 7])
                    p = i % 2
                    k_aug, q_aug, R_ka, R_qa = k_augs[p], q_augs[p], R_kas[p], R_qas[p]
                    for (aug, R_aug, tt, R_tt, dram, R_d, key, eng) in ((q_aug, R_qa, qT_t, R_qTt, QT, R_QTd, "sQT", "act"), (k_aug, R_ka, kT_t, R_kTt, KT, R_KTd, "sKT", "dve")):
                        for (h0, h1) in ((0, 8), (8, 12)):
                            bk = nb()
                            for h in range(h0, h1):
                                tr(bk.b[0:66, (h - h0) * 128:(h - h0 + 1) * 128], aug[:, h, 0:66], id_b[:], [R_aug, R_const], [bk.res], signal=(h == h1 - 1))
                            src = bk.b[0:66, 0:(h1 - h0) * 128].rearrange("p (h t) -> p h t", h=h1 - h0)
                            if eng == "act":
                                op("act", lambda: nc.scalar.copy(tt[:, h0:h1, :], src), [bk.res], [R_tt])
                            else:
                                op("dve", lambda: nc.vector.tensor_copy(tt[:, h0:h1, :], src), [bk.res], [R_tt])
                        T.dma("sp", key, dram.rearrange("h p t -> p h t")[:, :, i * 128:(i + 1) * 128], tt[:], reads=[R_tt], writes=[R_d[i]])

                for step in range(NTILE + 2):
                    chains = []
                    if step < NTILE:
                        chains.append(lambda: S1(step))
                    if 0 <= step - 1 < NTILE:
                        chains.append(lambda: S2(step - 1))
                    if 0 <= step - 2 < NTILE:
                        chains.append(lambda: S2b(step - 2))
                    COOP.run(chains)

        def phase_C(v_all, R_vall, kbias, R_kb, o_all, R_oall):
            with contextlib.ExitStack() as es:
                kTh = [sbt(es, "kTh%d" % p, [66, S], BF16) for p in range(2)]
                qTh = [sbt(es, "qTh%d" % p, [66, S], BF16) for p in range(2)]
                R_kTh = [Res("kTh%d" % p) for p in range(2)]
                R_qTh = [Res("qTh%d" % p) for p in range(2)]
                NPB = 6
                pex = [sbt(es, "pex%d" % j, [128, 512], BF16) for j in range(NPB)]
                R_pex = [Res("pex%d" % j) for j in range(NPB)]
                rdc = sbt(es, "rdc", [128, 4], F32)
                R_rdc = Res("rdc")
                set_rot([4, 5, 6, 7])
                acc_b = [banks[j] for j in range(4)]

                def load_h(h):
                    p = h % 2
                    T.dma("sp", "ldk%d" % p, kTh[p][:], KT[h], reads=R_KTd, writes=[R_kTh[p]])
                    T.dma("sp", "ldq%d" % p, qTh[p][:], QT[h], reads=R_QTd, writes=[R_qTh[p]])

                load_h(0)
                units = [(h, Q, c) for h in range(12) for Q in range(8) for c in range(4 * Q + 4)]
                sb_of = {}
                DEPTH = 3

                def qk(u):
                    h, Q, c = units[u]
                    p = h % 2
                    if Q == 0 and c == 0 and h + 1 < 12:
                        load_h(h + 1)
                    lo = 128 * max(0, c - 4 * Q)
                    bs = nb()
                    mm(bs.f[:, lo:512], kTh[p][0:66, c * 128:(c + 1) * 128], qTh[p][0:66, Q * 512 + lo:(Q + 1) * 512], True, True,
                       [R_kTh[p], R_qTh[p]], [bs.res])
                    sb_of[u] = bs

                def rest(u):
                    h, Q, c = units[u]
                    j0 = max(0, c - 4 * Q)
                    lo = 128 * j0
                    bs = sb_of.pop(u)
                    pj = u % NPB
                    op("act", lambda: nc.scalar.activation(pex[pj][:, lo:512], bs.f[:, lo:512], AF.Exp, bias=kbias[:, c, h:h + 1], scale=0.125),
                       [bs.res, R_kb[c]], [R_pex[pj]])
                    if c >= 4 * Q:
                        op("dve", lambda: nc.vector.tensor_tensor(out=pex[pj][:, lo:lo + 128], in0=pex[pj][:, lo:lo + 128], in1=triu_b[:], op=ALU.mult),
                           [R_pex[pj], R_const], [R_pex[pj]])
                    for j in range(j0, 4):
                        mm(acc_b[j].f[:, 0:65], pex[pj][:, j * 128:(j + 1) * 128], v_all[:, c, h, 0:65], c == 0, c == 4 * Q + j,
                           [R_pex[pj], R_vall[c]], [acc_b[j].res], signal=(j == 3))
                    if c == 4 * Q + 3:
                        for j in range(4):
                            ti = 4 * Q + j
                            op("dve", lambda: nc.vector.reciprocal(rdc[:, j:j + 1], acc_b[j].f[:, 64:65]), [acc_b[j].res], [R_rdc])
                            op("dve", lambda: nc.vector.tensor_scalar(o_all[:, ti, h * 64:(h + 1) * 64], acc_b[j].f[:, 0:64], rdc[:, j:j + 1], None, op0=ALU.mult),
                               [acc_b[j].res, R_rdc], [R_oall[ti]])

                for u in range(min(DEPTH, len(units))):
                    qk(u)
                for u in range(len(units)):
                    if u + DEPTH < len(units):
                        qk(u + DEPTH)
                    rest(u)

        def phase_D(o_all, R_oall):
            with contextlib.ExitStack() as es:
                use_rot("D0", range(8))
                wout_b = sbt(es, "wout_d", [128, 8, D], BF16)
                R_wout = Res("woutD")
                T.dma("pool", "wD", wout_b[:], w_out[1].rearrange("(kc p) n -> p kc n", p=128), writes=[R_wout])
                g_b, b_b, R_lnp = load_ln(es, 1, ln_mix_g, ln_mix_b, "D")
                NX = 6
                xt = [sbt(es, "dxt%d" % p, [128, D], F32) for p in range(NX)]
                og = [sbt(es, "dog%d" % p, [128, D], BF16) for p in range(2)]
                R_xt = [Res("dxt%d" % p) for p in range(NX)]
                R_og = [Res("dog%d" % p) for p in range(2)]
                cats = [sbt(es, "dcat%d" % p, [128, D], BF16) for p in range(4)]
                R_cats = [Res("dcat%d" % p) for p in range(4)]
                memt = alloc_mem_tiles(es)
                outts = [alloc_out_tiles(es), alloc_out_tiles(es)]

                def S1(i):
                    p = i % 2
                    T.dma("sp", "dldx%d" % (i % NX), xt[i % NX][:], XB[i * 128:(i + 1) * 128, :], reads=[R_XB[i]], writes=[R_xt[i % NX]])
                    T.dma("sp", "dldo%d" % p, og[p][:], OGQ[i * 128:(i + 1) * 128, :], reads=[R_OGQ[i]], writes=[R_og[p]])

                def S2(i):
                    p = i % 2
                    cat, R_cat = cats[i % 4], R_cats[i % 4]
                    op("dve", lambda: nc.vector.tensor_tensor(out=cat[:, 0:768], in0=o_all[:, i, :], in1=og[p][:, 0:768], op=ALU.mult), [R_oall[i], R_og[p]], [R_cat])
                    mem_attn(1, memt, og[p][:, 768:1024], R_og[p], cat[:, 768:1024], R_cat)

                def M(s2):
                    use_rot("D2", [0, 1])
                    S1(2 * s2)
                    S1(2 * s2 + 1)
                    S2(2 * s2)
                    S2(2 * s2 + 1)

                def S3a(i):
                    use_rot("D3a%d" % (i % 2), [2, 3] if i % 2 == 0 else [4, 5])
                    out_ln_router(1, i, outts[i % 2], cats[i % 4], R_cats[i % 4], xt[i % NX], R_xt[i % NX], wout_b, g_b, b_b, R_wout, R_lnp, part="a")

                def S3b(i):
                    use_rot("D3b%d" % (i % 2), [6] if i % 2 == 0 else [7])
                    out_ln_router(1, i, outts[i % 2], None, None, xt[i % NX], R_xt[i % NX], wout_b, g_b, b_b, R_wout, R_lnp, part="b")

                NP = NTILE // 2
                for s2 in range(NP + 2):
                    chains = []
                    if s2 < NP:
                        chains.append(lambda: M(s2))
                    if 0 <= s2 - 1 < NP:
                        chains.append(lambda: S3a(2 * (s2 - 1)))
                        chains.append(lambda: S3a(2 * (s2 - 1) + 1))
                    if 0 <= s2 - 2 < NP:
                        chains.append(lambda: S3b(2 * (s2 - 2)))
                        chains.append(lambda: S3b(2 * (s2 - 2) + 1))
                    COOP.run(chains)

        if stop_after in ("A", "P0"):
            pass
        else:
            phase_M(0, XA, R_XA, XB, R_XB, ln_ffn_g, ln_ffn_b)
            _barrier(T)
            if stop_after != "M0":
                with contextlib.ExitStack() as esL:
                    v_all = sbt(esL, "v_all", [128, NTILE, 12, 72], BF16)
                    kbias = sbt(esL, "kbias", [128, NTILE, 12], F32)
                    R_vall = [Res("vall%d" % i) for i in range(NTILE)]
                    R_kb = [Res("kb%d" % i) for i in range(NTILE)]
                    phase_B(v_all, R_vall, kbias, R_kb)
                    _barrier(T)
                    if stop_after != "B":
                        with contextlib.ExitStack() as esO:
                            o_all = sbt(esO, "o_all", [128, NTILE, 768], BF16)
                            R_oall = [Res("oall%d" % i) for i in range(NTILE)]
                            phase_C(v_all, R_vall, kbias, R_kb, o_all, R_oall)
                            _barrier(T)
                            phase_D(o_all, R_oall)
                            _barrier(T)
                            if dbg:
                                T.dma("sp", "dg", DG[:, 1, :, :], gates_all[:], reads=R_gates)
                _barrier(T)
                if stop_after not in ("B", "D"):
                    phase_M(1, XA, R_XA, out, R_out, ln_ffn_g, ln_ffn_b)

        _barrier(T)
    print("build: ninst=%d nwait=%d" % (T.ninst, T.nwait))
    T.close()
    return nc


_CONSTS = None


def _consts():
    global _CONSTS
    if _CONSTS is None:
        ident = np.eye(128, dtype=np.float32)
        triu = np.triu(np.ones((128, 128), dtype=np.float32))
        _CONSTS = {"c_ident": ident, "c_triu": triu}
    return _CONSTS


def make_in_map(inputs, b):
    f = lambda a: np.ascontiguousarray(np.asarray(a, dtype=np.float32))
    m = {
        "x": f(inputs["x"][b]), "mem": f(inputs["mem"][b]),
        "w_in_a": f(inputs["w_in_a"][0]), "w_gate_up_a": f(inputs["w_gate_up_a"][0]),
        "b_gate_a": f(inputs["b_gate_a"]).reshape(1, 384), "gla_norm_g": f(inputs["gla_norm_g"]).reshape(1, 192),
        "w_in_b": f(inputs["w_in_b"][0]), "q_norm_g": f(inputs["q_norm_g"]).reshape(1, 64),
        "w_kv_shared": f(inputs["w_kv_shared"]), "b_forget": f(inputs["b_forget"]).reshape(1, 12),
        "k_norm_g": f(inputs["k_norm_g"]).reshape(1, 64), "w_mem_kv": f(inputs["w_mem_kv"]),
        "w_out": f(inputs["w_out"]), "ln_mix_g": f(inputs["ln_mix_g"]), "ln_mix_b": f(inputs["ln_mix_b"]),
        "ln_ffn_g": f(inputs["ln_ffn_g"]), "ln_ffn_b": f(inputs["ln_ffn_b"]),
        "w_router": f(inputs["w_router"]), "b_router": f(inputs["b_router"]).reshape(1, 16),
        "w_exp_gate": f(inputs["w_exp_gate"]), "w_exp_up": f(inputs["w_exp_up"]), "w_exp_down": f(inputs["w_exp_down"]),
    }
    m.update(_consts())
    return m


def kernel(**inputs):
    nc = build()
    in_maps = [make_in_map(inputs, b) for b in range(N_CORES)]
    res = run_bass_kernel_spmd(nc, in_maps, core_ids=list(range(N_CORES)))
    return np.stack([np.asarray(r["out"], dtype=np.float32) for r in res.results], axis=0)
```

```python
import contextlib
import os
import threading
import numpy as np
import concourse.bass as bass
import concourse.mybir as mybir
from concourse.bass_utils import run_bass_kernel_spmd

F32 = mybir.dt.float32
BF16 = mybir.dt.bfloat16
AF = mybir.ActivationFunctionType
ALU = mybir.AluOpType
AX = mybir.AxisListType

S = 4096
D = 1024
NTILE = S // 128
N_CORES = 8
ALPHA = (2.0 * 2) ** 0.25
LN_EPS = 1e-5
RMS_EPS = 1e-6
A_IN = 2576
B_IN = 1792
KV_IN = 1548
NEXP = 16
ST_TOK = 2048
ST_TILES = ST_TOK // 128


class Coop:
    def __init__(self):
        self.tl = threading.local()

    def switch(self):
        st = getattr(self.tl, "st", None)
        if st is None:
            return
        i, sems, done, n = st
        for k in range(1, n):
            j = (i + k) % n
            if not done[j]:
                sems[j].release()
                sems[i].acquire()
                return

    def run(self, fns):
        n = len(fns)
        if n == 1:
            fns[0]()
            return
        sems = [threading.Semaphore(0) for _ in range(n)]
        done = [False] * n
        main = threading.Semaphore(0)
        errs = []

        def worker(i):
            sems[i].acquire()
            self.tl.st = (i, sems, done, n)
            try:
                fns[i]()
            except BaseException as e:
                errs.append(e)
            done[i] = True
            for k in range(1, n):
                j = (i + k) % n
                if not done[j]:
                    sems[j].release()
                    return
            main.release()

        ths = [threading.Thread(target=worker, args=(i,)) for i in range(n)]
        for t in ths:
            t.start()
        sems[0].release()
        main.acquire()
        for t in ths:
            t.join()
        if errs:
            raise errs[0]


COOP = Coop()


class Res:
    __slots__ = ("name", "w", "r", "excl")

    def __init__(self, name, excl=False):
        self.name = name
        self.w = None
        self.r = []
        self.excl = excl


class Trk:
    def __init__(self, nc):
        self.nc = nc
        self.eng = {"pe": nc.tensor, "act": nc.scalar, "dve": nc.vector, "pool": nc.gpsimd, "sp": nc.sync}
        self.sem = {}
        self.cnt = {}
        self.known = {e: {} for e in self.eng}
        self._stack = []
        for e in self.eng:
            cm = nc.semaphore("s_" + e)
            self.sem[e] = cm.__enter__()
            self._stack.append(cm)
            self.cnt[e] = 0
        self.dsem = {}
        self.dcnt = {}
        self.nwait = 0
        self.ninst = 0

    def close(self):
        for cm in reversed(self._stack):
            cm.__exit__(None, None, None)

    def _dma_sem(self, key):
        if key not in self.dsem:
            cm = self.nc.semaphore("d_" + key)
            self.dsem[key] = cm.__enter__()
            self._stack.append(cm)
            self.dcnt[key] = 0
        return self.dsem[key]

    def _need(self, e, toks):
        best = {}
        for t in toks:
            if t is None:
                continue
            name, sem, val, src = t
            if src == "pe" and e == "pe":
                continue
            if self.known[e].get(name, 0) >= val:
                continue
            if best.get(name, (None, 0))[1] < val:
                best[name] = (sem, val)
        for name, (sem, val) in best.items():
            self.eng[e].wait_ge(sem, val)
            self.known[e][name] = val
            self.nwait += 1

    @staticmethod
    def _compact(lst):
        best = {}
        for t in lst:
            if t[0] not in best or best[t[0]][2] < t[2]:
                best[t[0]] = t
        return list(best.values())

    def op(self, e, fn, reads=(), writes=(), signal=True):
        toks = []
        for r in reads:
            toks.append(r.w)
            if r.excl and e != "pe":
                toks.extend(r.r)
        for w in writes:
            toks.append(w.w)
            toks.extend(w.r)
        self._need(e, toks)
        ins = fn()
        self.ninst += 1
        if signal:
            self.cnt[e] += 1
            ins.then_inc(self.sem[e], 1)
            tok = ("E" + e, self.sem[e], self.cnt[e], e)
        else:
            tok = ("E" + e, self.sem[e], self.cnt[e] + 1, e)
        for r in reads:
            if r in writes:
                continue
            r.r.append(tok)
            if len(r.r) > 16:
                r.r = self._compact(r.r)
        for w in writes:
            w.w = tok
            w.r = []
        COOP.switch()
        return ins

    def dma(self, q, key, out, in_, reads=(), writes=()):
        toks = []
        for r in reads:
            toks.append(r.w)
        for w in writes:
            toks.append(w.w)
            toks.extend(w.r)
        self._need(q, toks)
        sem = self._dma_sem(key)
        self.dcnt[key] += 16
        self.eng[q].dma_start(out=out, in_=in_).then_inc(sem, 16)
        self.ninst += 1
        tok = ("D" + key, sem, self.dcnt[key], "dma")
        for r in reads:
            r.r.append(tok)
            if len(r.r) > 16:
                r.r = self._compact(r.r)
        for w in writes:
            w.w = tok
            w.r = []
        COOP.switch()
        return tok


def _barrier(T):
    for e in T.eng:
        for f in T.eng:
            if T.cnt[f] > 0 and T.known[e].get("E" + f, 0) < T.cnt[f]:
                T.eng[e].wait_ge(T.sem[f], T.cnt[f])
                T.known[e]["E" + f] = T.cnt[f]
                T.nwait += 1
        for key in list(T.dsem.keys()):
            if T.known[e].get("D" + key, 0) < T.dcnt[key]:
                T.eng[e].wait_ge(T.dsem[key], T.dcnt[key])
                T.known[e]["D" + key] = T.dcnt[key]
                T.nwait += 1


class Bank:
    def __init__(self, f, res):
        self.f = f
        self.b = f.bitcast(BF16)
        self.res = res


def build(dbg=False, stop_after=None):
    nc = bass.Bass("TRN2", target_bir_lowering=False)

    def din(name, shape):
        return nc.dram_tensor(name, list(shape), F32, kind="ExternalInput").ap()

    x = din("x", [S, D])
    mem = din("mem", [256, D])
    w_in_a = din("w_in_a", [D, A_IN])
    w_gate_up_a = din("w_gate_up_a", [16, 384])
    b_gate_a = din("b_gate_a", [1, 384])
    gla_norm_g = din("gla_norm_g", [1, 192])
    w_in_b = din("w_in_b", [D, B_IN])
    q_norm_g = din("q_norm_g", [1, 64])
    w_kv_shared = din("w_kv_shared", [D, KV_IN])
    b_forget = din("b_forget", [1, 12])
    k_norm_g = din("k_norm_g", [1, 64])
    w_mem_kv = din("w_mem_kv", [2, D, 512])
    w_out = din("w_out", [2, D, D])
    ln_mix_g = din("ln_mix_g", [2, D])
    ln_mix_b = din("ln_mix_b", [2, D])
    ln_ffn_g = din("ln_ffn_g", [2, D])
    ln_ffn_b = din("ln_ffn_b", [2, D])
    w_router = din("w_router", [D, 16])
    b_router = din("b_router", [1, 16])
    w_exp_gate = din("w_exp_gate", [2, NEXP, D, 512])
    w_exp_up = din("w_exp_up", [2, NEXP, D, 512])
    w_exp_down = din("w_exp_down", [2, NEXP, 512, D])
    c_ident = din("c_ident", [128, 128])
    c_triu = din("c_triu", [128, 128])

    out = nc.dram_tensor("out", [S, D], F32, kind="ExternalOutput").ap()
    kscr = "ExternalOutput" if dbg else "Internal"
    XA = nc.dram_tensor("XA", [S, D], F32, kind=kscr).ap()
    XB = nc.dram_tensor("XB", [S, D], F32, kind=kscr).ap()
    XT = nc.dram_tensor("XT", [128, 8, S], BF16, kind="Internal").ap()
    QT = nc.dram_tensor("QT", [12, 66, S], BF16, kind="Internal").ap()
    KT = nc.dram_tensor("KT", [12, 66, S], BF16, kind="Internal").ap()
    OGQ = nc.dram_tensor("OGQ", [S, D], BF16, kind="Internal").ap()
    if dbg:
        DG = nc.dram_tensor("DG", [128, 2, NTILE, 16], F32, kind="ExternalOutput").ap()
        DCAT = nc.dram_tensor("DCAT", [2, S, D], BF16, kind="ExternalOutput").ap()

    T = Trk(nc)
    out_toks = []
    R_XA = [Res('XA%d' % i) for i in range(NTILE)]
    R_XB = [Res('XB%d' % i) for i in range(NTILE)]
    R_XTd = [Res('XTd%d' % i) for i in range(NTILE)]
    R_OGQ = [Res('OGQ%d' % i) for i in range(NTILE)]
    R_QTd = [Res('QTd%d' % i) for i in range(NTILE)]
    R_KTd = [Res('KTd%d' % i) for i in range(NTILE)]

    with contextlib.ExitStack() as ges:
        uid = [0]

        def sbt(es, name, shape, dt):
            uid[0] += 1
            return es.enter_context(nc.sbuf_tensor("%s_%d" % (name, uid[0]), list(shape), dt))

        banks = []
        for i in range(8):
            pt = ges.enter_context(nc.psum_tensor("pb%d" % i, [128, 512], F32))
            banks.append(Bank(pt[:], Res("pb%d" % i, excl=True)))
        rots = {}
        rtl = threading.local()

        def use_rot(name, lst):
            if name not in rots:
                rots[name] = {"lst": list(lst), "i": 0}
            rtl.cur = rots[name]

        def set_rot(lst):
            use_rot("main%s" % (tuple(lst),), lst)

        def nb():
            rot = rtl.cur
            b = banks[rot["lst"][rot["i"] % len(rot["lst"])]]
            rot["i"] += 1
            return b

        def op(e, fn, reads=(), writes=(), signal=True):
            return T.op(e, fn, reads, writes, signal)

        def mm(o, lhsT, rhs, start, stop, reads, writes, signal=True):
            return T.op("pe", lambda: nc.tensor.matmul(o, lhsT, rhs, start=start, stop=stop), reads, writes, signal)

        def tr(o, in_, ident, reads, writes, signal=True):
            return T.op("pe", lambda: nc.tensor.transpose(o, in_, ident), reads, writes, signal)

        id_f = sbt(ges, "id_f", [128, 128], F32)
        id_b = sbt(ges, "id_b", [128, 128], BF16)
        triu = sbt(ges, "triu", [128, 128], F32)
        triu_b = sbt(ges, "triu_b", [128, 128], BF16)
        ones_f = sbt(ges, "ones_f", [128, 128], F32)
        cst = sbt(ges, "cst", [128, 4], F32)
        gates_all = sbt(ges, "gates_all", [128, NTILE, 16], F32)
        mkT = sbt(ges, "mkT", [128, 2, 2, 256], BF16)
        mv_aug = sbt(ges, "mv_aug", [128, 2, 2, 4, 72], BF16)
        wr_f = sbt(ges, "wr_f", [128, 8, 16], F32)
        br_b = sbt(ges, "br_b", [128, 16], F32)
        R_const = Res("const")
        R_gates = [Res("gates%d" % i) for i in range(NTILE)]
        R_mk = Res("mk")
        T.dma("sp", "c0", id_f[:], c_ident, writes=[R_const])
        T.dma("sp", "c1", triu[:], c_triu, writes=[R_const])
        T.dma("pool", "c2", id_b[:], c_ident, writes=[R_const])
        T.dma("pool", "c3", triu_b[:], c_triu, writes=[R_const])
        T.dma("sp", "c4", wr_f[:], w_router.rearrange("(kc p) e -> p kc e", p=128), writes=[R_const])
        T.dma("sp", "c5", br_b[:], b_router.broadcast_to([128, 16]), writes=[R_const])
        op("dve", lambda: nc.vector.memset(ones_f[:], 1.0), writes=[R_const])
        op("dve", lambda: nc.vector.memset(cst[:, 0:1], LN_EPS), writes=[R_const])
        op("dve", lambda: nc.vector.memset(cst[:, 1:2], RMS_EPS), writes=[R_const])
        op("dve", lambda: nc.vector.memset(cst[:, 2:3], 1.0), writes=[R_const])
        op("dve", lambda: nc.vector.memset(mv_aug[:], 1.0), writes=[R_mk])
        EPS_LN = cst[:, 0:1]
        EPS_RMS = cst[:, 1:2]
        ONE = cst[:, 2:3]

        with contextlib.ExitStack() as es:
            set_rot(range(8))
            mem_f = sbt(es, "mem_f", [128, 2, D], F32)
            memT = sbt(es, "memT", [128, 8, 256], BF16)
            wm = sbt(es, "wm", [128, 8, 512], BF16)
            R_memf, R_memT, R_wm = Res("memf"), Res("memT"), Res("wm")
            T.dma("sp", "p0a", mem_f[:], mem.rearrange("(mc p) d -> p mc d", p=128), writes=[R_memf])
            for mc in range(2):
                for half in range(2):
                    bk = nb()
                    for q in range(4):
                        kc = half * 4 + q
                        tr(bk.f[:, q * 128:(q + 1) * 128], mem_f[:, mc, kc * 128:(kc + 1) * 128], id_f[:],
                           [R_memf, R_const], [bk.res], signal=(q == 3))
                    op("act", lambda: nc.scalar.copy(memT[:, half * 4:(half + 1) * 4, mc * 128:(mc + 1) * 128],
                                                     bk.f[:].rearrange("p (q t) -> p q t", q=4)),
                       [bk.res], [R_memT])
            for l in range(2):
                T.dma("pool", "p0b", wm[:], w_mem_kv[l].rearrange("(kc p) n -> p kc n", p=128), writes=[R_wm])
                for j in range(2):
                    bk = nb()
                    for kc in range(8):
                        mm(bk.f[:, 0:256], wm[:, kc, j * 128:(j + 1) * 128], memT[:, kc, :], kc == 0, kc == 7,
                           [R_wm, R_memT], [bk.res], signal=(kc == 7))
                    op("act", lambda: nc.scalar.copy(mkT[:, l, j, :], bk.f[:, 0:256]), [bk.res], [R_mk])
                for mc in range(2):
                    bk = nb()
                    for kc in range(8):
                        mm(bk.f[:, 0:256], memT[:, kc, mc * 128:(mc + 1) * 128], wm[:, kc, 256:512], kc == 0, kc == 7,
                           [R_wm, R_memT], [bk.res], signal=(kc == 7))
                    op("dve", lambda: nc.vector.tensor_copy(mv_aug[:, l, mc, :, 0:64],
                                                            bk.f[:, 0:256].rearrange("p (h e) -> p h e", h=4)),
                       [bk.res], [R_mk])

        _barrier(T)
        def layer_norm(es_tiles, zt, R_zt, g_b, b_b, R_gb, R_ln, outap, R_out):
            st, mv, rs = es_tiles
            op("dve", lambda: nc.vector.bn_stats(st[:, 0:6], zt[:, 0:512]), [R_zt], [R_ln])
            op("dve", lambda: nc.vector.bn_stats(st[:, 6:12], zt[:, 512:1024]), [R_zt], [R_ln])
            op("dve", lambda: nc.vector.bn_aggr(mv[:, 0:2], st[:, 0:12]), [R_ln], [R_ln])
            op("act", lambda: nc.scalar.activation(rs[:, 0:1], mv[:, 1:2], AF.Sqrt, bias=EPS_LN, scale=1.0), [R_ln, R_const], [R_ln])
            op("dve", lambda: nc.vector.reciprocal(rs[:, 1:2], rs[:, 0:1]), [R_ln], [R_ln])
            op("dve", lambda: nc.vector.scalar_tensor_tensor(out=zt, in0=zt, scalar=mv[:, 0:1], in1=g_b, op0=ALU.subtract, op1=ALU.mult), [R_zt, R_ln, R_gb], [R_zt])
            op("dve", lambda: nc.vector.scalar_tensor_tensor(out=outap, in0=zt, scalar=rs[:, 1:2], in1=b_b, op0=ALU.mult, op1=ALU.add), [R_zt, R_ln, R_gb], [R_out])

        def mem_attn(l, tl, mq_b, R_mq, cat_mem, R_cat):
            mqT, pexp, rd, R_t = tl
            bk = nb()
            for j in range(2):
                tr(bk.b[:, j * 128:(j + 1) * 128], mq_b[:, j * 128:(j + 1) * 128], id_b[:], [R_mq, R_const], [bk.res], signal=(j == 1))
            op("act", lambda: nc.scalar.copy(mqT[:], bk.b[:, 0:256].rearrange("p (j t) -> p j t", j=2)), [bk.res], [R_t])
            MA = int(os.environ.get("MA", 99))
            if MA < 2:
                return
            bkh = [nb(), nb()]
            for hp in range(2):
                for hh in range(2):
                    pb = hh * 64
                    for mc in range(2):
                        mm(bkh[hh].f[:, (hp * 2 + mc) * 128:(hp * 2 + mc + 1) * 128], mkT[pb:pb + 64, l, hp, mc * 128:(mc + 1) * 128],
                           mqT[pb:pb + 64, hp, :], True, True, [R_mk, R_t], [bkh[hh].res], signal=(hp == 1 and mc == 1))
            for hh in range(2):
                op("act", lambda: nc.scalar.activation(pexp[:, hh * 4:(hh + 1) * 4, :], bkh[hh].f[:].rearrange("p (a t) -> p a t", a=4),
                                                       AF.Exp, scale=0.125), [bkh[hh].res], [R_t])
            if MA < 3:
                return
            bk = nb()
            for h in range(4):
                for mc in range(2):
                    mm(bk.f[:, h * 128:h * 128 + 65], pexp[:, (h % 2) * 4 + (h // 2) * 2 + mc, :], mv_aug[:, l, mc, h, 0:65], mc == 0, mc == 1,
                       [R_t, R_mk], [bk.res], signal=(h == 3 and mc == 1))
            pv = bk.f[:].rearrange("p (h e) -> p h e", e=128)
            if MA < 4:
                return
            op("dve", lambda: nc.vector.reciprocal(rd[:, 0:4], pv[:, :, 64]), [bk.res], [R_t])
            op("dve", lambda: nc.vector.tensor_tensor(out=cat_mem.rearrange("p (h e) -> p h e", h=4), in0=pv[:, :, 0:64],
                                                      in1=rd[:, 0:4].unsqueeze(2).broadcast_to([128, 4, 64]), op=ALU.mult),
               [bk.res, R_t], [R_cat])

        def out_ln_router(l, i, tl, cat, R_cat, xt, R_xt, wout_b, g_b, b_b, R_w, R_gb, part="ab"):
            catT, R_catT, lnt, R_lnt, x1T_f, x1T_b, R_x1T, rt, R_rt = tl
            if "a" in part:
                out_ln_a(l, i, tl, cat, R_cat, xt, R_xt, wout_b, g_b, b_b, R_w, R_gb)
            if "b" in part:
                out_ln_b(l, i, tl, xt, R_xt)

        def out_ln_a(l, i, tl, cat, R_cat, xt, R_xt, wout_b, g_b, b_b, R_w, R_gb):
            catT, R_catT, lnt, R_lnt, x1T_f, x1T_b, R_x1T, rt, R_rt = tl
            rc = list(R_cat) if isinstance(R_cat, (list, tuple)) else [R_cat]
            if dbg:
                T.dma("sp", "dcat", DCAT[l, i * 128:(i + 1) * 128, :], cat[:], reads=rc)
            bk = nb()
            for kc in range(8):
                tr(bk.b[:, kc * 128:(kc + 1) * 128], cat[:, kc * 128:(kc + 1) * 128], id_b[:], rc + [R_const], [bk.res], signal=(kc == 7))
            op("act", lambda: nc.scalar.copy(catT[:], bk.b[:].rearrange("p (k t) -> p k t", k=8)), [bk.res], [R_catT])
            for half in range(2):
                bk = nb()
                for kc in range(8):
                    mm(bk.f[:], catT[:, kc, :], wout_b[:, kc, half * 512:(half + 1) * 512], kc == 0, kc == 7,
                       [R_catT, R_w], [bk.res], signal=(kc == 7))
                op("dve", lambda: nc.vector.scalar_tensor_tensor(out=xt[:, half * 512:(half + 1) * 512], in0=xt[:, half * 512:(half + 1) * 512],
                                                                 scalar=ALPHA, in1=bk.f[:], op0=ALU.mult, op1=ALU.add),
                   [R_xt, bk.res], [R_xt])
            layer_norm(lnt, xt[:], R_xt, g_b[:], b_b[:], R_gb, R_lnt, xt[:], R_xt)
            T.dma("sp", "sXA%d" % (i % 2), XA[i * 128:(i + 1) * 128, :], xt[:], reads=[R_xt], writes=[R_XA[i]])

        def out_ln_b(l, i, tl, xt, R_xt):
            catT, R_catT, lnt, R_lnt, x1T_f, x1T_b, R_x1T, rt, R_rt = tl
            for half in range(2):
                bk = nb()
                for q in range(4):
                    kc = half * 4 + q
                    tr(bk.f[:, q * 128:(q + 1) * 128], xt[:, kc * 128:(kc + 1) * 128], id_f[:], [R_xt, R_const], [bk.res], signal=(q == 3))
                op("act", lambda: nc.scalar.copy(x1T_f[:, half * 4:(half + 1) * 4, :], bk.f[:].rearrange("p (q t) -> p q t", q=4)), [bk.res], [R_x1T])
                op("dve", lambda: nc.vector.tensor_copy(x1T_b[:, half * 4:(half + 1) * 4, :], bk.f[:].rearrange("p (q t) -> p q t", q=4)), [bk.res], [R_x1T])
            T.dma("sp", "sXT%d" % (i % 2), XT[:, :, i * 128:(i + 1) * 128], x1T_b[:], reads=[R_x1T], writes=[R_XTd[i]])
            bk = nb()
            for kc in range(8):
                mm(bk.f[:, 0:16], x1T_f[:, kc, :], wr_f[:, kc, :], kc == 0, kc == 7, [R_x1T, R_const], [bk.res], signal=(kc == 7))
            aff, sel, m1, eq, m2, sc, gm = rt
            v3 = lambda a: a.rearrange("p (g e) -> p g e", g=4)
            bc3 = lambda a: a.unsqueeze(2).broadcast_to([128, 4, 4])
            op("act", lambda: nc.scalar.activation(aff[:], bk.f[:, 0:16], AF.Sigmoid), [bk.res], [R_rt])
            op("dve", lambda: nc.vector.tensor_tensor(out=sel[:], in0=aff[:], in1=br_b[:], op=ALU.add), [R_rt, R_const], [R_rt])
            op("dve", lambda: nc.vector.tensor_reduce(out=m1[:], in_=v3(sel[:]), axis=AX.X, op=ALU.max), [R_rt], [R_rt])
            op("dve", lambda: nc.vector.tensor_tensor(out=v3(eq[:]), in0=v3(sel[:]), in1=bc3(m1[:]), op=ALU.is_equal), [R_rt], [R_rt])
            op("dve", lambda: nc.vector.scalar_tensor_tensor(out=eq[:], in0=eq[:], scalar=-1e9, in1=sel[:], op0=ALU.mult, op1=ALU.add), [R_rt], [R_rt])
            op("dve", lambda: nc.vector.tensor_reduce(out=m2[:], in_=v3(eq[:]), axis=AX.X, op=ALU.max), [R_rt], [R_rt])
            op("dve", lambda: nc.vector.tensor_tensor(out=sc[:], in0=m1[:], in1=m2[:], op=ALU.add), [R_rt], [R_rt])
            op("dve", lambda: nc.vector.tensor_reduce(out=gm[:, 0:1], in_=sc[:], axis=AX.X, op=ALU.max), [R_rt], [R_rt])
            op("dve", lambda: nc.vector.tensor_scalar(sc[:], sc[:], gm[:, 0:1], None, op0=ALU.is_ge), [R_rt], [R_rt])
            op("dve", lambda: nc.vector.tensor_tensor(out=v3(eq[:]), in0=v3(sel[:]), in1=bc3(m2[:]), op=ALU.is_ge), [R_rt], [R_rt])
            op("dve", lambda: nc.vector.tensor_tensor(out=v3(eq[:]), in0=v3(eq[:]), in1=bc3(sc[:]), op=ALU.mult), [R_rt], [R_rt])
            op("dve", lambda: nc.vector.tensor_tensor(out=eq[:], in0=eq[:], in1=aff[:], op=ALU.mult), [R_rt], [R_rt])
            op("dve", lambda: nc.vector.tensor_reduce(out=gm[:, 1:2], in_=eq[:], axis=AX.X, op=ALU.add), [R_rt], [R_rt])
            op("dve", lambda: nc.vector.reciprocal(gm[:, 2:3], gm[:, 1:2]), [R_rt], [R_rt])
            op("dve", lambda: nc.vector.tensor_scalar(gates_all[:, i, :], eq[:], gm[:, 2:3], None, op0=ALU.mult), [R_rt], [R_gates[i]])

        def alloc_out_tiles(es):
            catT = sbt(es, "catT", [128, 8, 128], BF16)
            st = sbt(es, "ln_st", [128, 12], F32)
            mv = sbt(es, "ln_mv", [128, 2], F32)
            rs = sbt(es, "ln_rs", [128, 2], F32)
            x1T_f = sbt(es, "x1T_f", [128, 8, 128], F32)
            x1T_b = sbt(es, "x1T_b", [128, 8, 128], BF16)
            rt = (sbt(es, "r_aff", [128, 16], F32), sbt(es, "r_sel", [128, 16], F32), sbt(es, "r_m1", [128, 4], F32),
                  sbt(es, "r_eq", [128, 16], F32), sbt(es, "r_m2", [128, 4], F32), sbt(es, "r_sc", [128, 4], F32),
                  sbt(es, "r_gm", [128, 4], F32))
            return (catT, Res("catT"), (st, mv, rs), Res("lnt"), x1T_f, x1T_b, Res("x1T"), rt, Res("rt"))

        def alloc_mem_tiles(es):
            return (sbt(es, "mqT", [128, 2, 128], BF16), sbt(es, "pexp_m", [128, 8, 128], BF16), sbt(es, "rd_m", [128, 4], F32), Res("memt"))

        def load_ln(es, l, gsrc, bsrc, key):
            g_b = sbt(es, "g_b" + key, [128, D], F32)
            b_b = sbt(es, "b_b" + key, [128, D], F32)
            R = Res("ln" + key)
            T.dma("sp", "lng" + key, g_b[:], gsrc[l:l + 1, :].broadcast_to([128, D]), writes=[R])
            T.dma("sp", "lnb" + key, b_b[:], bsrc[l:l + 1, :].broadcast_to([128, D]), writes=[R])
            return g_b, b_b, R

        def phase_A():
            with contextlib.ExitStack() as es:
                use_rot("A0", range(8))
                wina = sbt(es, "wina", [128, 8, A_IN], BF16)
                wout_b = sbt(es, "wout_b", [128, 8, D], BF16)
                wup17 = sbt(es, "wup17", [17, 384], F32)
                gn_b = sbt(es, "gn_b", [128, 192], F32)
                R_w = Res("wA")
                R_wina = [Res("wina%d" % kc) for kc in range(8)]
                R_wout = Res("woutA")
                wv = w_in_a.rearrange("(kc p) n -> p kc n", p=128)
                for kc in range(8):
                    T.dma("pool", "wA_%d" % kc, wina[:, kc, :], wv[:, kc, :], writes=[R_wina[kc]])
                T.dma("pool", "wA2", wout_b[:], w_out[0].rearrange("(kc p) n -> p kc n", p=128), writes=[R_wout])
                T.dma("sp", "wA3", wup17[0:16, :], w_gate_up_a, writes=[R_w])
                T.dma("sp", "wA4", wup17[16:17, :], b_gate_a, writes=[R_w])
                T.dma("sp", "wA5", gn_b[:], gla_norm_g.broadcast_to([128, 192]), writes=[R_w])
                g_b, b_b, R_lnp = load_ln(es, 0, ln_mix_g, ln_mix_b, "A")
                xt = [sbt(es, "xt%d" % p, [128, D], F32) for p in range(4)]
                xT = [sbt(es, "xT%d" % p, [128, 8, 128], BF16) for p in range(2)]
                xb = [sbt(es, "xb%d" % p, [128, D], BF16) for p in range(2)]
                R_xb = [Res("xb%d" % p) for p in range(2)]
                hs = [sbt(es, "hs%d" % p, [128, A_IN], F32) for p in range(2)]
                R_xt = [Res("xt%d" % p) for p in range(4)]
                R_xT = [Res("xT%d" % p) for p in range(2)]
                R_hs = [[Res("hs%d_%d" % (p, g)) for g in range(6)] for p in range(2)]
                gT17 = [sbt(es, "gT17_%d" % p, [17, 128], F32) for p in range(2)]
                R_gT = [Res("gT%d" % p) for p in range(2)]
                for p in range(2):
                    op("dve", lambda: nc.vector.memset(gT17[p][:], 1.0), [], [R_gT[p]])
                l_sb = sbt(es, "l_sb", [128, 384], F32)
                eb = sbt(es, "eb", [128, 384], F32)
                enb = sbt(es, "enb", [128, 384], F32)
                qd = sbt(es, "qd", [128, 384], BF16)
                ki = sbt(es, "ki", [128, 384], BF16)
                v_b = sbt(es, "v_b", [128, 768], BF16)
                dec = sbt(es, "dec", [96, 4], F32)
                qdT = sbt(es, "qdT", [96, 4, 128], BF16)
                kiT = sbt(es, "kiT", [96, 4, 128], BF16)
                attm = sbt(es, "attm", [128, 4, 128], BF16)
                Sst = sbt(es, "Sst", [96, 4, 192], F32)
                S_b = sbt(es, "S_b", [96, 4, 192], BF16)
                kvd = sbt(es, "kvd", [96, 4, 192], F32)
                sq = sbt(es, "sq", [128, 768], F32)
                ss = sbt(es, "ss", [128, 8], F32)
                on = sbt(es, "on", [128, 768], F32)
                sr = sbt(es, "sr", [128, 768], F32)
                cats = [sbt(es, "cat%d" % p, [128, D], BF16) for p in range(2)]
                mq_b = sbt(es, "mq_b", [128, 256], BF16)
                R_l, R_eb, R_qd, R_ki, R_vb, R_dec, R_qdT, R_kiT, R_attm = (Res(n) for n in ["l", "eb", "qd", "ki", "vb", "dec", "qdT", "kiT", "attm"])
                R_S = [Res("S%d" % h) for h in range(4)]
                R_Sb = [Res("Sb%d" % h) for h in range(4)]
                R_kvd = [Res("kvd%d" % h) for h in range(4)]
                R_sq, R_ss, R_on, R_sr, R_mqb = (Res(n) for n in ["sq", "ss", "on", "sr", "mqb"])
                R_cats = [Res("cat%d" % p) for p in range(2)]
                R_catm = [Res("catm%d" % p) for p in range(2)]
                op("dve", lambda: nc.vector.memset(Sst[:], 0.0), [], R_S)
                op("pool", lambda: nc.gpsimd.memset(S_b[:], 0.0), [], R_Sb)
                memt = alloc_mem_tiles(es)
                outt = alloc_out_tiles(es)
                GRP = [(0, 512), (512, 1024), (1024, 1536), (1536, 2048), (2048, 2560), (2560, 2576)]

                def grp_of(lo, hi):
                    return [g for g, (a, b) in enumerate(GRP) if a < hi and b > lo]

                def S1(i):
                    use_rot("A1", [0])
                    p = i % 2
                    p3 = i % 4
                    T.dma("sp", "ldx%d" % p3, xt[p3][:], x[i * 128:(i + 1) * 128, :], writes=[R_xt[p3]])
                    op("pool", lambda: nc.gpsimd.tensor_copy(xb[p][:], xt[p3][:]), [R_xt[p3]], [R_xb[p]])
                    bk = nb()
                    for kc in range(8):
                        tr(bk.b[:, kc * 128:(kc + 1) * 128], xb[p][:, kc * 128:(kc + 1) * 128], id_b[:], [R_xb[p], R_const], [bk.res], signal=(kc == 7))
                    op("act", lambda: nc.scalar.copy(xT[p][:], bk.b[:].rearrange("p (k t) -> p k t", k=8)), [bk.res], [R_xT[p]])
                    for g, (a, b) in enumerate(GRP):
                        bk = nb()
                        for kc in range(8):
                            mm(bk.f[:, 0:b - a], xT[p][:, kc, :], wina[:, kc, a:b], kc == 0, kc == 7, [R_xT[p], R_wina[kc]], [bk.res], signal=(kc == 7))
                        if g % 2 == 0:
                            op("dve", lambda: nc.vector.tensor_copy(hs[p][:, a:b], bk.f[:, 0:b - a]), [bk.res], [R_hs[p][g]])
                        else:
                            op("act", lambda: nc.scalar.copy(hs[p][:, a:b], bk.f[:, 0:b - a]), [bk.res], [R_hs[p][g]])

                S2L = int(os.environ.get("S2_LIMIT", 99))

                def S2(i):
                    use_rot("A2", [1, 2, 3, 4])
                    p = i % 2
                    cat = cats[p]
                    R_cat = R_cats[p]
                    h = hs[p]
                    Rh = lambda lo, hi: [R_hs[p][g] for g in grp_of(lo, hi)]
                    bk = nb()
                    tr(bk.f[0:16, 0:128], h[:, 1536:1552], id_f[:], Rh(1536, 1552) + [R_const], [bk.res])
                    op("act", lambda: nc.scalar.copy(gT17[p][0:16, :], bk.f[0:16, 0:128]), [bk.res], [R_gT[p]])
                    bk = nb()
                    mm(bk.f[:, 0:384], gT17[p][0:17, :], wup17[0:17, :], True, True, [R_gT[p], R_w], [bk.res])
                    op("act", lambda: nc.scalar.activation(l_sb[:], bk.f[:, 0:384], AF.Exp, scale=-1.0), [bk.res], [R_l])
                    op("act", lambda: nc.scalar.activation(l_sb[:], l_sb[:], AF.Ln, bias=ONE, scale=1.0), [R_l, R_const], [R_l])
                    if S2L < 1:
                        return
                    bk = nb()
                    mm(bk.f[:, 0:384], triu[:], l_sb[:], True, True, [R_const, R_l], [bk.res])
                    op("act", lambda: nc.scalar.activation(eb[:], bk.f[:, 0:384], AF.Exp, scale=-1.0 / 16.0), [bk.res], [R_eb])
                    op("act", lambda: nc.scalar.activation(enb[:], bk.f[:, 0:384], AF.Exp, scale=1.0 / 16.0), [bk.res], [R_eb])
                    op("dve", lambda: nc.vector.scalar_tensor_tensor(out=qd[:], in0=h[:, 0:384], scalar=96.0 ** -0.5, in1=eb[:], op0=ALU.mult, op1=ALU.mult),
                       Rh(0, 384) + [R_eb], [R_qd])
                    op("dve", lambda: nc.vector.tensor_tensor(out=ki[:], in0=h[:, 384:768], in1=enb[:], op=ALU.mult), Rh(384, 768) + [R_eb], [R_ki])
                    op("pool", lambda: nc.gpsimd.tensor_copy(v_b[:], h[:, 768:1536]), Rh(768, 1536), [R_vb])
                    if S2L < 2:
                        return
                    bk = nb()
                    for hd in range(4):
                        mm(bk.f[0:96, hd:hd + 1], l_sb[:, hd * 96:(hd + 1) * 96], ones_f[:, 0:1], True, True, [R_l, R_const], [bk.res], signal=(hd == 3))
                    op("act", lambda: nc.scalar.activation(dec[:], bk.f[0:96, 0:4], AF.Exp, scale=-1.0 / 16.0), [bk.res], [R_dec])
                    if S2L < 3:
                        return
                    bk = nb()
                    for hd in range(4):
                        tr(bk.b[0:96, hd * 128:(hd + 1) * 128], qd[:, hd * 96:(hd + 1) * 96], id_b[:], [R_qd, R_const], [bk.res], signal=(hd == 3))
                    op("act", lambda: nc.scalar.copy(qdT[:], bk.b[0:96, 0:512].rearrange("p (h t) -> p h t", h=4)), [bk.res], [R_qdT])
                    bk = nb()
                    for hd in range(4):
                        tr(bk.b[0:96, hd * 128:(hd + 1) * 128], ki[:, hd * 96:(hd + 1) * 96], id_b[:], [R_ki, R_const], [bk.res], signal=(hd == 3))
                    op("dve", lambda: nc.vector.tensor_copy(kiT[:], bk.b[0:96, 0:512].rearrange("p (h t) -> p h t", h=4)), [bk.res], [R_kiT])
                    if S2L < 4:
                        return
                    bk = nb()
                    for hd in range(4):
                        mm(bk.f[:, hd * 128:(hd + 1) * 128], kiT[:, hd, :], qdT[:, hd, :], True, True, [R_kiT, R_qdT], [bk.res], signal=(hd == 3))
                    op("dve", lambda: nc.vector.tensor_tensor(out=attm[:], in0=bk.f[:].rearrange("p (h t) -> p h t", h=4),
                                                              in1=triu[:].unsqueeze(1).broadcast_to([128, 4, 128]), op=ALU.mult),
                       [bk.res, R_const], [R_attm])
                    if S2L < 5:
                        return
                    ob = [nb(), nb()]
                    for hd in range(4):
                        bo = ob[hd // 2]
                        oo = bo.f[:, (hd % 2) * 192:(hd % 2 + 1) * 192]
                        mm(oo, attm[:, hd, :], v_b[:, hd * 192:(hd + 1) * 192], True, False, [R_attm, R_vb], [bo.res], signal=False)
                        mm(oo, qdT[:, hd, :], S_b[:, hd, :], False, True, [R_qdT, R_Sb[hd]], [bo.res], signal=True)
                    if S2L < 6:
                        return
                    kb = [nb(), nb()]
                    for hd in range(4):
                        bkv = kb[hd // 2]
                        kk = bkv.f[0:96, (hd % 2) * 192:(hd % 2 + 1) * 192]
                        mm(kk, ki[:, hd * 96:(hd + 1) * 96], v_b[:, hd * 192:(hd + 1) * 192], True, True, [R_ki, R_vb], [bkv.res])
                        op("dve", lambda: nc.vector.tensor_scalar(kvd[:, hd, :], kk, dec[:, hd:hd + 1], None, op0=ALU.mult), [bkv.res, R_dec], [R_kvd[hd]])
                        op("dve", lambda: nc.vector.scalar_tensor_tensor(out=Sst[:, hd, :], in0=Sst[:, hd, :], scalar=dec[:, hd:hd + 1], in1=kvd[:, hd, :],
                                                                         op0=ALU.mult, op1=ALU.add), [R_S[hd], R_dec, R_kvd[hd]], [R_S[hd]])
                        op("pool", lambda: nc.gpsimd.tensor_copy(S_b[:, hd, :], Sst[:, hd, :]), [R_S[hd]], [R_Sb[hd]])
                    if S2L < 7:
                        return
                    for j in range(2):
                        op("act", lambda: nc.scalar.activation(sq[:, j * 384:(j + 1) * 384], ob[j].f[:, 0:384], AF.Square), [ob[j].res], [R_sq])
                    S7 = int(os.environ.get("S7", 99))
                    if S7 < 2:
                        return
                    op("dve", lambda: nc.vector.tensor_reduce(out=ss[:, 0:4], in_=sq[:].rearrange("p (h e) -> p h e", h=4), axis=AX.X, op=ALU.add), [R_sq], [R_ss])
                    if S7 < 3:
                        return
                    op("act", lambda: nc.scalar.activation(ss[:, 0:4], ss[:, 0:4], AF.Sqrt, bias=EPS_RMS, scale=1.0 / 192.0), [R_ss, R_const], [R_ss])
                    if S7 < 4:
                        return
                    op("dve", lambda: nc.vector.reciprocal(ss[:, 4:8], ss[:, 0:4]), [R_ss], [R_ss])
                    if S7 < 5:
                        return
                    for hd in range(4):
                        op("dve", lambda: nc.vector.scalar_tensor_tensor(out=on[:, hd * 192:(hd + 1) * 192], in0=ob[hd // 2].f[:, (hd % 2) * 192:(hd % 2 + 1) * 192],
                                                                         scalar=ss[:, 4 + hd:5 + hd], in1=gn_b[:], op0=ALU.mult, op1=ALU.mult),
                           [ob[hd // 2].res, R_ss, R_w], [R_on])
                    if S7 < 6:
                        return
                    op("act", lambda: nc.scalar.activation(sr[:], h[:, 1552:2320], AF.Silu), Rh(1552, 2320), [R_sr])
                    if S7 < 7:
                        return
                    VV = os.environ.get("VV", "0")
                    if VV == "1":
                        op("dve", lambda: nc.vector.tensor_tensor(out=sq[:], in0=on[:], in1=sr[:], op=ALU.mult), [R_on, R_sr], [R_sq])
                    elif VV == "2":
                        op("dve", lambda: nc.vector.tensor_copy(cat[:, 0:768], on[:]), [R_on], [R_cat])
                    elif VV == "3":
                        op("dve", lambda: nc.vector.tensor_copy(cat[:, 0:768], sr[:]), [R_sr], [R_cat])
                    else:
                        op("dve", lambda: nc.vector.tensor_tensor(out=cat[:, 0:768], in0=on[:], in1=sr[:], op=ALU.mult), [R_on, R_sr], [R_cat])

                def S2m(i):
                    p = i % 2
                    op("pool", lambda: nc.gpsimd.tensor_copy(mq_b[:], hs[p][:, 2320:2576]), [R_hs[p][g] for g in grp_of(2320, 2576)], [R_mqb])
                    mem_attn(0, memt, mq_b, R_mqb, cats[p][:, 768:1024], R_catm[p])

                def S3a(i):
                    use_rot("A3a", [5])
                    out_ln_router(0, i, outt, cats[i % 2], [R_cats[i % 2], R_catm[i % 2]], xt[i % 4], R_xt[i % 4], wout_b, g_b, b_b, R_wout, R_lnp, part="a")

                def SMB(step):
                    use_rot("A3b", [6, 7])
                    if 0 <= step - 1 < NTILE:
                        S2m(step - 1)
                    if 0 <= step - 3 < NTILE:
                        out_ln_router(0, step - 3, outt, None, None, xt[(step - 3) % 4], R_xt[(step - 3) % 4], wout_b, g_b, b_b, R_wout, R_lnp, part="b")

                for step in range(NTILE + 3):
                    chains = []
                    if step < NTILE:
                        chains.append(lambda: S1(step))
                    if 0 <= step - 1 < NTILE:
                        chains.append(lambda: S2(step - 1))
                    if 0 <= step - 2 < NTILE:
                        chains.append(lambda: S3a(step - 2))
                    if 0 <= step - 1 < NTILE or 0 <= step - 3 < NTILE:
                        chains.append(lambda: SMB(step))
                    COOP.run(chains)

        def phase_M(l, src, R_src, dst, R_dst, gsrc, bsrc):
            with contextlib.ExitStack() as es:
                set_rot(range(8))
                xTs = sbt(es, "xTs", [128, 8, ST_TOK], BF16)
                acc = sbt(es, "acc", [128, ST_TILES, D], F32)
                NTB = ST_TOK // 512
                R_xTs = [Res("xTs%d" % tb) for tb in range(NTB)]
                R_acc = [Res("acc%d" % t) for t in range(ST_TILES)]
                wg = [sbt(es, "wg%d" % p, [128, 8, 512], BF16) for p in range(2)]
                wu = [sbt(es, "wu%d" % p, [128, 8, 512], BF16) for p in range(2)]
                wd = [sbt(es, "wd%d" % p, [128, 4, D], BF16) for p in range(2)]
                R_wg = [Res("wg%d" % p) for p in range(2)]
                R_wu = [Res("wu%d" % p) for p in range(2)]
                R_wd = [Res("wd%d" % p) for p in range(2)]
                HT = [sbt(es, "HT%d" % p, [128, 4, 512], BF16) for p in range(2)]
                R_HT = [Res("HT%d" % p) for p in range(2)]
                sg = [sbt(es, "sg%d" % p, [128, 512], F32) for p in range(2)]
                R_sg = [Res("sg%d" % p) for p in range(2)]
                g_b, b_b, R_lnp = load_ln(es, l, gsrc, bsrc, "M%d" % l)
                NXR = 4
                xr = [sbt(es, "xr%d" % p, [128, D], F32) for p in range(NXR)]
                R_xr = [Res("xr%d" % p) for p in range(NXR)]
                lnt = (sbt(es, "m_st", [128, 12], F32), sbt(es, "m_mv", [128, 2], F32), sbt(es, "m_rs", [128, 2], F32))
                R_lnt = Res("mlnt")
                NST = S // ST_TOK
                steps = [(st, e) for st in range(NST) for e in range(NEXP)]

                def load_w(k):
                    st, e = steps[k]
                    p = k % 2
                    T.dma("pool", "lwg%d" % p, wg[p][:], w_exp_gate[l, e].rearrange("(kc p) f -> p kc f", p=128), writes=[R_wg[p]])
                    T.dma("pool", "lwu%d" % p, wu[p][:], w_exp_up[l, e].rearrange("(kc p) f -> p kc f", p=128), writes=[R_wu[p]])
                    T.dma("pool", "lwd%d" % p, wd[p][:], w_exp_down[l, e].rearrange("(fc p) d -> p fc d", p=128), writes=[R_wd[p]])

                def load_x(st):
                    for tb in range(NTB):
                        t0 = st * ST_TOK + tb * 512
                        T.dma("sp", "ldxTs%d" % tb, xTs[:, :, tb * 512:(tb + 1) * 512], XT[:, :, t0:t0 + 512],
                              reads=R_XTd[t0 // 128:t0 // 128 + 4], writes=[R_xTs[tb]])

                blocks = [(k, tb) for k in range(len(steps)) for tb in range(NTB)]

                def GU(bi):
                    k, tb = blocks[bi]
                    st, e = steps[k]
                    p = k % 2
                    hp = bi % 2
                    last = (e == NEXP - 1)
                    for fc in range(4):
                        bg = nb()
                        for kc in range(8):
                            mm(bg.f[:], wg[p][:, kc, fc * 128:(fc + 1) * 128], xTs[:, kc, tb * 512:(tb + 1) * 512], kc == 0, kc == 7,
                               [R_wg[p], R_xTs[tb]], [bg.res], signal=(kc == 7))
                        bu = nb()
                        for kc in range(8):
                            mm(bu.f[:], wu[p][:, kc, fc * 128:(fc + 1) * 128], xTs[:, kc, tb * 512:(tb + 1) * 512], kc == 0, kc == 7,
                               [R_wu[p], R_xTs[tb]], [bu.res], signal=(kc == 7))
                        sp_ = fc % 2
                        op("act", lambda: nc.scalar.activation(sg[sp_][:], bg.f[:], AF.Silu), [bg.res], [R_sg[sp_]])
                        op("dve", lambda: nc.vector.tensor_tensor(out=HT[hp][:, fc, :], in0=bu.f[:], in1=sg[sp_][:], op=ALU.mult),
                           [bu.res, R_sg[sp_]], [R_HT[hp]])
                    if last and st + 1 < NST:
                        t0 = (st + 1) * ST_TOK + tb * 512
                        T.dma("sp", "ldxTs%d" % tb, xTs[:, :, tb * 512:(tb + 1) * 512], XT[:, :, t0:t0 + 512],
                              reads=R_XTd[t0 // 128:t0 // 128 + 4], writes=[R_xTs[tb]])

                def DOWN(bi):
                    k, tb = blocks[bi]
                    st, e = steps[k]
                    p = k % 2
                    hp = bi % 2
                    last = (e == NEXP - 1)
                    if last:
                        for tt in range(4):
                            gi = st * ST_TILES + tb * 4 + tt
                            T.dma("sp", "ldxr%d" % (gi % NXR), xr[gi % NXR][:], src[gi * 128:(gi + 1) * 128, :], reads=[R_src[gi]], writes=[R_xr[gi % NXR]])
                    for tt in range(4):
                        ti = tb * 4 + tt
                        gi = st * ST_TILES + ti
                        for dh in range(2):
                            bo = nb()
                            for fc in range(4):
                                mm(bo.f[:], HT[hp][:, fc, tt * 128:(tt + 1) * 128], wd[p][:, fc, dh * 512:(dh + 1) * 512], fc == 0, fc == 3,
                                   [R_HT[hp], R_wd[p]], [bo.res], signal=(fc == 3))
                            a_ap = acc[:, ti, dh * 512:(dh + 1) * 512]
                            if e == 0:
                                op("dve", lambda: nc.vector.tensor_scalar(a_ap, bo.f[:], gates_all[:, gi, e:e + 1], None, op0=ALU.mult),
                                   [bo.res, R_gates[gi]], [R_acc[ti]])
                            else:
                                op("dve", lambda: nc.vector.scalar_tensor_tensor(out=a_ap, in0=bo.f[:], scalar=gates_all[:, gi, e:e + 1], in1=a_ap,
                                                                                 op0=ALU.mult, op1=ALU.add),
                                   [bo.res, R_gates[gi], R_acc[ti]], [R_acc[ti]])
                        if last:
                            q = gi % NXR
                            op("dve", lambda: nc.vector.scalar_tensor_tensor(out=xr[q][:], in0=xr[q][:], scalar=ALPHA, in1=acc[:, ti, :], op0=ALU.mult, op1=ALU.add),
                               [R_xr[q], R_acc[ti]], [R_xr[q]])
                            layer_norm(lnt, xr[q][:], R_xr[q], g_b[:], b_b[:], R_lnp, R_lnt, xr[q][:], R_xr[q])
                            T.dma("sp", "stM%d" % q, dst[gi * 128:(gi + 1) * 128, :], xr[q][:], reads=[R_xr[q]], writes=[R_dst[gi]])
                    if tb == NTB - 1 and k + 2 < len(steps):
                        load_w(k + 2)

                load_w(0)
                load_w(1)
                load_x(0)
                GU(0)
                for bi in range(len(blocks)):
                    if bi + 1 < len(blocks):
                        GU(bi + 1)
                    DOWN(bi)

        if stop_after != "P0":
            phase_A()
            _barrier(T)
        if dbg:
            T.dma("sp", "dg", DG[:, 0, :, :], gates_all[:], reads=R_gates)
        R_out = [Res("out%d" % i) for i in range(NTILE)]

        def phase_B(v_all, R_vall, kbias, R_kb):
            with contextlib.ExitStack() as es:
                use_rot("B0", range(8))
                wkv = sbt(es, "wkv", [128, 8, KV_IN], BF16)
                winb = sbt(es, "winb", [128, 8, B_IN], BF16)
                R_wkv = [Res("wkv%d" % kc) for kc in range(8)]
                R_winb = [Res("winb%d" % kc) for kc in range(8)]
                wkv_v = w_kv_shared.rearrange("(kc p) n -> p kc n", p=128)
                winb_v = w_in_b.rearrange("(kc p) n -> p kc n", p=128)
                for kc in range(8):
                    T.dma("pool", "wB_%d" % kc, wkv[:, kc, :], wkv_v[:, kc, :], writes=[R_wkv[kc]])
                    T.dma("pool", "wBb_%d" % kc, winb[:, kc, :], winb_v[:, kc, :], writes=[R_winb[kc]])
                gk_b = sbt(es, "gk_b", [128, 64], F32)
                gq_b = sbt(es, "gq_b", [128, 64], F32)
                bf_b = sbt(es, "bf_b", [128, 12], F32)
                R_pb = Res("parB")
                T.dma("sp", "pB0", gk_b[:], k_norm_g.broadcast_to([128, 64]), writes=[R_pb])
                T.dma("sp", "pB1", gq_b[:], q_norm_g.broadcast_to([128, 64]), writes=[R_pb])
                T.dma("sp", "pB2", bf_b[:], b_forget.broadcast_to([128, 12]), writes=[R_pb])
                xt = [sbt(es, "bxt%d" % p, [128, D], F32) for p in range(2)]
                xT = [sbt(es, "bxT%d" % p, [128, 8, 128], BF16) for p in range(2)]
                xb = [sbt(es, "bxb%d" % p, [128, D], BF16) for p in range(2)]
                R_xb = [Res("bxb%d" % p) for p in range(2)]
                hk = [sbt(es, "hk%d" % p, [128, KV_IN], F32) for p in range(2)]
                hb = [sbt(es, "hb%d" % p, [128, B_IN], F32) for p in range(2)]
                R_xt = [Res("bxt%d" % p) for p in range(2)]
                R_xT = [Res("bxT%d" % p) for p in range(2)]
                GK = [(0, 512), (512, 1024), (1024, 1536), (1536, KV_IN)]
                GB = [(0, 512), (512, 1024), (1024, 1536), (1536, B_IN)]
                R_hk = [[Res("hk%d_%d" % (p, g)) for g in range(4)] for p in range(2)]
                R_hb = [[Res("hb%d_%d" % (p, g)) for g in range(4)] for p in range(2)]
                sqt = sbt(es, "b_sq", [128, 768], F32)
                nrm = sbt(es, "b_nrm", [128, 768], F32)
                ssn = sbt(es, "b_ss", [128, 24], F32)
                k_augs = [sbt(es, "k_aug%d" % p, [128, 12, 72], BF16) for p in range(2)]
                q_augs = [sbt(es, "q_aug%d" % p, [128, 12, 72], BF16) for p in range(2)]
                lf = sbt(es, "b_lf", [128, 12], F32)
                carry = sbt(es, "b_carry", [128, 12], F32)
                t1 = sbt(es, "b_t1", [128, 12], F32)
                ogq = sbt(es, "b_ogq", [128, D], BF16)
                qT_t = sbt(es, "qT_t", [66, 12, 128], BF16)
                kT_t = sbt(es, "kT_t", [66, 12, 128], BF16)
                R_sq, R_nrm, R_ssn, R_lf, R_carry, R_t1, R_ogq, R_qTt, R_kTt = (
                    Res(n) for n in ["bsq", "bnrm", "bss", "lf", "carry", "t1", "ogq", "qTt", "kTt"])
                R_kas = [Res("ka%d" % p) for p in range(2)]
                R_qas = [Res("qa%d" % p) for p in range(2)]
                for p in range(2):
                    op("dve", lambda: nc.vector.memset(k_augs[p][:], 1.0), [], [R_kas[p]])
                    op("dve", lambda: nc.vector.memset(q_augs[p][:], 0.0), [], [R_qas[p]])
                op("dve", lambda: nc.vector.memset(carry[:], 0.0), [], [R_carry])
                for c4 in range(4):
                    op("pool", lambda: nc.gpsimd.memset(v_all[:, c4 * 8:(c4 + 1) * 8, :, :], 1.0), [], [R_vall[c4 * 8 + j] for j in range(8)])

                def grp_of(G, lo, hi):
                    return [g for g, (a, b) in enumerate(G) if a < hi and b > lo]

                def S1(i):
                    use_rot("B1", [0, 1, 2])
                    p = i % 2
                    T.dma("sp", "bldx%d" % p, xt[p][:], XB[i * 128:(i + 1) * 128, :], reads=[R_XB[i]], writes=[R_xt[p]])
                    op("pool", lambda: nc.gpsimd.tensor_copy(xb[p][:], xt[p][:]), [R_xt[p]], [R_xb[p]])
                    bk = nb()
                    for kc in range(8):
                        tr(bk.b[:, kc * 128:(kc + 1) * 128], xb[p][:, kc * 128:(kc + 1) * 128], id_b[:], [R_xb[p], R_const], [bk.res], signal=(kc == 7))
                    op("act", lambda: nc.scalar.copy(xT[p][:], bk.b[:].rearrange("p (k t) -> p k t", k=8)), [bk.res], [R_xT[p]])
                    n = 0
                    for (G, w, Rw, dst, Rd) in ((GK, wkv, R_wkv, hk[p], R_hk[p]), (GB, winb, R_winb, hb[p], R_hb[p])):
                        for g, (a, b) in enumerate(G):
                            bk = nb()
                            for kc in range(8):
                                mm(bk.f[:, 0:b - a], xT[p][:, kc, :], w[:, kc, a:b], kc == 0, kc == 7, [R_xT[p], Rw[kc]], [bk.res], signal=(kc == 7))
                            if n % 2 == 0:
                                op("dve", lambda: nc.vector.tensor_copy(dst[:, a:b], bk.f[:, 0:b - a]), [bk.res], [Rd[g]])
                            else:
                                op("act", lambda: nc.scalar.copy(dst[:, a:b], bk.f[:, 0:b - a]), [bk.res], [Rd[g]])
                            n += 1

                def rmsn(src, Rsrc, gb, aug, R_aug, off):
                    v3 = lambda a: a.rearrange("p (h e) -> p h e", h=12)
                    op("act", lambda: nc.scalar.activation(sqt[:], src, AF.Square), Rsrc, [R_sq])
                    op("dve", lambda: nc.vector.tensor_reduce(out=ssn[:, off:off + 12], in_=v3(sqt[:]), axis=AX.X, op=ALU.add), [R_sq], [R_ssn])
                    op("act", lambda: nc.scalar.activation(ssn[:, off:off + 12], ssn[:, off:off + 12], AF.Sqrt, bias=EPS_RMS, scale=1.0 / 64.0), [R_ssn, R_const], [R_ssn])
                    op("dve", lambda: nc.vector.reciprocal(ssn[:, off:off + 12], ssn[:, off:off + 12]), [R_ssn], [R_ssn])
                    op("dve", lambda: nc.vector.tensor_tensor(out=v3(nrm[:]), in0=v3(src), in1=ssn[:, off:off + 12].unsqueeze(2).broadcast_to([128, 12, 64]), op=ALU.mult),
                       Rsrc + [R_ssn], [R_nrm])
                    op("dve", lambda: nc.vector.tensor_tensor(out=aug[:, :, 0:64], in0=v3(nrm[:]), in1=gb[:].unsqueeze(1).broadcast_to([128, 12, 64]), op=ALU.mult),
                       [R_nrm, R_pb], [R_aug])

                def S2(i):
                    use_rot("B2", [3])
                    p = i % 2
                    k_aug, q_aug, R_ka, R_qa = k_augs[p], q_augs[p], R_kas[p], R_qas[p]
                    Rk = lambda lo, hi: [R_hk[p][g] for g in grp_of(GK, lo, hi)]
                    Rb = lambda lo, hi: [R_hb[p][g] for g in grp_of(GB, lo, hi)]
                    rmsn(hk[p][:, 0:768], Rk(0, 768), gk_b, k_aug, R_ka, 0)
                    rmsn(hb[p][:, 0:768], Rb(0, 768), gq_b, q_aug, R_qa, 12)
                    op("dve", lambda: nc.vector.tensor_tensor(out=lf[:], in0=hk[p][:, 1536:1548], in1=bf_b[:], op=ALU.add), Rk(1536, 1548) + [R_pb], [R_lf])
                    op("act", lambda: nc.scalar.activation(lf[:], lf[:], AF.Exp, scale=-1.0), [R_lf], [R_lf])
                    op("act", lambda: nc.scalar.activation(lf[:], lf[:], AF.Ln, bias=ONE, scale=1.0), [R_lf, R_const], [R_lf])
                    bk = nb()
                    mm(bk.f[:, 0:12], triu[:], lf[:], True, True, [R_const, R_lf], [bk.res], signal=False)
                    mm(bk.f[:, 16:28], ones_f[:], lf[:], True, True, [R_const, R_lf], [bk.res], signal=True)
                    op("dve", lambda: nc.vector.tensor_tensor(out=kbias[:, i, :], in0=bk.f[:, 0:12], in1=carry[:], op=ALU.add), [bk.res, R_carry], [R_kb[i]])
                    op("dve", lambda: nc.vector.tensor_tensor(out=carry[:], in0=bk.f[:, 16:28], in1=carry[:], op=ALU.add), [bk.res, R_carry], [R_carry])
                    op("dve", lambda: nc.vector.tensor_scalar(t1[:], kbias[:, i, :], -8.0, None, op0=ALU.mult), [R_kb[i]], [R_t1])
                    op("dve", lambda: nc.vector.tensor_copy(q_aug[:, :, 64], t1[:]), [R_t1], [R_qa])
                    op("dve", lambda: nc.vector.tensor_tensor(out=q_aug[:, :, 65], in0=t1[:], in1=q_aug[:, :, 64], op=ALU.subtract), [R_t1, R_qa], [R_qa])
                    op("pool", lambda: nc.gpsimd.tensor_copy(v_all[:, i, :, 0:64], hk[p][:, 768:1536].rearrange("p (h e) -> p h e", h=12)), Rk(768, 1536), [R_vall[i]])
                    op("act", lambda: nc.scalar.activation(ogq[:, 0:768], hb[p][:, 768:1536], AF.Sigmoid), Rb(768, 1536), [R_ogq])
                    op("pool", lambda: nc.gpsimd.tensor_copy(ogq[:, 768:1024], hb[p][:, 1536:1792]), Rb(1536, 1792), [R_ogq])
                    T.dma("sp", "sOGQ", OGQ[i * 128:(i + 1) * 128, :], ogq[:], reads=[R_ogq], writes=[R_OGQ[i]])

                def S2b(i):
                    use_rot("B2b", [4, 5, 6, 7])
                    p = i % 2
                    k_aug, q_aug, R_ka, R_qa = k_augs[p], q_augs[p], R_kas[p], R_qas[p]
                    for (aug, R_aug, tt, R_tt, dram, R_d, key, eng) in ((q_aug, R_qa, qT_t, R_qTt, QT, R_QTd, "sQT", "act"), (k_aug, R_ka, kT_t, R_kTt, KT, R_KTd, "sKT", "dve")):
                        for (h0, h1) in ((0, 8), (8, 12)):
                            bk = nb()
                            for h in range(h0, h1):
                                tr(bk.b[0:66, (h - h0) * 128:(h - h0 + 1) * 128], aug[:, h, 0:66], id_b[:], [R_aug, R_const], [bk.res], signal=(h == h1 - 1))
                            src = bk.b[0:66, 0:(h1 - h0) * 128].rearrange("p (h t) -> p h t", h=h1 - h0)
                            if eng == "act":
                                op("act", lambda: nc.scalar.copy(tt[:, h0:h1, :], src), [bk.res], [R_tt])
                            else:
                                op("dve", lambda: nc.vector.tensor_copy(tt[:, h0:h1, :], src), [bk.res], [R_tt])
                        T.dma("sp", key, dram.rearrange("h p t -> p h t")[:, :, i * 128:(i + 1) * 128], tt[:], reads=[R_tt], writes=[R_d[i]])

                for step in range(NTILE + 2):
                    chains = []
                    if step < NTILE:
                        chains.append(lambda: S1(step))
                    if 0 <= step - 1 < NTILE:
                        chains.append(lambda: S2(step - 1))
                    if 0 <= step - 2 < NTILE:
                        chains.append(lambda: S2b(step - 2))
                    COOP.run(chains)

        def phase_C(v_all, R_vall, kbias, R_kb, o_all, R_oall):
            with contextlib.ExitStack() as es:
                kTh = [sbt(es, "kTh%d" % p, [66, S], BF16) for p in range(2)]
                qTh = [sbt(es, "qTh%d" % p, [66, S], BF16) for p in range(2)]
                R_kTh = [Res("kTh%d" % p) for p in range(2)]
                R_qTh = [Res("qTh%d" % p) for p in range(2)]
                NPB = 6
                pex = [sbt(es, "pex%d" % j, [128, 512], BF16) for j in range(NPB)]
                R_pex = [Res("pex%d" % j) for j in range(NPB)]
                rdc = sbt(es, "rdc", [128, 4], F32)
                R_rdc = Res("rdc")
                set_rot([4, 5, 6, 7])
                acc_b = [banks[j] for j in range(4)]

                def load_h(h):
                    p = h % 2
                    T.dma("sp", "ldk%d" % p, kTh[p][:], KT[h], reads=R_KTd, writes=[R_kTh[p]])
                    T.dma("sp", "ldq%d" % p, qTh[p][:], QT[h], reads=R_QTd, writes=[R_qTh[p]])

                load_h(0)
                units = [(h, Q, c) for h in range(12) for Q in range(8) for c in range(4 * Q + 4)]
                sb_of = {}
                DEPTH = 3

                def qk(u):
                    h, Q, c = units[u]
                    p = h % 2
                    if Q == 0 and c == 0 and h + 1 < 12:
                        load_h(h + 1)
                    lo = 128 * max(0, c - 4 * Q)
                    bs = nb()
                    mm(bs.f[:, lo:512], kTh[p][0:66, c * 128:(c + 1) * 128], qTh[p][0:66, Q * 512 + lo:(Q + 1) * 512], True, True,
                       [R_kTh[p], R_qTh[p]], [bs.res])
                    sb_of[u] = bs

                def rest(u):
                    h, Q, c = units[u]
                    j0 = max(0, c - 4 * Q)
                    lo = 128 * j0
                    bs = sb_of.pop(u)
                    pj = u % NPB
                    op("act", lambda: nc.scalar.activation(pex[pj][:, lo:512], bs.f[:, lo:512], AF.Exp, bias=kbias[:, c, h:h + 1], scale=0.125),
                       [bs.res, R_kb[c]], [R_pex[pj]])
                    if c >= 4 * Q:
                        op("dve", lambda: nc.vector.tensor_tensor(out=pex[pj][:, lo:lo + 128], in0=pex[pj][:, lo:lo + 128], in1=triu_b[:], op=ALU.mult),
                           [R_pex[pj], R_const], [R_pex[pj]])
                    for j in range(j0, 4):
                        mm(acc_b[j].f[:, 0:65], pex[pj][:, j * 128:(j + 1) * 128], v_all[:, c, h, 0:65], c == 0, c == 4 * Q + j,
                           [R_pex[pj], R_vall[c]], [acc_b[j].res], signal=(j == 3))
                    if c == 4 * Q + 3:
                        for j in range(4):
                            ti = 4 * Q + j
                            op("dve", lambda: nc.vector.reciprocal(rdc[:, j:j + 1], acc_b[j].f[:, 64:65]), [acc_b[j].res], [R_rdc])
                            op("dve", lambda: nc.vector.tensor_scalar(o_all[:, ti, h * 64:(h + 1) * 64], acc_b[j].f[:, 0:64], rdc[:, j:j + 1], None, op0=ALU.mult),
                               [acc_b[j].res, R_rdc], [R_oall[ti]])

                for u in range(min(DEPTH, len(units))):
                    qk(u)
                for u in range(len(units)):
                    if u + DEPTH < len(units):
                        qk(u + DEPTH)
                    rest(u)

        def phase_D(o_all, R_oall):
            with contextlib.ExitStack() as es:
                use_rot("D0", range(8))
                wout_b = sbt(es, "wout_d", [128, 8, D], BF16)
                R_wout = Res("woutD")
                T.dma("pool", "wD", wout_b[:], w_out[1].rearrange("(kc p) n -> p kc n", p=128), writes=[R_wout])
                g_b, b_b, R_lnp = load_ln(es, 1, ln_mix_g, ln_mix_b, "D")
                NX = 6
                xt = [sbt(es, "dxt%d" % p, [128, D], F32) for p in range(NX)]
                og = [sbt(es, "dog%d" % p, [128, D], BF16) for p in range(2)]
                R_xt = [Res("dxt%d" % p) for p in range(NX)]
                R_og = [Res("dog%d" % p) for p in range(2)]
                cats = [sbt(es, "dcat%d" % p, [128, D], BF16) for p in range(4)]
                R_cats = [Res("dcat%d" % p) for p in range(4)]
                memt = alloc_mem_tiles(es)
                outts = [alloc_out_tiles(es), alloc_out_tiles(es)]

                def S1(i):
                    p = i % 2
                    T.dma("sp", "dldx%d" % (i % NX), xt[i % NX][:], XB[i * 128:(i + 1) * 128, :], reads=[R_XB[i]], writes=[R_xt[i % NX]])
                    T.dma("sp", "dldo%d" % p, og[p][:], OGQ[i * 128:(i + 1) * 128, :], reads=[R_OGQ[i]], writes=[R_og[p]])

                def S2(i):
                    p = i % 2
                    cat, R_cat = cats[i % 4], R_cats[i % 4]
                    op("dve", lambda: nc.vector.tensor_tensor(out=cat[:, 0:768], in0=o_all[:, i, :], in1=og[p][:, 0:768], op=ALU.mult), [R_oall[i], R_og[p]], [R_cat])
                    mem_attn(1, memt, og[p][:, 768:1024], R_og[p], cat[:, 768:1024], R_cat)

                def M(s2):
                    use_rot("D2", [0, 1])
                    S1(2 * s2)
                    S1(2 * s2 + 1)
                    S2(2 * s2)
                    S2(2 * s2 + 1)

                def S3a(i):
                    use_rot("D3a%d" % (i % 2), [2, 3] if i % 2 == 0 else [4, 5])
                    out_ln_router(1, i, outts[i % 2], cats[i % 4], R_cats[i % 4], xt[i % NX], R_xt[i % NX], wout_b, g_b, b_b, R_wout, R_lnp, part="a")

                def S3b(i):
                    use_rot("D3b%d" % (i % 2), [6] if i % 2 == 0 else [7])
                    out_ln_router(1, i, outts[i % 2], None, None, xt[i % NX], R_xt[i % NX], wout_b, g_b, b_b, R_wout, R_lnp, part="b")

                NP = NTILE // 2
                for s2 in range(NP + 2):
                    chains = []
                    if s2 < NP:
                        chains.append(lambda: M(s2))
                    if 0 <= s2 - 1 < NP:
                        chains.append(lambda: S3a(2 * (s2 - 1)))
                        chains.append(lambda: S3a(2 * (s2 - 1) + 1))
                    if 0 <= s2 - 2 < NP:
                        chains.append(lambda: S3b(2 * (s2 - 2)))
                        chains.append(lambda: S3b(2 * (s2 - 2) + 1))
                    COOP.run(chains)

        if stop_after in ("A", "P0"):
            pass
        else:
            phase_M(0, XA, R_XA, XB, R_XB, ln_ffn_g, ln_ffn_b)
            _barrier(T)
            if stop_after != "M0":
                with contextlib.ExitStack() as esL:
                    v_all = sbt(esL, "v_all", [128, NTILE, 12, 72], BF16)
                    kbias = sbt(esL, "kbias", [128, NTILE, 12], F32)
                    R_vall = [Res("vall%d" % i) for i in range(NTILE)]
                    R_kb = [Res("kb%d" % i) for i in range(NTILE)]
                    phase_B(v_all, R_vall, kbias, R_kb)
                    _barrier(T)
                    if stop_after != "B":
                        with contextlib.ExitStack() as esO:
                            o_all = sbt(esO, "o_all", [128, NTILE, 768], BF16)
                            R_oall = [Res("oall%d" % i) for i in range(NTILE)]
                            phase_C(v_all, R_vall, kbias, R_kb, o_all, R_oall)
                            _barrier(T)
                            phase_D(o_all, R_oall)
                            _barrier(T)
                            if dbg:
                                T.dma("sp", "dg", DG[:, 1, :, :], gates_all[:], reads=R_gates)
                _barrier(T)
                if stop_after not in ("B", "D"):
                    phase_M(1, XA, R_XA, out, R_out, ln_ffn_g, ln_ffn_b)

        _barrier(T)
    print("build: ninst=%d nwait=%d" % (T.ninst, T.nwait))
    T.close()
    return nc


_CONSTS = None


def _consts():
    global _CONSTS
    if _CONSTS is None:
        ident = np.eye(128, dtype=np.float32)
        triu = np.triu(np.ones((128, 128), dtype=np.float32))
        _CONSTS = {"c_ident": ident, "c_triu": triu}
    return _CONSTS


def make_in_map(inputs, b):
    f = lambda a: np.ascontiguousarray(np.asarray(a, dtype=np.float32))
    m = {
        "x": f(inputs["x"][b]), "mem": f(inputs["mem"][b]),
        "w_in_a": f(inputs["w_in_a"][0]), "w_gate_up_a": f(inputs["w_gate_up_a"][0]),
        "b_gate_a": f(inputs["b_gate_a"]).reshape(1, 384), "gla_norm_g": f(inputs["gla_norm_g"]).reshape(1, 192),
        "w_in_b": f(inputs["w_in_b"][0]), "q_norm_g": f(inputs["q_norm_g"]).reshape(1, 64),
        "w_kv_shared": f(inputs["w_kv_shared"]), "b_forget": f(inputs["b_forget"]).reshape(1, 12),
        "k_norm_g": f(inputs["k_norm_g"]).reshape(1, 64), "w_mem_kv": f(inputs["w_mem_kv"]),
        "w_out": f(inputs["w_out"]), "ln_mix_g": f(inputs["ln_mix_g"]), "ln_mix_b": f(inputs["ln_mix_b"]),
        "ln_ffn_g": f(inputs["ln_ffn_g"]), "ln_ffn_b": f(inputs["ln_ffn_b"]),
        "w_router": f(inputs["w_router"]), "b_router": f(inputs["b_router"]).reshape(1, 16),
        "w_exp_gate": f(inputs["w_exp_gate"]), "w_exp_up": f(inputs["w_exp_up"]), "w_exp_down": f(inputs["w_exp_down"]),
    }
    m.update(_consts())
    return m


def kernel(**inputs):
    nc = build()
    in_maps = [make_in_map(inputs, b) for b in range(N_CORES)]
    res = run_bass_kernel_spmd(nc, in_maps, core_ids=list(range(N_CORES)))
    return np.stack([np.asarray(r["out"], dtype=np.float32) for r in res.results], axis=0)
```
